# Optimizing a Trainium2 kernel written in Bass

```python
import math
import jax, jax.numpy as jnp
from jax import lax
import numpy as np

D_MODEL = 1024
BATCH = 2
SEQ = 8192
DEPTH = 2

N_MIXERS = 2
N_EVEN = (DEPTH + 1) // 2
N_ODD = DEPTH // 2
ALPHA = (2.0 * DEPTH) ** 0.25
BETA_INIT = (8.0 * DEPTH) ** -0.25
LN_EPS = 1e-5

GDN_K_HEADS = 4
GDN_V_HEADS = 8
GDN_HEAD_K = 128
GDN_HEAD_V = 128
GDN_KDIM = GDN_K_HEADS * GDN_HEAD_K
GDN_VDIM = GDN_V_HEADS * GDN_HEAD_V
GDN_CONV = 4
GDN_CHUNK = 64
GDN_QKV = 2 * GDN_KDIM + GDN_VDIM
GDN_IN = GDN_QKV + GDN_VDIM + 2 * GDN_V_HEADS
GDN_EPS = 1e-6

SWA_Q_HEADS = 16
SWA_KV_HEADS = 2
SWA_GROUP = SWA_Q_HEADS // SWA_KV_HEADS
SWA_HEAD_DIM = 64
SWA_WINDOW = 128
SWA_BLOCK = 128
SWA_QDIM = SWA_Q_HEADS * SWA_HEAD_DIM
SWA_KVDIM = SWA_KV_HEADS * SWA_HEAD_DIM
SWA_IN = SWA_QDIM + 2 * SWA_KVDIM

REL_BUCKETS = 32
REL_MAX_DIST = 128

FFN_DIM = 2816
N_EXPERTS = 8
TOP_K = 2
EXPERT_DIM = 3584

kernel_name = 'hybrid_gdn_swa_moe_deepnorm'


def layer_norm(x, g, b):
    xf = x.astype(jnp.float32)
    mu = jnp.mean(xf, axis=-1, keepdims=True)
    var = jnp.mean(jnp.square(xf - mu), axis=-1, keepdims=True)
    return ((xf - mu) * lax.rsqrt(var + LN_EPS) * g.astype(jnp.float32) + b.astype(jnp.float32)).astype(x.dtype)


def l2_normalize(x):
    xf = x.astype(jnp.float32)
    return xf * lax.rsqrt(jnp.sum(xf * xf, axis=-1, keepdims=True) + GDN_EPS)


def causal_depthwise_conv(x, w):
    width = w.shape[0]
    return lax.conv_general_dilated(
        x, w[:, None, :].astype(x.dtype), window_strides=(1,), padding=[(width - 1, 0)],
        dimension_numbers=('NWC', 'WIO', 'NWC'), feature_group_count=x.shape[-1])


def gated_delta_rule_chunked(q, k, v, g, beta):
    bsz, t, h, dk = q.shape
    dv = v.shape[-1]
    c = GDN_CHUNK
    n = t // c
    f32 = jnp.float32

    def blocks(a):
        a = a.astype(f32).reshape((bsz, n, c, h) + a.shape[3:])
        return jnp.moveaxis(a, 2, 3)

    q, k, v, g, beta = blocks(q), blocks(k), blocks(v), blocks(g), blocks(beta)
    g_cum = jnp.cumsum(g, axis=-1)
    causal = jnp.tril(jnp.ones((c, c), dtype=bool))
    strict = jnp.tril(jnp.ones((c, c), dtype=bool), -1)
    diff = g_cum[..., :, None] - g_cum[..., None, :]
    decay = jnp.exp(jnp.where(causal, diff, -jnp.inf))
    k_beta = k * beta[..., None]
    v_beta = v * beta[..., None]
    a_kk = jnp.where(strict, jnp.einsum('bnhid,bnhjd->bnhij', k_beta, k) * decay, 0.0)
    eye = jnp.eye(c, dtype=f32)
    t_inv = lax.linalg.triangular_solve(eye + a_kk, jnp.broadcast_to(eye, a_kk.shape),
                                        left_side=True, lower=True, unit_diagonal=True)
    u = jnp.einsum('bnhij,bnhje->bnhie', t_inv, v_beta)
    w = jnp.einsum('bnhij,bnhjd->bnhid', t_inv, k_beta * jnp.exp(g_cum)[..., None])
    a_qk = jnp.einsum('bnhid,bnhjd->bnhij', q, k) * decay
    g_last = g_cum[..., -1]
    q_dec = q * jnp.exp(g_cum)[..., None]
    k_dec = k * jnp.exp(g_last[..., None] - g_cum)[..., None]
    xs = (jnp.moveaxis(q_dec, 1, 0), jnp.moveaxis(k_dec, 1, 0), jnp.moveaxis(u, 1, 0),
          jnp.moveaxis(w, 1, 0), jnp.moveaxis(a_qk, 1, 0), jnp.moveaxis(jnp.exp(g_last), 1, 0))

    def step(s, inp):
        qd, kd, uc, wc, aqk, dl = inp
        v_new = uc - jnp.einsum('bhcd,bhde->bhce', wc, s)
        o = jnp.einsum('bhcd,bhde->bhce', qd, s) + jnp.einsum('bhcs,bhse->bhce', aqk, v_new)
        s = s * dl[..., None, None] + jnp.einsum('bhcd,bhce->bhde', kd, v_new)
        return s, o

    s0 = jnp.zeros((bsz, h, dk, dv), f32)
    _, o = lax.scan(step, s0, xs)
    o = jnp.moveaxis(o, 0, 1)
    return jnp.moveaxis(o, 2, 3).reshape(bsz, t, h, dv)


def gated_deltanet(x, w_in, conv_w, a_log, dt_bias, norm_w, w_out):
    bsz, t, _ = x.shape
    proj = x @ w_in
    o1 = GDN_QKV
    o2 = o1 + GDN_VDIM
    o3 = o2 + GDN_V_HEADS
    qkv = jax.nn.silu(causal_depthwise_conv(proj[..., :o1], conv_w))
    z = proj[..., o1:o2].reshape(bsz, t, GDN_V_HEADS, GDN_HEAD_V)
    b_logit = proj[..., o2:o3]
    a_logit = proj[..., o3:]
    q = qkv[..., :GDN_KDIM].reshape(bsz, t, GDN_K_HEADS, GDN_HEAD_K)
    k = qkv[..., GDN_KDIM:2 * GDN_KDIM].reshape(bsz, t, GDN_K_HEADS, GDN_HEAD_K)
    v = qkv[..., 2 * GDN_KDIM:].reshape(bsz, t, GDN_V_HEADS, GDN_HEAD_V)
    rep = GDN_V_HEADS // GDN_K_HEADS
    q = jnp.repeat(l2_normalize(q) * (GDN_HEAD_K ** -0.5), rep, axis=2)
    k = jnp.repeat(l2_normalize(k), rep, axis=2)
    beta = jax.nn.sigmoid(b_logit.astype(jnp.float32))
    g = -jnp.exp(a_log.astype(jnp.float32)) * jax.nn.softplus(
        a_logit.astype(jnp.float32) + dt_bias.astype(jnp.float32))
    o = gated_delta_rule_chunked(q, k, v, g, beta)
    o = o * lax.rsqrt(jnp.mean(o * o, axis=-1, keepdims=True) + GDN_EPS) * norm_w.astype(jnp.float32)
    o = o * jax.nn.silu(z.astype(jnp.float32))
    return o.reshape(bsz, t, GDN_VDIM).astype(x.dtype) @ w_out


def t5_bucket(dist):
    max_exact = REL_BUCKETS // 2
    df = jnp.maximum(dist, 1).astype(jnp.float32)
    large = max_exact + (jnp.log(df / max_exact) / math.log(REL_MAX_DIST / max_exact)
                         * (REL_BUCKETS - max_exact)).astype(jnp.int32)
    large = jnp.minimum(large, REL_BUCKETS - 1)
    return jnp.where(dist < max_exact, dist, large)


def swa_sink_attention(x, w_in, b_in, sinks, rel_bias, w_out):
    bsz, t, _ = x.shape
    nb = t // SWA_BLOCK
    proj = x @ w_in + b_in
    q = proj[..., :SWA_QDIM].reshape(bsz, nb, SWA_BLOCK, SWA_KV_HEADS, SWA_GROUP, SWA_HEAD_DIM)
    k = proj[..., SWA_QDIM:SWA_QDIM + SWA_KVDIM]
    v = proj[..., SWA_QDIM + SWA_KVDIM:]

    def band(a):
        ab = a.reshape(bsz, nb, SWA_BLOCK, SWA_KV_HEADS, SWA_HEAD_DIM)
        prev = jnp.pad(ab, ((0, 0), (1, 0), (0, 0), (0, 0), (0, 0)))[:, :-1]
        return jnp.concatenate([prev, ab], axis=2)

    kb, vb = band(k), band(v)
    s = jnp.einsum('bnqkgd,bnskd->bnkgqs', q, kb).astype(jnp.float32) * (SWA_HEAD_DIM ** -0.5)
    qi = jnp.arange(SWA_BLOCK)[:, None]
    kj = jnp.arange(2 * SWA_BLOCK)[None, :]
    dist = qi + SWA_BLOCK - kj
    bias = rel_bias.astype(jnp.float32)[t5_bucket(jnp.maximum(dist, 0))]
    bias = jnp.transpose(bias, (2, 0, 1)).reshape(SWA_KV_HEADS, SWA_GROUP, SWA_BLOCK, 2 * SWA_BLOCK)
    band_ok = (dist >= 0) & (dist < SWA_WINDOW)
    blk = jnp.arange(nb)[:, None, None]
    mask = band_ok[None] & ((blk > 0) | (kj >= SWA_BLOCK)[None])
    s = jnp.where(mask[None, :, None, None], s + bias, -jnp.inf)
    sink = jnp.broadcast_to(sinks.astype(jnp.float32).reshape(SWA_KV_HEADS, SWA_GROUP, 1, 1),
                            s.shape[:-1] + (1,))
    p = jax.nn.softmax(jnp.concatenate([s, sink], axis=-1), axis=-1)[..., :-1]
    o = jnp.einsum('bnkgqs,bnskd->bnqkgd', p.astype(vb.dtype), vb).reshape(bsz, t, SWA_QDIM)
    return o @ w_out


def swiglu(x, w_gate, w_up, w_down):
    return (jax.nn.silu(x @ w_gate) * (x @ w_up)) @ w_down


def moe_swiglu(x, w_router, w_gate, w_up, w_down):
    bsz, t, d = x.shape
    xf = x.reshape(bsz * t, d)
    logits = (xf @ w_router).astype(jnp.float32)
    top_val, top_idx = lax.top_k(logits, TOP_K)
    top_w = jax.nn.softmax(top_val, axis=-1)
    gates = jnp.einsum('nk,nke->ne', top_w, jax.nn.one_hot(top_idx, N_EXPERTS, dtype=jnp.float32))
    out = jnp.zeros_like(xf)
    for e in range(N_EXPERTS):
        h = jax.nn.silu(xf @ w_gate[e]) * (xf @ w_up[e])
        out = out + gates[:, e:e + 1].astype(xf.dtype) * (h @ w_down[e])
    return out.reshape(bsz, t, d)


def setup_inputs(seed: int = 0) -> dict:
    key = jax.random.key(seed)
    ks = jax.random.split(key, 24)

    def nrm(k, shape, scale):
        return jax.random.normal(k, shape, jnp.float32) * scale

    x = nrm(ks[0], (BATCH, SEQ, D_MODEL), 1.0)
    a_w_in = nrm(ks[1], (N_EVEN, D_MODEL, GDN_IN), D_MODEL ** -0.5)
    a_conv_w = nrm(ks[2], (N_EVEN, GDN_CONV, GDN_QKV), GDN_CONV ** -0.5)
    a_a_log = jnp.log(jax.random.uniform(ks[3], (N_EVEN, GDN_V_HEADS), jnp.float32, 1.0, 16.0))
    dt = jnp.exp(jax.random.uniform(ks[4], (N_EVEN, GDN_V_HEADS), jnp.float32,
                                    math.log(1e-3), math.log(1e-1)))
    a_dt_bias = dt + jnp.log(-jnp.expm1(-dt))
    a_norm_w = 1.0 + nrm(ks[5], (N_EVEN, GDN_HEAD_V), 0.02)
    a_w_out = nrm(ks[6], (N_EVEN, GDN_VDIM, D_MODEL), GDN_VDIM ** -0.5 * BETA_INIT)
    b_w_in = nrm(ks[7], (N_ODD, D_MODEL, SWA_IN), D_MODEL ** -0.5)
    b_b_in = nrm(ks[8], (N_ODD, SWA_IN), 0.02)
    b_sinks = nrm(ks[9], (N_ODD, SWA_Q_HEADS), 1.0)
    b_w_out = nrm(ks[10], (N_ODD, SWA_QDIM, D_MODEL), SWA_QDIM ** -0.5 * BETA_INIT)
    rel_bias = nrm(ks[11], (REL_BUCKETS, SWA_Q_HEADS), 0.3)
    ffn_w_gate = nrm(ks[12], (N_EVEN, D_MODEL, FFN_DIM), D_MODEL ** -0.5)
    ffn_w_up = nrm(ks[13], (N_EVEN, D_MODEL, FFN_DIM), D_MODEL ** -0.5)
    ffn_w_down = nrm(ks[14], (N_EVEN, FFN_DIM, D_MODEL), FFN_DIM ** -0.5 * BETA_INIT)
    moe_router = nrm(ks[15], (N_ODD, D_MODEL, N_EXPERTS), D_MODEL ** -0.5)
    moe_w_gate = nrm(ks[16], (N_ODD, N_EXPERTS, D_MODEL, EXPERT_DIM), D_MODEL ** -0.5)
    moe_w_up = nrm(ks[17], (N_ODD, N_EXPERTS, D_MODEL, EXPERT_DIM), D_MODEL ** -0.5)
    moe_w_down = nrm(ks[18], (N_ODD, N_EXPERTS, EXPERT_DIM, D_MODEL), EXPERT_DIM ** -0.5 * BETA_INIT)
    ln_g = 1.0 + nrm(ks[19], (DEPTH, 2, D_MODEL), 0.02)
    ln_b = nrm(ks[20], (DEPTH, 2, D_MODEL), 0.02)
    return {'x': x, 'a_w_in': a_w_in, 'a_conv_w': a_conv_w, 'a_a_log': a_a_log, 'a_dt_bias': a_dt_bias,
            'a_norm_w': a_norm_w, 'a_w_out': a_w_out, 'b_w_in': b_w_in, 'b_b_in': b_b_in,
            'b_sinks': b_sinks, 'b_w_out': b_w_out, 'rel_bias': rel_bias, 'ffn_w_gate': ffn_w_gate,
            'ffn_w_up': ffn_w_up, 'ffn_w_down': ffn_w_down, 'moe_router': moe_router,
            'moe_w_gate': moe_w_gate, 'moe_w_up': moe_w_up, 'moe_w_down': moe_w_down,
            'ln_g': ln_g, 'ln_b': ln_b}


def reference(x, a_w_in, a_conv_w, a_a_log, a_dt_bias, a_norm_w, a_w_out, b_w_in, b_b_in, b_sinks,
              b_w_out, rel_bias, ffn_w_gate, ffn_w_up, ffn_w_down, moe_router, moe_w_gate, moe_w_up,
              moe_w_down, ln_g, ln_b):
    for i in range(DEPTH):
        j = i // N_MIXERS
        if i % N_MIXERS == 0:
            h = gated_deltanet(x, a_w_in[j], a_conv_w[j], a_a_log[j], a_dt_bias[j], a_norm_w[j], a_w_out[j])
        else:
            h = swa_sink_attention(x, b_w_in[j], b_b_in[j], b_sinks[j], rel_bias, b_w_out[j])
        x = layer_norm(ALPHA * x + h, ln_g[i, 0], ln_b[i, 0])
        if i % 2 == 0:
            f = swiglu(x, ffn_w_gate[j], ffn_w_up[j], ffn_w_down[j])
        else:
            f = moe_swiglu(x, moe_router[j], moe_w_gate[j], moe_w_up[j], moe_w_down[j])
        x = layer_norm(ALPHA * x + f, ln_g[i, 1], ln_b[i, 1])
    return x
```

```python
import math
import numpy as np
from contextlib import ExitStack
import concourse.bass as bass
import concourse.mybir as mybir
from concourse.bass_utils import run_bass_kernel_spmd

F32 = mybir.dt.float32
BF16 = mybir.dt.bfloat16
AF = mybir.ActivationFunctionType
ALU = mybir.AluOpType
AX = mybir.AxisListType


class Res:
    __slots__ = ("name", "lw", "rd", "psum")

    def __init__(self, name, psum=False):
        self.name = name
        self.psum = psum
        self.lw = None
        self.rd = {}


class FW:
    def __init__(self, nc, es):
        self.nc = nc
        self.es = es
        self.engs = {"pe": nc.tensor, "dve": nc.vector, "act": nc.scalar, "pool": nc.gpsimd, "sp": nc.sync}
        self.sem = {}
        self.cnt = {}
        for k in self.engs:
            self.sem[k] = es.enter_context(nc.semaphore("sem_" + k))
            self.cnt[k] = 0
        self.known = {k: {} for k in self.engs}
        self.dsem = {}
        self.dcnt = {}
        self.nwaits = 0
        self.ninstr = 0

    def sb(self, name, shape, dt):
        return self.es.enter_context(self.nc.sbuf_tensor(name, list(shape), dt))

    def ps(self, name, shape, dt=F32):
        return self.es.enter_context(self.nc.psum_tensor(name, list(shape), dt))

    def dma_sem(self, name):
        if name not in self.dsem:
            self.dsem[name] = self.es.enter_context(self.nc.semaphore("dq_" + name))
            self.dcnt[name] = 0
        return name

    def _semobj(self, key):
        return self.sem[key] if key in self.sem else self.dsem[key]

    def _deps(self, eng, reads, writes):
        deps = {}
        def add(d):
            if d is None:
                return
            k, v = d
            if deps.get(k, 0) < v:
                deps[k] = v
        for r in reads:
            add(r.lw)
            if r.psum:
                for k, v in r.rd.items():
                    if k != eng:
                        add((k, v))
        for w in writes:
            add(w.lw)
            for k, v in w.rd.items():
                add((k, v))
        for k, v in deps.items():
            if k == eng and eng == "pe":
                continue
            if k in self.dsem:
                v = self.dcnt[k]
            if self.known[eng].get(k, 0) >= v:
                continue
            self.engs[eng].wait_ge(self._semobj(k), v)
            self.known[eng][k] = v
            self.nwaits += 1

    def _commit(self, key, val, reads, writes):
        for w in writes:
            w.lw = (key, val)
            w.rd = {}
        for r in reads:
            if r in writes:
                continue
            if r.rd.get(key, 0) < val:
                r.rd[key] = val

    def op(self, eng, fn, reads=(), writes=()):
        self._deps(eng, reads, writes)
        ins = fn()
        if isinstance(ins, (list, tuple)):
            ins = ins[-1]
        ins.then_inc(self.sem[eng], 1)
        self.cnt[eng] += 1
        self.ninstr += 1
        self._commit(eng, self.cnt[eng], reads, writes)
        return ins

    def dma(self, q, out, in_, slot, reads=(), writes=(), **kw):
        self.dma_sem(slot)
        self._deps(q, reads, writes)
        ins = self.engs[q].dma_start(out=out, in_=in_, **kw)
        ins.then_inc(self.dsem[slot], 16)
        self.dcnt[slot] += 16
        self._commit(slot, self.dcnt[slot], reads, writes)
        return ins

    def wait_all(self, eng, ress):
        self._deps(eng, ress, [])


NEG = -30000.0


class GTL:
    def __init__(self, t, name):
        self.t = t
        self.r = Res(name)


def build_gdn(nc, T, stop_at=99):
    NTILE = T // 512
    xT = nc.dram_tensor("xT", [1024, T], F32, kind="ExternalInput").ap()
    wqkv = nc.dram_tensor("wqkv", [1024, 512], F32, kind="ExternalInput").ap()
    wzba = nc.dram_tensor("wzba", [1024, 260], F32, kind="ExternalInput").ap()
    convw = nc.dram_tensor("convw", [512, 4], F32, kind="ExternalInput").ap()
    hp = nc.dram_tensor("hp", [1, 4], F32, kind="ExternalInput").ap()
    normw = nc.dram_tensor("normw", [1, 128], F32, kind="ExternalInput").ap()
    og = nc.dram_tensor("og", [T, 256], F32, kind="ExternalOutput").ap()

    with ExitStack() as es:
        fw = FW(nc, es)
        V, A, G, PE = nc.vector, nc.scalar, nc.gpsimd, nc.tensor

        def sb(name, shape, dt=F32):
            return GTL(fw.sb(name, shape, dt), name)

        identb = sb("identb", [128, 128], BF16)
        identf = sb("identf", [128, 128])
        U = sb("U", [128, 128])
        Uneg = sb("Uneg", [128, 128])
        onesf = sb("onesf", [128, 128])
        onesb = sb("onesb", [128, 128], BF16)
        MASKS = sb("MASKS", [128, 128])
        BDm = sb("BDm", [128, 128])
        OFFm = sb("OFFm", [128, 128])

        def pool(fn, reads=(), writes=()):
            return fw.op("pool", fn, [x.r for x in reads], [x.r for x in writes])

        def dve(fn, reads=(), writes=()):
            return fw.op("dve", fn, [x.r for x in reads], [x.r for x in writes])

        def act(fn, reads=(), writes=()):
            return fw.op("act", fn, [x.r for x in reads], [x.r for x in writes])

        def pe(fn, reads=(), writes=()):
            return fw.op("pe", fn, [x.r for x in reads], [x.r for x in writes])

        def mm(out_ap, pairs, reads, writes):
            def f():
                n = len(pairs)
                last = None
                for i, (l, r) in enumerate(pairs):
                    last = PE.matmul(out_ap, lhsT=l, rhs=r, start=(i == 0), stop=(i == n - 1))
                return last
            return pe(f, reads, writes)

        for t_, val in ((identb, 0.0), (identf, 0.0), (U, 1.0), (Uneg, -1.0), (onesf, 1.0), (onesb, 1.0),
                        (MASKS, 0.0), (BDm, 0.0), (OFFm, 0.0)):
            pool(lambda t_=t_, val=val: G.memset(t_.t[:], val), writes=[t_])
        for t_ in (identb, identf):
            pool(lambda t_=t_: G.affine_select(out=t_.t[:], in_=t_.t[:], pattern=[[-1, 128]], compare_op=ALU.not_equal,
                                               fill=1.0, base=0, channel_multiplier=1), reads=[t_], writes=[t_])
        for t_ in (U, Uneg):
            pool(lambda t_=t_: G.affine_select(out=t_.t[:], in_=t_.t[:], pattern=[[1, 128]], compare_op=ALU.is_ge,
                                               fill=0.0, base=0, channel_multiplier=-1), reads=[t_], writes=[t_])
        pool(lambda: G.affine_select(out=MASKS.t[:], in_=MASKS.t[:], pattern=[[-1, 128]], compare_op=ALU.is_gt,
                                     fill=NEG, base=0, channel_multiplier=1), reads=[MASKS], writes=[MASKS])
        pool(lambda: G.memset(BDm.t[0:64, 0:64], 1.0), writes=[BDm])
        pool(lambda: G.memset(BDm.t[64:128, 64:128], 1.0), writes=[BDm])
        pool(lambda: G.memset(OFFm.t[64:128, 0:64], 1.0), writes=[OFFm])

        wqkv_b = sb("wqkv_b", [128, 8, 512], BF16)
        wzba_b = sb("wzba_b", [128, 8, 320], BF16)
        convw_s = sb("convw_s", [128, 4, 4])
        hp_s = sb("hp_s", [128, 4])
        normw_s = sb("normw_s", [128, 128])
        fw.dma("pool", wqkv_b.t[:], wqkv.rearrange("(c p) n -> p c n", p=128), "w0", writes=[wqkv_b.r])
        fw.dma("pool", wzba_b.t[:, :, 0:260], wzba.rearrange("(c p) n -> p c n", p=128), "w1", writes=[wzba_b.r])
        fw.dma("sp", convw_s.t[:], convw.rearrange("(c p) j -> p c j", p=128), "w2", writes=[convw_s.r])
        fw.dma("sp", hp_s.t[:], hp[0:1, :].partition_broadcast(128), "w2", writes=[hp_s.r])
        fw.dma("sp", normw_s.t[:], normw[0:1, :].partition_broadcast(128), "w2", writes=[normw_s.r])
        negA8 = sb("negA8", [128, 4, 2])
        dtb8 = sb("dtb8", [128, 4, 2])
        ea = sb("ea", [128, 2])
        act(lambda: A.activation(out=ea.t[:], in_=hp_s.t[:, 0:2], func=AF.Exp), reads=[hp_s], writes=[ea])
        for c in range(4):
            dve(lambda c=c: V.tensor_scalar(out=negA8.t[:, c, :], in0=ea.t[:], scalar1=-1.0, scalar2=None, op0=ALU.mult),
                reads=[ea], writes=[negA8])
            dve(lambda c=c: V.tensor_copy(out=dtb8.t[:, c, :], in_=hp_s.t[:, 2:4]), reads=[hp_s], writes=[dtb8])

        S = [sb(f"S{h}", [128, 128]) for h in range(2)]
        Sb = [sb(f"Sb{h}", [128, 128], BF16) for h in range(2)]
        for h in range(2):
            pool(lambda h=h: G.memset(S[h].t[:], 0.0), writes=[S[h]])
            pool(lambda h=h: G.memset(Sb[h].t[:], 0.0), writes=[Sb[h]])

        xb = [sb(f"xb{i}", [128, 8, 512], BF16) for i in range(2)]
        pre = [[sb(f"pre{i}_{ch}", [128, 515]) for ch in range(4)] for i in range(2)]
        for ch in range(4):
            pool(lambda ch=ch: G.memset(pre[0][ch].t[:, 0:3], 0.0), writes=[pre[0][ch]])
        cacc = [sb(f"cacc{ch}", [128, 512]) for ch in range(4)]
        qs = [sb(f"qs{i}", [128, 512]) for i in range(2)]
        sq = [sb(f"sq{i}", [128, 512], BF16) for i in range(2)]
        lnr = [sb(f"lnr{i}", [128, 512]) for i in range(2)]
        rr = [sb(f"rr{i}", [128, 512]) for i in range(2)]
        qnT = sb("qnT", [128, 512], BF16)
        knT = sb("knT", [128, 512], BF16)
        vT = [sb(f"vT{h}", [128, 512], BF16) for h in range(2)]
        zs = sb("zs", [128, 4, 256])
        nwz = sb("nwz", [128, 4, 2, 128])
        ba = sb("ba", [128, 4, 4])
        eb = sb("eb", [128, 4, 2])
        beta = sb("beta", [128, 4, 2])
        nbeta = sb("nbeta", [128, 4, 2])
        t1 = sb("t1", [128, 4, 2])
        e1 = sb("e1", [128, 4, 2])
        l1 = sb("l1", [128, 4, 2])
        gt = sb("gt", [128, 4, 2])
        gs = sb("gs", [128, 4])
        exa = sb("exa", [128, 4])
        dk = sb("dk", [128, 2])
        ekd = sb("ekd", [128, 2])
        bg = sb("bg", [128, 2])
        vb = [sb(f"vb{h}", [128, 128], BF16) for h in range(2)]
        kbg = [sb(f"kbg{h}", [128, 128], BF16) for h in range(2)]
        kd = [sb(f"kd{h}", [128, 128], BF16) for h in range(2)]
        gB = [sb(f"gB{h}", [128, 128]) for h in range(2)]
        dec = sb("dec", [128, 512])
        NA = [sb(f"NA{h}", [128, 128]) for h in range(2)]
        MoffT = [sb(f"MoffT{h}", [128, 128]) for h in range(2)]
        decc = [sb(f"decc{h}", [128, 128]) for h in range(2)]
        aqk = [sb(f"aqk{h}", [128, 128], BF16) for h in range(2)]
        aqkT = [sb(f"aqkT{h}", [128, 128], BF16) for h in range(2)]
        qdT = [sb(f"qdT{h}", [128, 128], BF16) for h in range(2)]
        PP = [[sb(f"PP{h}_{i}", [128, 384]) for i in range(2)] for h in range(2)]
        Tt = [[sb(f"Tt{h}_{i}", [128, 128]) for i in range(2)] for h in range(2)]
        TdY = [sb(f"TdY{h}", [128, 256]) for h in range(2)]
        TTb = [sb(f"TTb{h}", [128, 128], BF16) for h in range(2)]
        u_s = sb("u_s", [128, 256])
        wT_s = sb("wT_s", [128, 256], BF16)
        vn = [sb(f"vn{h}", [128, 128], BF16) for h in range(2)]
        junk = [sb(f"junk{h}", [128, 128]) for h in range(2)]
        ss = [sb(f"ss{h}", [128, 1]) for h in range(2)]
        lns = [sb(f"lns{h}", [128, 1]) for h in range(2)]
        rinv = [sb(f"rinv{h}", [128, 1]) for h in range(2)]
        ogt = [sb(f"ogt{i}", [128, 256]) for i in range(2)]

        def pst(name, shape, dt=F32):
            return GTL(fw.ps(name, shape, dt), name)
        b0 = fw.ps("b0", [128, 512]); b1 = fw.ps("b1", [128, 512]); b2 = fw.ps("b2", [128, 512])
        b3 = fw.ps("b3", [128, 1024], BF16)
        b4 = fw.ps("b4", [128, 512]); b5 = fw.ps("b5", [128, 512]); b6 = fw.ps("b6", [128, 512]); b7 = fw.ps("b7", [128, 512])

        RB = [Res(f"bank{i}", psum=True) for i in range(8)]
        banks = [b0, b1, b2, b3, b4, b5, b6, b7]

        class PT:
            def __init__(self, bi, lo, hi, name):
                self.bank = banks[bi]; self.lo = lo; self.hi = hi; self.r = RB[bi]
            @property
            def ap(self):
                return self.bank[:, self.lo:self.hi]
        paA = [PT(0, 0, 512, "paA0"), PT(1, 0, 512, "paA1")]
        pzt = [PT(6, 0, 260, "pzt0"), PT(7, 0, 260, "pzt1")]
        pD = PT(2, 0, 512, "pD")
        pT_k = PT(3, 0, 128, "pT_k")
        pT_v = [PT(3, 128, 256, "pT_v0"), PT(3, 256, 384, "pT_v1")]
        pT_a = [PT(3, 384, 512, "pT_a0"), PT(3, 512, 640, "pT_a1")]
        pKK = PT(6, 0, 128, "pKK"); pQK = PT(6, 128, 256, "pQK"); pgcc = PT(6, 256, 260, "pgcc")
        pi_sq = [PT(4, 0, 256, "pisq0"), PT(5, 0, 256, "pisq1")]
        pi_pr = [PT(4, 256, 384, "pipr0"), PT(5, 256, 384, "pipr1")]
        pu = PT(2, 0, 256, "pu"); pwT = PT(2, 256, 512, "pwT")
        p_wS = [PT(0, 0, 128, "pwS0"), PT(1, 0, 128, "pwS1")]
        p_Sn = [PT(0, 128, 256, "pSn0"), PT(1, 128, 256, "pSn1")]
        p_o = [PT(7, 0, 128, "po0"), PT(7, 128, 256, "po1")]

        xTr = xT.rearrange("(c p) t -> p c t", p=128)

        def load_x(n):
            s = n % 2
            fw.dma("pool", xb[s].t[:], xTr[:, :, n * 512:(n + 1) * 512], f"x{s}", writes=[xb[s].r])

        def body():
            load_x(0)
            for n in range(NTILE):
                s = n % 2
                if n + 1 < NTILE:
                    load_x(n + 1)
                X = xb[s]
                if stop_at == 0: return
                for ch in range(4):
                    pa = paA[ch % 2]
                    mm(pa.ap, [(wqkv_b.t[:, kc, ch * 128:(ch + 1) * 128], X.t[:, kc, :]) for kc in range(8)],
                       [wqkv_b, X], [pa])
                    P_ = pre[s][ch]
                    act(lambda pa=pa, P_=P_: A.copy(out=P_.t[:, 3:515], in_=pa.ap), reads=[pa], writes=[P_])
                    if n + 1 < NTILE:
                        Pn = pre[1 - s][ch]
                        pool(lambda P_=P_, Pn=Pn: G.tensor_copy(out=Pn.t[:, 0:3], in_=P_.t[:, 512:515]), reads=[P_], writes=[Pn])
                if stop_at == 1: return
                for j in (3, 2, 1, 0):
                    for ch in range(4):
                        P_ = pre[s][ch]; C_ = cacc[ch]
                        if j == 3:
                            dve(lambda P_=P_, C_=C_, ch=ch, j=j: V.tensor_scalar(out=C_.t[:], in0=P_.t[:, j:j + 512],
                                scalar1=convw_s.t[:, ch, j:j + 1], scalar2=None, op0=ALU.mult), reads=[P_, convw_s], writes=[C_])
                        else:
                            dve(lambda P_=P_, C_=C_, ch=ch, j=j: V.scalar_tensor_tensor(out=C_.t[:], in0=P_.t[:, j:j + 512],
                                scalar=convw_s.t[:, ch, j:j + 1], in1=C_.t[:], op0=ALU.mult, op1=ALU.add),
                                reads=[P_, convw_s, C_], writes=[C_])
                for i in range(2):
                    act(lambda i=i: A.activation(out=qs[i].t[:], in_=cacc[i].t[:], func=AF.Silu), reads=[cacc[i]], writes=[qs[i]])
                for h in range(2):
                    act(lambda h=h: A.activation(out=vT[h].t[:], in_=cacc[2 + h].t[:], func=AF.Silu), reads=[cacc[2 + h]], writes=[vT[h]])
                if stop_at == 2: return
                for c in range(4):
                    pz = pzt[c % 2]
                    mm(pz.ap, [(X.t[:, kc, c * 128:(c + 1) * 128], wzba_b.t[:, kc, 0:260]) for kc in range(8)], [wzba_b, X], [pz])
                    act(lambda pz=pz, c=c: A.activation(out=zs.t[:, c, :], in_=pz.bank[:, 0:256], func=AF.Silu), reads=[pz], writes=[zs])
                    act(lambda pz=pz, c=c: A.copy(out=ba.t[:, c, :], in_=pz.bank[:, 256:260]), reads=[pz], writes=[ba])
                if stop_at == 25: return
                for c in range(4):
                    for h in range(2):
                        pool(lambda c=c, h=h: G.tensor_tensor(out=nwz.t[:, c, h, :], in0=zs.t[:, c, h * 128:(h + 1) * 128],
                                                              in1=normw_s.t[:], op=ALU.mult), reads=[zs, normw_s], writes=[nwz])
                if stop_at == 3: return
                for i in range(2):
                    act(lambda i=i: A.activation(out=sq[i].t[:], in_=qs[i].t[:], func=AF.Square), reads=[qs[i]], writes=[sq[i]])
                for i in range(2):
                    pa = paA[i]
                    mm(pa.ap, [(onesb.t[:], sq[i].t[:])], [onesb, sq[i]], [pa])
                    act(lambda i=i, pa=pa: A.activation(out=lnr[i].t[:], in_=pa.ap, func=AF.Ln, bias=1e-6), reads=[pa], writes=[lnr[i]])
                for i in range(2):
                    act(lambda i=i: A.activation(out=rr[i].t[:], in_=lnr[i].t[:], func=AF.Exp, scale=-0.5), reads=[lnr[i]], writes=[rr[i]])
                dve(lambda: V.scalar_tensor_tensor(out=qnT.t[:], in0=qs[0].t[:], scalar=float(128 ** -0.5), in1=rr[0].t[:],
                                                   op0=ALU.mult, op1=ALU.mult), reads=[qs[0], rr[0]], writes=[qnT])
                dve(lambda: V.tensor_tensor(out=knT.t[:], in0=qs[1].t[:], in1=rr[1].t[:], op=ALU.mult), reads=[qs[1], rr[1]], writes=[knT])
                if stop_at == 4: return
                act(lambda: A.activation(out=eb.t[:], in_=ba.t[:, :, 0:2], func=AF.Exp, scale=-1.0), reads=[ba], writes=[eb])
                dve(lambda: V.tensor_scalar(out=eb.t[:], in0=eb.t[:], scalar1=1.0, scalar2=None, op0=ALU.add), reads=[eb], writes=[eb])
                dve(lambda: V.reciprocal(out=beta.t[:], in_=eb.t[:]), reads=[eb], writes=[beta])
                dve(lambda: V.tensor_scalar(out=nbeta.t[:], in0=beta.t[:], scalar1=-1.0, scalar2=None, op0=ALU.mult), reads=[beta], writes=[nbeta])
                dve(lambda: V.tensor_tensor(out=t1.t[:], in0=ba.t[:, :, 2:4], in1=dtb8.t[:], op=ALU.add), reads=[ba, dtb8], writes=[t1])
                act(lambda: A.activation(out=e1.t[:], in_=t1.t[:], func=AF.Exp), reads=[t1], writes=[e1])
                act(lambda: A.activation(out=l1.t[:], in_=e1.t[:], func=AF.Ln, bias=1.0), reads=[e1], writes=[l1])
                dve(lambda: V.tensor_tensor(out=gt.t[:], in0=l1.t[:], in1=negA8.t[:], op=ALU.mult), reads=[l1, negA8], writes=[gt])

                if stop_at == 5: return
                for c in range(4):
                    cs = slice(c * 128, (c + 1) * 128)
                    pe(lambda: PE.transpose(pT_k.ap, knT.t[:, cs], identb.t[:]), reads=[knT, identb], writes=[pT_k])
                    for h in range(2):
                        pe(lambda h=h: PE.transpose(pT_v[h].ap, vT[h].t[:, cs], identb.t[:]), reads=[vT[h], identb], writes=[pT_v[h]])
                    mm(b6[:, 256:258], [(U.t[:], gt.t[:, c, :])], [U, gt], [pgcc])
                    mm(b6[:, 258:260], [(onesf.t[:], gt.t[:, c, :])], [onesf, gt], [pgcc])
                    mm(pKK.ap, [(knT.t[:, cs], knT.t[:, cs])], [knT], [pKK])
                    mm(pQK.ap, [(qnT.t[:, cs], knT.t[:, cs])], [qnT, knT], [pQK])
                    act(lambda: A.copy(out=gs.t[:], in_=pgcc.ap), reads=[pgcc], writes=[gs])
                    act(lambda: A.activation(out=exa.t[:], in_=gs.t[:], func=AF.Exp), reads=[gs], writes=[exa])
                    for h in range(2):
                        act(lambda h=h: A.activation(out=ekd.t[:, h:h + 1], in_=gs.t[:, h:h + 1], func=AF.Exp, scale=-1.0,
                                                     bias=gs.t[:, 2 + h:3 + h]), reads=[gs], writes=[ekd])
                    for h in range(2):
                        dve(lambda h=h: V.tensor_scalar(out=vb[h].t[:], in0=pT_v[h].ap, scalar1=beta.t[:, c, h:h + 1], scalar2=None,
                                                        op0=ALU.mult), reads=[pT_v[h], beta], writes=[vb[h]])
                        dve(lambda h=h: V.tensor_scalar(out=kbg[h].t[:], in0=pT_k.ap, scalar1=beta.t[:, c, h:h + 1], scalar2=exa.t[:, h:h + 1],
                                                        op0=ALU.mult, op1=ALU.mult), reads=[pT_k, beta, exa], writes=[kbg[h]])
                        dve(lambda h=h: V.tensor_scalar(out=kd[h].t[:], in0=pT_k.ap, scalar1=ekd.t[:, h:h + 1], scalar2=None, op0=ALU.mult),
                            reads=[pT_k, ekd], writes=[kd[h]])
                        pool(lambda h=h: G.tensor_scalar(out=gB[h].t[:], in0=onesf.t[:], scalar1=gt.t[:, c, h:h + 1], scalar2=None,
                                                         op0=ALU.mult), reads=[onesf, gt], writes=[gB[h]])
                    if stop_at == 6: return
                    for h in range(2):
                        mm(b2[:, h * 128:(h + 1) * 128], [(U.t[:], gB[h].t[:]), (gB[h].t[:], Uneg.t[:]), (identf.t[:], MASKS.t[:])],
                           [U, Uneg, gB[h], identf, MASKS], [pD])
                        mm(b2[:, 256 + h * 128:256 + (h + 1) * 128], [(gB[h].t[:], U.t[:])], [gB[h], U], [pD])
                    act(lambda: A.activation(out=dec.t[:], in_=pD.ap, func=AF.Exp), reads=[pD], writes=[dec])
                    for h in range(2):
                        hs = slice(h * 128, (h + 1) * 128)
                        dve(lambda h=h, hs=hs: V.scalar_tensor_tensor(out=NA[h].t[:], in0=pKK.ap, scalar=nbeta.t[:, c, h:h + 1],
                            in1=dec.t[:, hs], op0=ALU.mult, op1=ALU.mult), reads=[pKK, nbeta, dec], writes=[NA[h]])
                        pool(lambda h=h: G.tensor_tensor(out=PP[h][0].t[:, 128:256], in0=NA[h].t[:], in1=BDm.t[:], op=ALU.mult),
                             reads=[NA[h], BDm], writes=[PP[h][0]])
                        pool(lambda h=h: G.tensor_tensor(out=MoffT[h].t[:], in0=NA[h].t[:], in1=OFFm.t[:], op=ALU.mult),
                             reads=[NA[h], OFFm], writes=[MoffT[h]])
                        pool(lambda h=h, hs=hs: G.tensor_tensor(out=decc[h].t[:], in0=dec.t[:, hs], in1=identf.t[:], op=ALU.add),
                             reads=[dec, identf], writes=[decc[h]])
                        dve(lambda h=h: V.tensor_tensor(out=aqk[h].t[:], in0=pQK.ap, in1=decc[h].t[:], op=ALU.mult),
                            reads=[pQK, decc[h]], writes=[aqk[h]])
                        pool(lambda h=h: G.tensor_tensor(out=qdT[h].t[:], in0=qnT.t[:, cs], in1=dec.t[:, 256 + h * 128:256 + (h + 1) * 128],
                                                         op=ALU.mult), reads=[qnT, dec], writes=[qdT[h]])
                    for h in range(2):
                        pe(lambda h=h: PE.transpose(pT_a[h].ap, aqk[h].t[:], identb.t[:]), reads=[aqk[h], identb], writes=[pT_a[h]])
                    for h in range(2):
                        act(lambda h=h: A.copy(out=aqkT[h].t[:], in_=pT_a[h].ap), reads=[pT_a[h]], writes=[aqkT[h]])
                    if stop_at == 7: return
                    def ev(h, dst_tile, dst_ap, src_pt, src_ap):
                        if h == 0:
                            act(lambda: A.copy(out=dst_ap, in_=src_ap), reads=[src_pt], writes=[dst_tile])
                        else:
                            dve(lambda: V.tensor_copy(out=dst_ap, in_=src_ap), reads=[src_pt], writes=[dst_tile])
                    for h in range(2):
                        X0 = PP[h][0]
                        bk = pi_sq[h].bank
                        pe(lambda: PE.matmul(bk[:, 0:128], lhsT=X0.t[:, 128:256], rhs=identf.t[:], start=True, stop=True),
                           reads=[X0, identf], writes=[pi_sq[h]])
                        mm(bk[:, 256:384], [(X0.t[:, 128:256], identf.t[:]), (identf.t[:], identf.t[:])], [X0, identf], [pi_sq[h]])
                    for h in range(2):
                        X0 = PP[h][0]
                        bk = pi_sq[h].bank
                        ev(h, X0, X0.t[:, 0:128], pi_sq[h], bk[:, 0:128])
                        ev(h, X0, X0.t[:, 256:384], pi_sq[h], bk[:, 256:384])
                    if stop_at == 75: return
                    for k in range(1, 7):
                        for h in range(2):
                            Ps = PP[h][(k - 1) % 2]
                            bk = pi_sq[h].bank
                            if k <= 4:
                                pe(lambda: PE.matmul(bk[:, 0:128], lhsT=Ps.t[:, 128:256], rhs=Ps.t[:, 0:128], start=True, stop=True),
                                   reads=[Ps], writes=[pi_sq[h]])
                            if k <= 5:
                                pe(lambda: PE.matmul(bk[:, 128:256], lhsT=Ps.t[:, 0:128], rhs=Ps.t[:, 128:256], start=True, stop=True),
                                   reads=[Ps], writes=[pi_sq[h]])
                            if k >= 2:
                                mm(bk[:, 256:384], [(Ps.t[:, 128:256], Ps.t[:, 256:384]), (identf.t[:], Ps.t[:, 256:384])], [Ps, identf], [pi_sq[h]])
                            else:
                                mm(bk[:, 256:384], [(identf.t[:], Ps.t[:, 256:384])], [Ps, identf], [pi_sq[h]])
                        for h in range(2):
                            Pd = PP[h][k % 2]
                            bk = pi_sq[h].bank
                            lo = 0 if k <= 4 else (128 if k == 5 else 256)
                            ev(h, Pd, Pd.t[:, lo:384], pi_sq[h], bk[:, lo:384])
                    if stop_at == 8: return
                    for h in range(2):
                        Pf = PP[h][0]
                        bk = pi_sq[h].bank
                        pe(lambda: PE.matmul(bk[:, 0:128], lhsT=Pf.t[:, 256:384], rhs=identf.t[:], start=True, stop=True),
                           reads=[Pf, identf], writes=[pi_sq[h]])
                        pe(lambda: PE.matmul(bk[:, 128:256], lhsT=MoffT[h].t[:], rhs=Pf.t[:, 256:384], start=True, stop=True),
                           reads=[MoffT[h], Pf], writes=[pi_sq[h]])
                    for h in range(2):
                        ev(h, TdY[h], TdY[h].t[:], pi_sq[h], pi_sq[h].bank[:, 0:256])
                    for h in range(2):
                        Pf = PP[h][0]
                        bk = pi_sq[h].bank
                        mm(bk[:, 256:384], [(TdY[h].t[:, 0:128], TdY[h].t[:, 128:256]), (identf.t[:], Pf.t[:, 256:384])],
                           [TdY[h], Pf, identf], [pi_sq[h]])
                    for h in range(2):
                        ev(h, TTb[h], TTb[h].t[:], pi_sq[h], pi_sq[h].bank[:, 256:384])
                    if stop_at == 9: return
                    for h in range(2):
                        hs = slice(h * 128, (h + 1) * 128)
                        pe(lambda h=h, hs=hs: PE.matmul(b2[:, hs], lhsT=TTb[h].t[:], rhs=vb[h].t[:], start=True, stop=True),
                           reads=[TTb[h], vb[h]], writes=[pu])
                    for h in range(2):
                        pe(lambda h=h: PE.matmul(b2[:, 256 + h * 128:256 + (h + 1) * 128], lhsT=kbg[h].t[:], rhs=TTb[h].t[:], start=True, stop=True),
                           reads=[TTb[h], kbg[h]], writes=[pwT])
                    act(lambda: A.copy(out=u_s.t[:], in_=pu.ap), reads=[pu], writes=[u_s])
                    act(lambda: A.copy(out=wT_s.t[:], in_=pwT.ap), reads=[pwT], writes=[wT_s])
                    if stop_at == 10: return
                    og_t = ogt[c % 2]
                    for h in range(2):
                        hs = slice(h * 128, (h + 1) * 128)
                        pe(lambda h=h, hs=hs: PE.matmul(p_wS[h].ap, lhsT=wT_s.t[:, hs], rhs=Sb[h].t[:], start=True, stop=True),
                           reads=[wT_s, Sb[h]], writes=[p_wS[h]])
                        dve(lambda h=h, hs=hs: V.tensor_tensor(out=vn[h].t[:], in0=u_s.t[:, hs], in1=p_wS[h].ap, op=ALU.subtract),
                            reads=[u_s, p_wS[h]], writes=[vn[h]])
                    for h in range(2):
                        mm(p_o[h].ap, [(qdT[h].t[:], Sb[h].t[:]), (aqkT[h].t[:], vn[h].t[:])], [qdT[h], Sb[h], aqkT[h], vn[h]], [p_o[h]])
                        mm(p_Sn[h].ap, [(kd[h].t[:], vn[h].t[:])], [kd[h], vn[h]], [p_Sn[h]])
                    for h in range(2):
                        dve(lambda h=h: V.scalar_tensor_tensor(out=S[h].t[:], in0=S[h].t[:], scalar=exa.t[:, 2 + h:3 + h], in1=p_Sn[h].ap,
                                                               op0=ALU.mult, op1=ALU.add), reads=[S[h], exa, p_Sn[h]], writes=[S[h]])
                        act(lambda h=h: A.copy(out=Sb[h].t[:], in_=S[h].t[:]), reads=[S[h]], writes=[Sb[h]])
                    for h in range(2):
                        act(lambda h=h: A.activation(out=junk[h].t[:], in_=p_o[h].ap, func=AF.Square, accum_out=ss[h].t[:, 0:1]),
                            reads=[p_o[h]], writes=[junk[h], ss[h]])
                    for h in range(2):
                        act(lambda h=h: A.activation(out=lns[h].t[:], in_=ss[h].t[:], func=AF.Ln, scale=1.0 / 128.0, bias=1e-6),
                            reads=[ss[h]], writes=[lns[h]])
                    for h in range(2):
                        act(lambda h=h: A.activation(out=rinv[h].t[:], in_=lns[h].t[:], func=AF.Exp, scale=-0.5), reads=[lns[h]], writes=[rinv[h]])
                    for h in range(2):
                        dve(lambda h=h, og_t=og_t: V.scalar_tensor_tensor(out=og_t.t[:, h * 128:(h + 1) * 128], in0=p_o[h].ap,
                            scalar=rinv[h].t[:, 0:1], in1=nwz.t[:, c, h, :], op0=ALU.mult, op1=ALU.mult),
                            reads=[p_o[h], rinv[h], nwz], writes=[og_t])
                    r0 = n * 512 + c * 128
                    fw.dma("sp", og[r0:r0 + 128, :], og_t.t[:], f"og{c % 2}", reads=[og_t.r])

        body()
        for k in ("og0", "og1"):
            if k in fw.dsem:
                nc.sync.wait_ge(fw.dsem[k], fw.dcnt[k])
        print("GDN instrs", fw.ninstr, "waits", fw.nwaits)
    return nc


ALPHA = float((2.0 * 2) ** 0.25)
LN_EPS = 1e-5


class TL:
    def __init__(self, t, name, psum=False):
        self.t = t
        self.r = Res(name, psum=psum)


class KC:
    def __init__(self, nc, es):
        self.nc = nc
        self.es = es
        self.fw = FW(nc, es)
        self.V, self.A, self.G, self.PE = nc.vector, nc.scalar, nc.gpsimd, nc.tensor
        self.banks = []
        self._uid = 0

    def sb(self, name, shape, dt=F32):
        return TL(self.fw.sb(name, shape, dt), name)

    def bank(self, name, dt=F32):
        cols = 512 if dt == F32 else 1024
        return TL(self.fw.ps(name, [128, cols], dt), name, psum=True)

    def op(self, eng, fn, reads=(), writes=()):
        return self.fw.op(eng, fn, [x.r for x in reads], [x.r for x in writes])

    def dve(self, fn, reads=(), writes=()):
        return self.op("dve", fn, reads, writes)

    def act(self, fn, reads=(), writes=()):
        return self.op("act", fn, reads, writes)

    def pool(self, fn, reads=(), writes=()):
        return self.op("pool", fn, reads, writes)

    def pe(self, fn, reads=(), writes=()):
        return self.op("pe", fn, reads, writes)

    def mm(self, out_ap, pairs, reads, writes):
        PE = self.PE
        def f():
            n = len(pairs)
            last = None
            for i, (l, r) in enumerate(pairs):
                last = PE.matmul(out_ap, lhsT=l, rhs=r, start=(i == 0), stop=(i == n - 1))
            return last
        return self.pe(f, reads, writes)

    def dma(self, q, out, in_, slot, reads=(), writes=(), **kw):
        return self.fw.dma(q, out, in_, slot, [x.r for x in reads], [x.r for x in writes], **kw)

    def barrier(self):
        fw = self.fw
        for e in fw.engs:
            for k in list(fw.sem) + list(fw.dsem):
                v = fw.cnt[k] if k in fw.sem else fw.dcnt[k]
                if k == e or v == 0 or fw.known[e].get(k, 0) >= v:
                    continue
                fw.engs[e].wait_ge(fw._semobj(k), v)
                fw.known[e][k] = v

    def finish(self, slots):
        for k in slots:
            if k in self.fw.dsem:
                self.nc.sync.wait_ge(self.fw.dsem[k], self.fw.dcnt[k])

    def consts(self):
        G = self.G
        self.onesf = self.sb("onesf", [128, 128])
        self.identf = self.sb("identf", [128, 128])
        self.identb = self.sb("identb", [128, 128], BF16)
        for t_, v in ((self.onesf, 1.0), (self.identf, 0.0), (self.identb, 0.0)):
            self.pool(lambda: G.memset(t_.t[:], v), writes=[t_])
        for t_ in (self.identf, self.identb):
            self.pool(lambda: G.affine_select(out=t_.t[:], in_=t_.t[:], pattern=[[-1, 128]], compare_op=ALU.not_equal,
                                              fill=1.0, base=0, channel_multiplier=1), reads=[t_], writes=[t_])

    def ln_alloc(self):
        self.ln_sq = [self.sb(f"ln_sq{i}", [128, 512]) for i in range(2)]
        self.ln_mean = self.sb("ln_mean", [128, 512])
        self.ln_msq = self.sb("ln_msq", [128, 512])
        self.ln_var = self.sb("ln_var", [128, 512])
        self.ln_lnv = self.sb("ln_lnv", [128, 512])
        self.ln_rstd = self.sb("ln_rstd", [128, 512])
        self.ln_t = [self.sb(f"ln_t{i}", [128, 512]) for i in range(2)]

    def layernorm(self, y, ycols, gb, gi, bi, ps1, ps2, out_f=None, out_f_cols=None, out_b=None, out_b_cols=None, N=512):
        V, A, G, PE = self.V, self.A, self.G, self.PE
        onesf = self.onesf
        self.mm(ps1.t[:, 0:N], [(onesf.t[:], y.t[:, c, ycols]) for c in range(8)], [onesf, y], [ps1])
        for c in range(8):
            sq = self.ln_sq[c % 2]
            self.act(lambda: A.activation(out=sq.t[:, 0:N], in_=y.t[:, c, ycols], func=AF.Square), reads=[y], writes=[sq])
            self.pe(lambda: PE.matmul(ps2.t[:, 0:N], lhsT=onesf.t[:], rhs=sq.t[:, 0:N], start=(c == 0), stop=(c == 7)),
                    reads=[onesf, sq], writes=[ps2])
        mean, msq, var, lnv, rstd = self.ln_mean, self.ln_msq, self.ln_var, self.ln_lnv, self.ln_rstd
        self.act(lambda: A.activation(out=mean.t[:, 0:N], in_=ps1.t[:, 0:N], func=AF.Copy, scale=1.0 / 1024.0), reads=[ps1], writes=[mean])
        self.act(lambda: A.activation(out=msq.t[:, 0:N], in_=ps1.t[:, 0:N], func=AF.Square, scale=1.0 / 1024.0), reads=[ps1], writes=[msq])
        self.dve(lambda: V.scalar_tensor_tensor(out=var.t[:, 0:N], in0=ps2.t[:, 0:N], scalar=1.0 / 1024.0, in1=msq.t[:, 0:N],
                                                op0=ALU.mult, op1=ALU.subtract), reads=[ps2, msq], writes=[var])
        self.act(lambda: A.activation(out=lnv.t[:, 0:N], in_=var.t[:, 0:N], func=AF.Ln, bias=LN_EPS), reads=[var], writes=[lnv])
        self.act(lambda: A.activation(out=rstd.t[:, 0:N], in_=lnv.t[:, 0:N], func=AF.Exp, scale=-0.5), reads=[lnv], writes=[rstd])
        for c in range(8):
            t = self.ln_t[c % 2]
            self.dve(lambda: V.tensor_tensor(out=t.t[:, 0:N], in0=y.t[:, c, ycols], in1=mean.t[:, 0:N], op=ALU.subtract), reads=[y, mean], writes=[t])
            self.dve(lambda: V.tensor_tensor(out=t.t[:, 0:N], in0=t.t[:, 0:N], in1=rstd.t[:, 0:N], op=ALU.mult), reads=[t, rstd], writes=[t])
            if out_f is not None:
                self.act(lambda: A.activation(out=out_f.t[:, c, out_f_cols], in_=t.t[:, 0:N], func=AF.Identity,
                                              scale=gb.t[:, gi, c:c + 1], bias=gb.t[:, bi, c:c + 1]), reads=[t, gb], writes=[out_f])
            if out_b is not None:
                self.pool(lambda: G.tensor_scalar(out=out_b.t[:, c, out_b_cols], in0=t.t[:, 0:N], scalar1=gb.t[:, gi, c:c + 1],
                                                  scalar2=gb.t[:, bi, c:c + 1], op0=ALU.mult, op1=ALU.add), reads=[t, gb], writes=[out_b])

    def glu(self, xb, acc, NT, wg, wu, wd, F, psA, psB, psD, gate=None, GCH=4):
        V, A, G, PE = self.V, self.A, self.G, self.PE
        if not hasattr(self, "glu_w"):
            self.glu_w = [(self.sb(f"glu_wg{i}", [128, 8, GCH * 128], BF16), self.sb(f"glu_wu{i}", [128, 8, GCH * 128], BF16),
                           self.sb(f"glu_wd{i}", [128, GCH, 1024], BF16)) for i in range(3)]
            self.glu_h = [self.sb(f"glu_h{i}", [128, GCH, 512], BF16) for i in range(2)]
            self.glu_sg = [self.sb(f"glu_sg{i}", [128, 512]) for i in range(2)]
            self.glu_tt = [self.sb(f"glu_tt{i}", [128, 512]) for i in range(2)]
            self.glu_cnt = 0
        wgr = wg.rearrange("(c p) f -> p c f", p=128)
        wur = wu.rearrange("(c p) f -> p c f", p=128)
        groups = []
        f0 = 0
        while f0 < F:
            nch = min(GCH, (F - f0) // 128)
            groups.append((f0, nch))
            f0 += nch * 128
        it = 0
        base = self.glu_cnt
        self.glu_cnt += len(groups)

        def load(gi_):
            f0, nch = groups[gi_]
            s = (base + gi_) % 3
            Wg, Wu, Wd = self.glu_w[s]
            fw_ = nch * 128
            self.dma("pool", Wg.t[:, :, 0:fw_], wgr[:, :, f0:f0 + fw_], f"glw{s}", writes=[Wg])
            self.dma("pool", Wu.t[:, :, 0:fw_], wur[:, :, f0:f0 + fw_], f"glw{s}", writes=[Wu])
            self.dma("pool", Wd.t[:, 0:nch, :], wd[f0:f0 + fw_, :].rearrange("(c p) o -> p c o", p=128), f"glw{s}", writes=[Wd])
        load(0)
        pending = None
        for gi_, (f0, nch) in enumerate(groups):
            if gi_ + 1 < len(groups):
                load(gi_ + 1)
            Wg, Wu, Wd = self.glu_w[(base + gi_) % 3]
            for tt in range(NT):
                ts_ = slice(tt * 512, (tt + 1) * 512)
                H = self.glu_h[it % 2]
                it += 1
                for fc in range(nch):
                    pa = psA[fc % 2]
                    pb = psB[fc % 2]
                    sg = self.glu_sg[fc % 2]
                    t2 = self.glu_tt[fc % 2]
                    self.mm(pa.t[:], [(Wg.t[:, kc, fc * 128:(fc + 1) * 128], xb.t[:, kc, ts_]) for kc in range(8)], [Wg, xb], [pa])
                    self.mm(pb.t[:], [(Wu.t[:, kc, fc * 128:(fc + 1) * 128], xb.t[:, kc, ts_]) for kc in range(8)], [Wu, xb], [pb])
                    self.act(lambda: A.activation(out=sg.t[:], in_=pa.t[:], func=AF.Silu), reads=[pa], writes=[sg])
                    if gate is None:
                        self.dve(lambda: V.tensor_tensor(out=H.t[:, fc, :], in0=sg.t[:], in1=pb.t[:], op=ALU.mult), reads=[sg, pb], writes=[H])
                    else:
                        self.dve(lambda: V.tensor_tensor(out=t2.t[:], in0=sg.t[:], in1=pb.t[:], op=ALU.mult), reads=[sg, pb], writes=[t2])
                        self.pool(lambda: G.tensor_tensor(out=H.t[:, fc, :], in0=t2.t[:], in1=gate.t[:, ts_], op=ALU.mult), reads=[t2, gate], writes=[H])
                def down(Wd=Wd, H=H, nch=nch, ts_=ts_):
                    for oc in range(8):
                        pd = psD[oc % 2]
                        self.mm(pd.t[:], [(Wd.t[:, fc, oc * 128:(oc + 1) * 128], H.t[:, fc, :]) for fc in range(nch)], [Wd, H], [pd])
                        self.dve(lambda: V.tensor_tensor(out=acc.t[:, oc, ts_], in0=acc.t[:, oc, ts_], in1=pd.t[:], op=ALU.add), reads=[acc, pd], writes=[acc])
                if pending is not None:
                    pending()
                pending = down
        if pending is not None:
            pending()


def build_l2(nc, TC):
    NT = TC // 512
    xT = nc.dram_tensor("xT", [1024, TC], F32, kind="ExternalInput").ap()
    ogT = nc.dram_tensor("ogT", [1024, TC], F32, kind="ExternalInput").ap()
    wo = nc.dram_tensor("wo", [1024, 1024], F32, kind="ExternalInput").ap()
    lnp = nc.dram_tensor("lnp", [128, 4, 8], F32, kind="ExternalInput").ap()
    wg = nc.dram_tensor("wg", [1024, 2816], F32, kind="ExternalInput").ap()
    wu = nc.dram_tensor("wu", [1024, 2816], F32, kind="ExternalInput").ap()
    wd = nc.dram_tensor("wd", [2816, 1024], F32, kind="ExternalInput").ap()
    outT = nc.dram_tensor("outT", [1024, TC], F32, kind="ExternalOutput").ap()
    with ExitStack() as es:
        k = KC(nc, es)
        V, A, G, PE = k.V, k.A, k.G, k.PE
        k.consts()
        banks = [k.bank(f"bk{i}") for i in range(8)]
        gb = k.sb("gb", [128, 4, 8])
        k.dma("sp", gb.t[:], lnp, "c0", writes=[gb])
        acc = k.sb("acc", [128, 8, TC])
        x1b = k.sb("x1b", [128, 8, TC], BF16)
        xTr = xT.rearrange("(c p) t -> p c t", p=128)
        ogTr = ogT.rearrange("(c p) t -> p c t", p=128)
        outTr = outT.rearrange("(c p) t -> p c t", p=128)
        with ExitStack() as es2:
            k2es = es2
            wo_b = TL(es2.enter_context(nc.sbuf_tensor("wo_b", [128, 8, 1024], BF16)), "wo_b")
            og_b = TL(es2.enter_context(nc.sbuf_tensor("og_b", [128, 8, 512], BF16)), "og_b")
            xf = TL(es2.enter_context(nc.sbuf_tensor("xf", [128, 8, 512], F32)), "xf")
            y = TL(es2.enter_context(nc.sbuf_tensor("y", [128, 8, 512], F32)), "y")
            k.es = es2
            k.fw.es = es2
            k.ln_alloc()
            k.es = es
            k.fw.es = es
            k.dma("pool", wo_b.t[:], wo.rearrange("(c p) n -> p c n", p=128), "c1", writes=[wo_b])
            for tt in range(NT):
                ts_ = slice(tt * 512, (tt + 1) * 512)
                k.dma("pool", og_b.t[:], ogTr[:, :, ts_], "og", writes=[og_b])
                k.dma("sp", xf.t[:], xTr[:, :, ts_], "xf", writes=[xf])
                for oc in range(8):
                    pb = banks[oc % 2]
                    k.mm(pb.t[:], [(wo_b.t[:, kc, oc * 128:(oc + 1) * 128], og_b.t[:, kc, :]) for kc in range(8)], [wo_b, og_b], [pb])
                    k.dve(lambda: V.scalar_tensor_tensor(out=y.t[:, oc, :], in0=xf.t[:, oc, :], scalar=ALPHA, in1=pb.t[:],
                                                         op0=ALU.mult, op1=ALU.add), reads=[xf, pb], writes=[y])
                k.layernorm(y, slice(0, 512), gb, 0, 1, banks[2], banks[3], out_f=xf, out_f_cols=slice(0, 512), out_b=x1b, out_b_cols=ts_)
                for c in range(8):
                    k.act(lambda: A.activation(out=acc.t[:, c, ts_], in_=xf.t[:, c, :], func=AF.Copy, scale=ALPHA), reads=[xf], writes=[acc])
            k.barrier()
        k.glu(x1b, acc, NT, wg, wu, wd, 2816, banks[0:2], banks[2:4], banks[4:6])
        k.barrier()
        k.ln_alloc2 = True
        k.ln_sq = [k.sb(f"l2_sq{i}", [128, 512]) for i in range(2)]
        k.ln_mean = k.sb("l2_mean", [128, 512]); k.ln_msq = k.sb("l2_msq", [128, 512]); k.ln_var = k.sb("l2_var", [128, 512])
        k.ln_lnv = k.sb("l2_lnv", [128, 512]); k.ln_rstd = k.sb("l2_rstd", [128, 512])
        k.ln_t = [k.sb(f"l2_t{i}", [128, 512]) for i in range(2)]
        for tt in range(NT):
            ts_ = slice(tt * 512, (tt + 1) * 512)
            k.layernorm(acc, ts_, gb, 2, 3, banks[6], banks[7], out_f=acc, out_f_cols=ts_)
            k.dma("sp", outTr[:, :, ts_], acc.t[:, :, ts_], f"out{tt % 2}", reads=[acc])
        k.finish(["out0", "out1"])
        print("L2 instrs", k.fw.ninstr, "waits", k.fw.nwaits)
    return nc


def build_l3b(nc, TC):
    NT = TC // 512
    NB = TC // 128
    x2T = nc.dram_tensor("x2T", [1024, TC], F32, kind="ExternalInput").ap()
    wr = nc.dram_tensor("wr", [1024, 8], F32, kind="ExternalInput").ap()
    lnp = nc.dram_tensor("lnp", [128, 2, 8], F32, kind="ExternalInput").ap()
    mwg = nc.dram_tensor("mwg", [8, 1024, 3584], F32, kind="ExternalInput").ap()
    mwu = nc.dram_tensor("mwu", [8, 1024, 3584], F32, kind="ExternalInput").ap()
    mwd = nc.dram_tensor("mwd", [8, 3584, 1024], F32, kind="ExternalInput").ap()
    outT = nc.dram_tensor("outT", [1024, TC], F32, kind="ExternalOutput").ap()
    with ExitStack() as es:
        k = KC(nc, es)
        V, A, G, PE = k.V, k.A, k.G, k.PE
        k.consts()
        banks = [k.bank(f"bk{i}") for i in range(8)]
        gb = k.sb("gb", [128, 2, 8])
        k.dma("sp", gb.t[:], lnp, "c0", writes=[gb])
        acc = k.sb("acc", [128, 8, TC])
        x2b = k.sb("x2b", [128, 8, TC], BF16)
        gates = k.sb("gates", [128, NB, 8])
        x2Tr = x2T.rearrange("(c p) t -> p c t", p=128)
        outTr = outT.rearrange("(c p) t -> p c t", p=128)
        k.dma("pool", x2b.t[:], x2Tr, "c1", writes=[x2b])
        with ExitStack() as es2:
            k.es = es2; k.fw.es = es2
            xf = [k.sb(f"xf{i}", [128, 8, 512]) for i in range(2)]
            wr_s = k.sb("wr_s", [128, 8, 8])
            lg = [k.sb(f"lg{i}", [128, 8]) for i in range(2)]
            m1 = [k.sb(f"m1{i}", [128, 1]) for i in range(2)]
            nm1 = [k.sb(f"nm1{i}", [128, 1]) for i in range(2)]
            eq1 = [k.sb(f"eq1{i}", [128, 8]) for i in range(2)]
            l2 = [k.sb(f"l2{i}", [128, 8]) for i in range(2)]
            m2 = [k.sb(f"m2{i}", [128, 1]) for i in range(2)]
            sel = [k.sb(f"sel{i}", [128, 8]) for i in range(2)]
            ex = [k.sb(f"ex{i}", [128, 8]) for i in range(2)]
            e2 = [k.sb(f"e2{i}", [128, 8]) for i in range(2)]
            ssum = [k.sb(f"ssum{i}", [128, 1]) for i in range(2)]
            rs = [k.sb(f"rs{i}", [128, 1]) for i in range(2)]
            k.es = es; k.fw.es = es
            k.dma("sp", wr_s.t[:], wr.rearrange("(c p) e -> p c e", p=128), "c0", writes=[wr_s])
            for tt in range(NT):
                ts_ = slice(tt * 512, (tt + 1) * 512)
                X = xf[tt % 2]
                k.dma("sp", X.t[:], x2Tr[:, :, ts_], f"xf{tt % 2}", writes=[X])
                for c in range(8):
                    k.act(lambda: A.activation(out=acc.t[:, c, ts_], in_=X.t[:, c, :], func=AF.Copy, scale=ALPHA), reads=[X], writes=[acc])
                for bl in range(4):
                    b_ = tt * 4 + bl
                    i = b_ % 2
                    pb = banks[i]
                    k.mm(pb.t[:, 0:8], [(X.t[:, kc, bl * 128:(bl + 1) * 128], wr_s.t[:, kc, :]) for kc in range(8)], [X, wr_s], [pb])
                    k.act(lambda: A.copy(out=lg[i].t[:], in_=pb.t[:, 0:8]), reads=[pb], writes=[lg[i]])
                    k.dve(lambda: V.tensor_reduce(out=m1[i].t[:], in_=lg[i].t[:], axis=AX.X, op=ALU.max), reads=[lg[i]], writes=[m1[i]])
                    k.act(lambda: A.activation(out=nm1[i].t[:], in_=m1[i].t[:], func=AF.Copy, scale=-1.0), reads=[m1[i]], writes=[nm1[i]])
                    k.dve(lambda: V.tensor_scalar(out=eq1[i].t[:], in0=lg[i].t[:], scalar1=m1[i].t[:, 0:1], scalar2=None, op0=ALU.is_equal),
                          reads=[lg[i], m1[i]], writes=[eq1[i]])
                    k.dve(lambda: V.scalar_tensor_tensor(out=l2[i].t[:], in0=eq1[i].t[:], scalar=-1e30, in1=lg[i].t[:], op0=ALU.mult, op1=ALU.add),
                          reads=[eq1[i], lg[i]], writes=[l2[i]])
                    k.dve(lambda: V.tensor_reduce(out=m2[i].t[:], in_=l2[i].t[:], axis=AX.X, op=ALU.max), reads=[l2[i]], writes=[m2[i]])
                    k.dve(lambda: V.tensor_scalar(out=sel[i].t[:], in0=lg[i].t[:], scalar1=m2[i].t[:, 0:1], scalar2=None, op0=ALU.is_ge),
                          reads=[lg[i], m2[i]], writes=[sel[i]])
                    k.act(lambda: A.activation(out=ex[i].t[:], in_=lg[i].t[:], func=AF.Exp, bias=nm1[i].t[:, 0:1]), reads=[lg[i], nm1[i]], writes=[ex[i]])
                    k.dve(lambda: V.tensor_tensor(out=e2[i].t[:], in0=ex[i].t[:], in1=sel[i].t[:], op=ALU.mult), reads=[ex[i], sel[i]], writes=[e2[i]])
                    k.dve(lambda: V.tensor_reduce(out=ssum[i].t[:], in_=e2[i].t[:], axis=AX.X, op=ALU.add), reads=[e2[i]], writes=[ssum[i]])
                    k.dve(lambda: V.reciprocal(out=rs[i].t[:], in_=ssum[i].t[:]), reads=[ssum[i]], writes=[rs[i]])
                    k.dve(lambda: V.tensor_scalar(out=gates.t[:, b_, :], in0=e2[i].t[:], scalar1=rs[i].t[:, 0:1], scalar2=None, op0=ALU.mult),
                          reads=[e2[i], rs[i]], writes=[gates])
            k.barrier()
        gcolB = [k.sb(f"gcolB{i}", [128, 128]) for i in range(2)]
        gbc = [k.sb(f"gbc{i}", [128, TC], BF16) for i in range(2)]
        for e in range(8):
            Gb = gbc[e % 2]
            for b_ in range(NB):
                gc_ = gcolB[b_ % 2]
                pb = banks[6 + (b_ // 4) % 2]
                k.act(lambda: A.activation(out=gc_.t[:], in_=k.onesf.t[:], func=AF.Copy, scale=gates.t[:, b_, e:e + 1]), reads=[k.onesf, gates], writes=[gc_])
                k.mm(pb.t[:, (b_ % 4) * 128:(b_ % 4 + 1) * 128], [(gc_.t[:], k.identf.t[:])], [gc_, k.identf], [pb])
                if b_ % 4 == 3:
                    t0 = (b_ // 4) * 512
                    k.act(lambda: A.copy(out=Gb.t[:, t0:t0 + 512], in_=pb.t[:]), reads=[pb], writes=[Gb])
            k.glu(x2b, acc, NT, mwg[e], mwu[e], mwd[e], 3584, banks[0:2], banks[2:4], banks[4:6], gate=Gb, GCH=2)
        k.barrier()
        k.ln_alloc()
        for tt in range(NT):
            ts_ = slice(tt * 512, (tt + 1) * 512)
            k.layernorm(acc, ts_, gb, 0, 1, banks[6], banks[7], out_f=acc, out_f_cols=ts_)
            k.dma("sp", outTr[:, :, ts_], acc.t[:, :, ts_], f"out{tt % 2}", reads=[acc])
        k.finish(["out0", "out1"])
        print("L3b instrs", k.fw.ninstr, "waits", k.fw.nwaits)
    return nc


class _Stop(Exception):
    pass


def build_l3a(nc, TC):
    STOP3 = 99
    NT = TC // 512
    TE = TC + 128
    NBE = TE // 128
    x1e = nc.dram_tensor("x1e", [1024, TE], F32, kind="ExternalInput").ap()
    wq = nc.dram_tensor("wq", [1024, 1024], F32, kind="ExternalInput").ap()
    bq = nc.dram_tensor("bq", [128, 8], F32, kind="ExternalInput").ap()
    wkv = nc.dram_tensor("wkv", [1024, 256], F32, kind="ExternalInput").ap()
    bkv = nc.dram_tensor("bkv", [128, 2], F32, kind="ExternalInput").ap()
    wo = nc.dram_tensor("wo", [1024, 1024], F32, kind="ExternalInput").ap()
    biasT = nc.dram_tensor("biasT", [128, 8, 512], F32, kind="ExternalInput").ap()
    maskT = nc.dram_tensor("maskT", [128, 512], F32, kind="ExternalInput").ap()
    fmT = nc.dram_tensor("fmT", [128, 512], F32, kind="ExternalInput").ap()
    sinkb = nc.dram_tensor("sinkb", [128, 16], F32, kind="ExternalInput").ap()
    lnp = nc.dram_tensor("lnp", [128, 2, 8], F32, kind="ExternalInput").ap()
    x2T = nc.dram_tensor("x2T", [1024, TC], F32, kind="ExternalOutput").ap()
    with ExitStack() as es:
        k = KC(nc, es)
        V, A, G, PE = k.V, k.A, k.G, k.PE
        k.consts()
        bk = [k.bank(f"bk{i}") for i in range(5)]
        btr = k.bank("btr", BF16)
        bl1 = k.bank("bl1"); bl2 = k.bank("bl2")
        gb = k.sb("gb", [128, 2, 8]); k.dma("sp", gb.t[:], lnp, "c0", writes=[gb])
        bq_s = k.sb("bq_s", [128, 8]); k.dma("sp", bq_s.t[:], bq, "c0", writes=[bq_s])
        bkv_s = k.sb("bkv_s", [128, 2]); k.dma("sp", bkv_s.t[:], bkv, "c0", writes=[bkv_s])
        bm = k.sb("bm", [128, 8, 512]); k.dma("sp", bm.t[:], biasT, "c2", writes=[bm])
        mk = k.sb("mk", [128, 512]); k.dma("sp", mk.t[:], maskT, "c0", writes=[mk])
        fm = k.sb("fm", [128, 512]); k.dma("sp", fm.t[:], fmT, "c0", writes=[fm])
        snk = k.sb("snk", [128, 16]); k.dma("sp", snk.t[:], sinkb, "c0", writes=[snk])
        esink = k.sb("esink", [128, 16])
        k.act(lambda: A.activation(out=esink.t[:], in_=snk.t[:], func=AF.Exp), reads=[snk], writes=[esink])
        bqs = k.sb("bqs", [128, 8])
        k.dve(lambda: V.tensor_scalar(out=bqs.t[:], in0=bq_s.t[:], scalar1=0.125, scalar2=None, op0=ALU.mult), reads=[bq_s], writes=[bqs])
        for c in range(8):
            k.pool(lambda: G.tensor_tensor(out=bm.t[:, c, :], in0=bm.t[:, c, :], in1=mk.t[:], op=ALU.add), reads=[bm, mk], writes=[bm])
        wq_b = k.sb("wq_b", [128, 8, 1024], BF16); k.dma("pool", wq_b.t[:], wq.rearrange("(c p) n -> p c n", p=128), "w0", writes=[wq_b])
        wkv_b = k.sb("wkv_b", [128, 8, 256], BF16); k.dma("pool", wkv_b.t[:], wkv.rearrange("(c p) n -> p c n", p=128), "w1", writes=[wkv_b])
        wo_b = k.sb("wo_b", [128, 8, 1024], BF16); k.dma("pool", wo_b.t[:], wo.rearrange("(c p) n -> p c n", p=128), "w2", writes=[wo_b])
        x1er = x1e.rearrange("(c p) t -> p c t", p=128)
        x2Tr = x2T.rearrange("(c p) t -> p c t", p=128)
        xbe = k.sb("xbe", [128, 8, TE], BF16); k.dma("pool", xbe.t[:], x1er, "w3", writes=[xbe])
        kTz = [k.sb(f"kTz{i}", [128, TE], BF16) for i in range(2)]
        for i in range(2):
            k.pool(lambda: G.memset(kTz[i].t[:], 0.0), writes=[kTz[i]])
        vTt = k.sb("vTt", [128, 512], BF16)
        vaug = k.sb("vaug", [128, NBE, 2, 80], BF16)
        k.pool(lambda: G.memset(vaug.t[:], 1.0), writes=[vaug])
        col = 0
        while col < TE:
            n = min(512, TE - col)
            cs = slice(col, col + n)
            k.mm(bk[0].t[:, 0:n], [(wkv_b.t[:, kc, 0:128], xbe.t[:, kc, cs]) for kc in range(8)], [wkv_b, xbe], [bk[0]])
            for kv in range(2):
                ps_ = slice(kv * 64, (kv + 1) * 64)
                k.act(lambda: A.activation(out=kTz[kv].t[ps_, cs], in_=bk[0].t[ps_, 0:n], func=AF.Identity, bias=bkv_s.t[ps_, 0:1]),
                      reads=[bk[0], bkv_s], writes=[kTz[kv]])
            k.mm(bk[1].t[:, 0:n], [(wkv_b.t[:, kc, 128:256], xbe.t[:, kc, cs]) for kc in range(8)], [wkv_b, xbe], [bk[1]])
            k.act(lambda: A.activation(out=vTt.t[:, 0:n], in_=bk[1].t[:, 0:n], func=AF.Identity, bias=bkv_s.t[:, 1:2]), reads=[bk[1], bkv_s], writes=[vTt])
            for j in range(n // 128):
                blk = col // 128 + j
                k.pe(lambda: PE.transpose(btr.t[:, 0:128], vTt.t[:, j * 128:(j + 1) * 128], k.identb.t[:]), reads=[vTt, k.identb], writes=[btr])
                k.act(lambda: A.copy(out=vaug.t[:, blk, :, 0:64], in_=btr.t[:, 0:128].rearrange("p (k d) -> p k d", k=2)), reads=[btr], writes=[vaug])
            col += n
        qT = k.sb("qT", [128, 8, 512], BF16)
        stop_here = (STOP3 == 1)
        attnT = k.sb("attnT", [128, 8, 512], BF16)
        sbs = [k.sb(f"sbs{i}", [128, 512]) for i in range(2)]
        pT = [k.sb(f"pT{i}", [128, 512], BF16) for i in range(2)]
        den = [k.sb(f"den{i}", [128, 2]) for i in range(2)]
        rinv = [k.sb(f"rinv{i}", [128, 2]) for i in range(2)]
        on = [k.sb(f"on{i}", [128, 128], BF16) for i in range(2)]
        xf = k.sb("xf", [128, 8, 512])
        y = k.sb("y", [128, 8, 512])
        k.ln_alloc()
        it = 0
        for tt in range(NT if not stop_here else 0):
            e0 = 128 + tt * 512
            for c in range(8):
                pb = bk[c % 2]
                k.mm(pb.t[:], [(wq_b.t[:, kc, c * 128:(c + 1) * 128], xbe.t[:, kc, e0:e0 + 512]) for kc in range(8)], [wq_b, xbe], [pb])
                k.act(lambda: A.activation(out=qT.t[:, c, :], in_=pb.t[:], func=AF.Identity, scale=0.125, bias=bqs.t[:, c:c + 1]),
                      reads=[pb, bqs], writes=[qT])
            if STOP3 == 2: break
            for bl in range(4):
                ne = 1 + tt * 4 + bl
                first = (tt == 0 and bl == 0)
                for c in range(8):
                    i = it % 2
                    it += 1
                    st = bk[2 + i]
                    def fsc():
                        last = None
                        for kv in range(2):
                            for half in range(2):
                                q0 = (kv * 2 + half) * 128
                                last = PE.matmul(st.t[:, q0:q0 + 128], lhsT=kTz[kv].t[:, (ne - 1 + half) * 128:(ne + half) * 128],
                                                 rhs=qT.t[:, c, bl * 128:(bl + 1) * 128], start=True, stop=True)
                        return last
                    k.pe(fsc, reads=[kTz[0], kTz[1], qT], writes=[st])
                    if STOP3 == 3: continue
                    S_ = sbs[i]
                    k.dve(lambda: V.tensor_tensor(out=S_.t[:], in0=st.t[:], in1=bm.t[:, c, :], op=ALU.add), reads=[st, bm], writes=[S_])
                    if first:
                        k.dve(lambda: V.tensor_tensor(out=S_.t[:], in0=S_.t[:], in1=fm.t[:], op=ALU.add), reads=[S_, fm], writes=[S_])
                    P_ = pT[i]
                    k.act(lambda: A.activation(out=P_.t[:], in_=S_.t[:], func=AF.Exp), reads=[S_], writes=[P_])
                    if STOP3 == 4: continue
                    ob = bk[4]
                    for kv in range(2):
                        k.mm(ob.t[:, kv * 128:kv * 128 + 65],
                             [(P_.t[:, (kv * 2 + half) * 128:(kv * 2 + half + 1) * 128], vaug.t[:, ne - 1 + half, kv, 0:65]) for half in range(2)],
                             [P_, vaug], [ob])
                    if STOP3 == 5: continue
                    D_ = den[i]; R_ = rinv[i]; O_ = on[i]
                    for kv in range(2):
                        k.act(lambda: A.activation(out=D_.t[:, kv:kv + 1], in_=ob.t[:, kv * 128 + 64:kv * 128 + 65], func=AF.Identity,
                                                   bias=esink.t[:, 2 * c + kv:2 * c + kv + 1]), reads=[ob, esink], writes=[D_])
                    k.dve(lambda: V.reciprocal(out=R_.t[:], in_=D_.t[:]), reads=[D_], writes=[R_])
                    for kv in range(2):
                        k.dve(lambda: V.tensor_scalar(out=O_.t[:, kv * 64:(kv + 1) * 64], in0=ob.t[:, kv * 128:kv * 128 + 64], scalar1=R_.t[:, kv:kv + 1],
                                                      scalar2=None, op0=ALU.mult), reads=[ob, R_], writes=[O_])
                    if STOP3 == 6: continue
                    k.pe(lambda: PE.transpose(btr.t[:, 0:128], O_.t[:], k.identb.t[:]), reads=[O_, k.identb], writes=[btr])
                    k.act(lambda: A.copy(out=attnT.t[:, c, bl * 128:(bl + 1) * 128], in_=btr.t[:, 0:128]), reads=[btr], writes=[attnT])
            k.dma("sp", xf.t[:], x1er[:, :, e0:e0 + 512], "xf", writes=[xf])
            for oc in range(8):
                pb = bk[oc % 2]
                k.mm(pb.t[:], [(wo_b.t[:, kc, oc * 128:(oc + 1) * 128], attnT.t[:, kc, :]) for kc in range(8)], [wo_b, attnT], [pb])
                k.dve(lambda: V.scalar_tensor_tensor(out=y.t[:, oc, :], in0=xf.t[:, oc, :], scalar=ALPHA, in1=pb.t[:], op0=ALU.mult, op1=ALU.add),
                      reads=[xf, pb], writes=[y])
            k.layernorm(y, slice(0, 512), gb, 0, 1, bl1, bl2, out_f=y, out_f_cols=slice(0, 512))
            k.dma("sp", x2Tr[:, :, tt * 512:(tt + 1) * 512], y.t[:], "out", reads=[y])
        k.finish(["out"])
        print("L3a instrs", k.fw.ninstr, "waits", k.fw.nwaits)
    return nc


def t5_bucket_np(dist):
    max_exact = 16
    df = np.maximum(dist, 1).astype(np.float32)
    large = max_exact + (np.log(df / max_exact) / math.log(128 / max_exact) * (32 - max_exact)).astype(np.int32)
    large = np.minimum(large, 31)
    return np.where(dist < max_exact, dist, large)
def swa_tables(rel_bias):
    s = np.arange(128)[:, None, None, None]
    kv = np.arange(2)[None, :, None, None]
    half = np.arange(2)[None, None, :, None]
    i = np.arange(128)[None, None, None, :]
    dist = i + 128 - half * 128 - s + 0 * kv
    valid = (dist >= 0) & (dist < 128)
    bucket = t5_bucket_np(np.maximum(dist, 0))
    biasT = np.zeros((128, 8, 2, 2, 128), np.float32)
    for c in range(8):
        for k_ in range(2):
            head = c + 8 * k_
            biasT[:, c, k_] = rel_bias[bucket[:, k_], head]
    maskT = np.where(valid, 0.0, -30000.0).astype(np.float32).reshape(128, 512)
    return biasT.reshape(128, 8, 512), maskT
def swa_inputs(x1e, b_w_in, b_b_in, b_sinks, b_w_out, rel_bias, ln_g, ln_b, first):
    perm = np.concatenate([np.concatenate([np.arange(c * 64, (c + 1) * 64), np.arange((8 + c) * 64, (9 + c) * 64)]) for c in range(8)])
    wq = np.ascontiguousarray(b_w_in[:, :1024][:, perm])
    bq = np.ascontiguousarray(b_b_in[:1024][perm].reshape(8, 128).T)
    wkv = np.ascontiguousarray(b_w_in[:, 1024:1280])
    bkv = np.ascontiguousarray(b_b_in[1024:1280].reshape(2, 128).T)
    wo = np.ascontiguousarray(b_w_out[perm, :])
    biasT, maskT = swa_tables(rel_bias)
    fm = np.zeros((128, 2, 2, 128), np.float32)
    if first:
        fm[:, :, 0, :] = -30000.0
    sink_order = np.array([c + 8 * k_ for c in range(8) for k_ in range(2)])
    sinkb = np.ascontiguousarray(np.broadcast_to(b_sinks[sink_order][None, :], (128, 16))).astype(np.float32)
    lnp = np.stack([ln_g, ln_b], 0).reshape(2, 8, 128).transpose(2, 0, 1).copy().astype(np.float32)
    return {"x1e": np.ascontiguousarray(x1e.T), "wq": wq, "bq": bq, "wkv": wkv, "bkv": bkv, "wo": wo, "biasT": biasT, "maskT": maskT,
            "fmT": fm.reshape(128, 512), "sinkb": sinkb, "lnp": lnp}


_NC_CACHE = {}


def _get_nc(name, builder, *args):
    if name not in _NC_CACHE:
        nc = bass.Bass("TRN2", target_bir_lowering=False)
        builder(nc, *args)
        _NC_CACHE[name] = nc
    return _NC_CACHE[name]


def _gdn_inputs(xb, j, w_in, conv_w, a_log, dt_bias, norm_w):
    q0 = 0; k0 = 512; v0 = 1024; z0 = 2048; b0 = 3072; a0 = 3080
    cols_qkv = np.concatenate([np.arange(q0 + j * 128, q0 + (j + 1) * 128), np.arange(k0 + j * 128, k0 + (j + 1) * 128),
                               np.arange(v0 + 2 * j * 128, v0 + (2 * j + 2) * 128)])
    cols_zba = np.concatenate([np.arange(z0 + 2 * j * 128, z0 + (2 * j + 2) * 128),
                               [b0 + 2 * j, b0 + 2 * j + 1, a0 + 2 * j, a0 + 2 * j + 1]])
    return {
        "xT": np.ascontiguousarray(xb.T),
        "wqkv": np.ascontiguousarray(w_in[:, cols_qkv]),
        "wzba": np.ascontiguousarray(w_in[:, cols_zba]),
        "convw": np.ascontiguousarray(conv_w[:, cols_qkv].T),
        "hp": np.array([[a_log[2 * j], a_log[2 * j + 1], dt_bias[2 * j], dt_bias[2 * j + 1]]], np.float32),
        "normw": np.ascontiguousarray(norm_w[None, :]).astype(np.float32),
    }


def _lncols(rows):
    return np.stack(rows, 0).reshape(len(rows), 8, 128).transpose(2, 0, 1).copy().astype(np.float32)


def kernel(x, a_w_in, a_conv_w, a_a_log, a_dt_bias, a_norm_w, a_w_out, b_w_in, b_b_in, b_sinks,
           b_w_out, rel_bias, ffn_w_gate, ffn_w_up, ffn_w_down, moe_router, moe_w_gate, moe_w_up,
           moe_w_down, ln_g, ln_b):
    f32 = lambda a: np.ascontiguousarray(np.asarray(a, dtype=np.float32))
    x = f32(x)
    B, T, D = x.shape
    NCORE = 8
    TC = (B * T) // NCORE
    per_b = T // TC
    cores = list(range(NCORE))
    ln_g = f32(ln_g); ln_b = f32(ln_b)

    nc1 = _get_nc("gdn", build_gdn, T)
    w_in = f32(a_w_in[0]); conv_w = f32(a_conv_w[0])
    a_log = f32(a_a_log[0]); dtb = f32(a_dt_bias[0]); nw = f32(a_norm_w[0])
    im1 = [_gdn_inputs(x[c // 4], c % 4, w_in, conv_w, a_log, dtb, nw) for c in cores]
    r1 = run_bass_kernel_spmd(nc1, im1, core_ids=cores).results
    og = np.empty((B, T, D), np.float32)
    for c in cores:
        og[c // 4, :, (c % 4) * 256:(c % 4 + 1) * 256] = r1[c]["og"]

    def tok(c):
        return c // per_b, (c % per_b) * TC

    nc2 = _get_nc("l2", build_l2, TC)
    wo0 = f32(a_w_out[0]); wg = f32(ffn_w_gate[0]); wu = f32(ffn_w_up[0]); wd = f32(ffn_w_down[0])
    lnp0 = _lncols([ln_g[0, 0], ln_b[0, 0], ln_g[0, 1], ln_b[0, 1]])
    im2 = []
    for c in cores:
        b, t0 = tok(c)
        im2.append({"xT": np.ascontiguousarray(x[b, t0:t0 + TC].T), "ogT": np.ascontiguousarray(og[b, t0:t0 + TC].T),
                    "wo": wo0, "lnp": lnp0, "wg": wg, "wu": wu, "wd": wd})
    r2 = run_bass_kernel_spmd(nc2, im2, core_ids=cores).results
    x1 = np.empty((B, T, D), np.float32)
    for c in cores:
        b, t0 = tok(c)
        x1[b, t0:t0 + TC] = r2[c]["outT"].T

    nc3 = _get_nc("l3a", build_l3a, TC)
    im3 = []
    for c in cores:
        b, t0 = tok(c)
        x1e = np.zeros((TC + 128, D), np.float32)
        if t0 > 0:
            x1e[:128] = x1[b, t0 - 128:t0]
        x1e[128:] = x1[b, t0:t0 + TC]
        im3.append(swa_inputs(x1e, f32(b_w_in[0]), f32(b_b_in[0]), f32(b_sinks[0]), f32(b_w_out[0]), f32(rel_bias),
                              ln_g[1, 0], ln_b[1, 0], t0 == 0))
    r3 = run_bass_kernel_spmd(nc3, im3, core_ids=cores).results

    nc4 = _get_nc("l3b", build_l3b, TC)
    wr = f32(moe_router[0]); mwg = f32(moe_w_gate[0]); mwu = f32(moe_w_up[0]); mwd = f32(moe_w_down[0])
    lnp1 = _lncols([ln_g[1, 1], ln_b[1, 1]])
    im4 = [{"x2T": np.ascontiguousarray(r3[c]["x2T"]), "wr": wr, "lnp": lnp1, "mwg": mwg, "mwu": mwu, "mwd": mwd} for c in cores]
    r4 = run_bass_kernel_spmd(nc4, im4, core_ids=cores).results
    out = np.empty((B, T, D), np.float32)
    for c in cores:
        b, t0 = tok(c)
        out[b, t0:t0 + TC] = r4[c]["outT"].T
    return out
```

```python
import math
import numpy as np
from contextlib import ExitStack
import concourse.bass as bass
import concourse.mybir as mybir
from concourse.bass_utils import run_bass_kernel_spmd

F32 = mybir.dt.float32
BF16 = mybir.dt.bfloat16
AF = mybir.ActivationFunctionType
ALU = mybir.AluOpType
AX = mybir.AxisListType


class Res:
    __slots__ = ("name", "lw", "rd", "psum")

    def __init__(self, name, psum=False):
        self.name = name
        self.psum = psum
        self.lw = None
        self.rd = {}


class FW:
    def __init__(self, nc, es):
        self.nc = nc
        self.es = es
        self.engs = {"pe": nc.tensor, "dve": nc.vector, "act": nc.scalar, "pool": nc.gpsimd, "sp": nc.sync}
        self.sem = {}
        self.cnt = {}
        for k in self.engs:
            self.sem[k] = es.enter_context(nc.semaphore("sem_" + k))
            self.cnt[k] = 0
        self.known = {k: {} for k in self.engs}
        self.dsem = {}
        self.dcnt = {}
        self.nwaits = 0
        self.ninstr = 0

    def sb(self, name, shape, dt):
        return self.es.enter_context(self.nc.sbuf_tensor(name, list(shape), dt))

    def ps(self, name, shape, dt=F32):
        return self.es.enter_context(self.nc.psum_tensor(name, list(shape), dt))

    def dma_sem(self, name):
        if name not in self.dsem:
            self.dsem[name] = self.es.enter_context(self.nc.semaphore("dq_" + name))
            self.dcnt[name] = 0
        return name

    def _semobj(self, key):
        return self.sem[key] if key in self.sem else self.dsem[key]

    def _deps(self, eng, reads, writes):
        deps = {}
        def add(d):
            if d is None:
                return
            k, v = d
            if deps.get(k, 0) < v:
                deps[k] = v
        for r in reads:
            add(r.lw)
            if r.psum:
                for k, v in r.rd.items():
                    if k != eng:
                        add((k, v))
        for w in writes:
            add(w.lw)
            for k, v in w.rd.items():
                add((k, v))
        for k, v in deps.items():
            if k == eng and eng == "pe":
                continue
            if k in self.dsem:
                v = self.dcnt[k]
            if self.known[eng].get(k, 0) >= v:
                continue
            self.engs[eng].wait_ge(self._semobj(k), v)
            self.known[eng][k] = v
            self.nwaits += 1

    def _commit(self, key, val, reads, writes):
        for w in writes:
            w.lw = (key, val)
            w.rd = {}
        for r in reads:
            if r in writes:
                continue
            if r.rd.get(key, 0) < val:
                r.rd[key] = val

    def op(self, eng, fn, reads=(), writes=()):
        self._deps(eng, reads, writes)
        ins = fn()
        if isinstance(ins, (list, tuple)):
            ins = ins[-1]
        ins.then_inc(self.sem[eng], 1)
        self.cnt[eng] += 1
        self.ninstr += 1
        self._commit(eng, self.cnt[eng], reads, writes)
        return ins

    def dma(self, q, out, in_, slot, reads=(), writes=(), **kw):
        self.dma_sem(slot)
        self._deps(q, reads, writes)
        ins = self.engs[q].dma_start(out=out, in_=in_, **kw)
        ins.then_inc(self.dsem[slot], 16)
        self.dcnt[slot] += 16
        self._commit(slot, self.dcnt[slot], reads, writes)
        return ins

    def wait_all(self, eng, ress):
        self._deps(eng, ress, [])


NEG = -30000.0


class GTL:
    def __init__(self, t, name):
        self.t = t
        self.r = Res(name)


def build_gdn(nc, T, stop_at=99):
    NTILE = T // 512
    xT = nc.dram_tensor("xT", [1024, T], F32, kind="ExternalInput").ap()
    wqkv = nc.dram_tensor("wqkv", [1024, 512], F32, kind="ExternalInput").ap()
    wzba = nc.dram_tensor("wzba", [1024, 260], F32, kind="ExternalInput").ap()
    convw = nc.dram_tensor("convw", [512, 4], F32, kind="ExternalInput").ap()
    hp = nc.dram_tensor("hp", [1, 4], F32, kind="ExternalInput").ap()
    normw = nc.dram_tensor("normw", [1, 128], F32, kind="ExternalInput").ap()
    og = nc.dram_tensor("og", [T, 256], F32, kind="ExternalOutput").ap()

    with ExitStack() as es:
        fw = FW(nc, es)
        V, A, G, PE = nc.vector, nc.scalar, nc.gpsimd, nc.tensor

        def sb(name, shape, dt=F32):
            return GTL(fw.sb(name, shape, dt), name)

        identb = sb("identb", [128, 128], BF16)
        identf = sb("identf", [128, 128])
        U = sb("U", [128, 128])
        Uneg = sb("Uneg", [128, 128])
        onesf = sb("onesf", [128, 128])
        onesb = sb("onesb", [128, 128], BF16)
        MASKS = sb("MASKS", [128, 128])
        BDm = sb("BDm", [128, 128])
        OFFm = sb("OFFm", [128, 128])

        def pool(fn, reads=(), writes=()):
            return fw.op("pool", fn, [x.r for x in reads], [x.r for x in writes])

        def dve(fn, reads=(), writes=()):
            return fw.op("dve", fn, [x.r for x in reads], [x.r for x in writes])

        def act(fn, reads=(), writes=()):
            return fw.op("act", fn, [x.r for x in reads], [x.r for x in writes])

        def pe(fn, reads=(), writes=()):
            return fw.op("pe", fn, [x.r for x in reads], [x.r for x in writes])

        def mm(out_ap, pairs, reads, writes):
            def f():
                n = len(pairs)
                last = None
                for i, (l, r) in enumerate(pairs):
                    last = PE.matmul(out_ap, lhsT=l, rhs=r, start=(i == 0), stop=(i == n - 1))
                return last
            return pe(f, reads, writes)

        for t_, val in ((identb, 0.0), (identf, 0.0), (U, 1.0), (Uneg, -1.0), (onesf, 1.0), (onesb, 1.0),
                        (MASKS, 0.0), (BDm, 0.0), (OFFm, 0.0)):
            pool(lambda t_=t_, val=val: G.memset(t_.t[:], val), writes=[t_])
        for t_ in (identb, identf):
            pool(lambda t_=t_: G.affine_select(out=t_.t[:], in_=t_.t[:], pattern=[[-1, 128]], compare_op=ALU.not_equal,
                                               fill=1.0, base=0, channel_multiplier=1), reads=[t_], writes=[t_])
        for t_ in (U, Uneg):
            pool(lambda t_=t_: G.affine_select(out=t_.t[:], in_=t_.t[:], pattern=[[1, 128]], compare_op=ALU.is_ge,
                                               fill=0.0, base=0, channel_multiplier=-1), reads=[t_], writes=[t_])
        pool(lambda: G.affine_select(out=MASKS.t[:], in_=MASKS.t[:], pattern=[[-1, 128]], compare_op=ALU.is_gt,
                                     fill=NEG, base=0, channel_multiplier=1), reads=[MASKS], writes=[MASKS])
        pool(lambda: G.memset(BDm.t[0:64, 0:64], 1.0), writes=[BDm])
        pool(lambda: G.memset(BDm.t[64:128, 64:128], 1.0), writes=[BDm])
        pool(lambda: G.memset(OFFm.t[64:128, 0:64], 1.0), writes=[OFFm])

        wqkv_b = sb("wqkv_b", [128, 8, 512], BF16)
        wzba_b = sb("wzba_b", [128, 8, 320], BF16)
        convw_s = sb("convw_s", [128, 4, 4])
        hp_s = sb("hp_s", [128, 4])
        normw_s = sb("normw_s", [128, 128])
        fw.dma("pool", wqkv_b.t[:], wqkv.rearrange("(c p) n -> p c n", p=128), "w0", writes=[wqkv_b.r])
        fw.dma("pool", wzba_b.t[:, :, 0:260], wzba.rearrange("(c p) n -> p c n", p=128), "w1", writes=[wzba_b.r])
        fw.dma("sp", convw_s.t[:], convw.rearrange("(c p) j -> p c j", p=128), "w2", writes=[convw_s.r])
        fw.dma("sp", hp_s.t[:], hp[0:1, :].partition_broadcast(128), "w2", writes=[hp_s.r])
        fw.dma("sp", normw_s.t[:], normw[0:1, :].partition_broadcast(128), "w2", writes=[normw_s.r])
        negA8 = sb("negA8", [128, 4, 2])
        dtb8 = sb("dtb8", [128, 4, 2])
        ea = sb("ea", [128, 2])
        act(lambda: A.activation(out=ea.t[:], in_=hp_s.t[:, 0:2], func=AF.Exp), reads=[hp_s], writes=[ea])
        for c in range(4):
            dve(lambda c=c: V.tensor_scalar(out=negA8.t[:, c, :], in0=ea.t[:], scalar1=-1.0, scalar2=None, op0=ALU.mult),
                reads=[ea], writes=[negA8])
            dve(lambda c=c: V.tensor_copy(out=dtb8.t[:, c, :], in_=hp_s.t[:, 2:4]), reads=[hp_s], writes=[dtb8])

        S = [sb(f"S{h}", [128, 128]) for h in range(2)]
        Sb = [sb(f"Sb{h}", [128, 128], BF16) for h in range(2)]
        for h in range(2):
            pool(lambda h=h: G.memset(S[h].t[:], 0.0), writes=[S[h]])
            pool(lambda h=h: G.memset(Sb[h].t[:], 0.0), writes=[Sb[h]])

        xb = [sb(f"xb{i}", [128, 8, 512], BF16) for i in range(2)]
        pre = [[sb(f"pre{i}_{ch}", [128, 515]) for ch in range(4)] for i in range(2)]
        for ch in range(4):
            pool(lambda ch=ch: G.memset(pre[0][ch].t[:, 0:3], 0.0), writes=[pre[0][ch]])
        cacc = [sb(f"cacc{ch}", [128, 512]) for ch in range(4)]
        qs = [sb(f"qs{i}", [128, 512]) for i in range(2)]
        sq = [sb(f"sq{i}", [128, 512], BF16) for i in range(2)]
        lnr = [sb(f"lnr{i}", [128, 512]) for i in range(2)]
        rr = [sb(f"rr{i}", [128, 512]) for i in range(2)]
        qnT = sb("qnT", [128, 512], BF16)
        knT = sb("knT", [128, 512], BF16)
        vT = [sb(f"vT{h}", [128, 512], BF16) for h in range(2)]
        zs = sb("zs", [128, 4, 256])
        nwz = sb("nwz", [128, 4, 2, 128])
        ba = sb("ba", [128, 4, 4])
        eb = sb("eb", [128, 4, 2])
        beta = sb("beta", [128, 4, 2])
        nbeta = sb("nbeta", [128, 4, 2])
        t1 = sb("t1", [128, 4, 2])
        e1 = sb("e1", [128, 4, 2])
        l1 = sb("l1", [128, 4, 2])
        gt = sb("gt", [128, 4, 2])
        gs = sb("gs", [128, 4])
        exa = sb("exa", [128, 4])
        dk = sb("dk", [128, 2])
        ekd = sb("ekd", [128, 2])
        bg = sb("bg", [128, 2])
        vb = [sb(f"vb{h}", [128, 128], BF16) for h in range(2)]
        kbg = [sb(f"kbg{h}", [128, 128], BF16) for h in range(2)]
        kd = [sb(f"kd{h}", [128, 128], BF16) for h in range(2)]
        gB = [sb(f"gB{h}", [128, 128]) for h in range(2)]
        dec = sb("dec", [128, 512])
        NA = [sb(f"NA{h}", [128, 128]) for h in range(2)]
        MoffT = [sb(f"MoffT{h}", [128, 128]) for h in range(2)]
        decc = [sb(f"decc{h}", [128, 128]) for h in range(2)]
        aqk = [sb(f"aqk{h}", [128, 128], BF16) for h in range(2)]
        aqkT = [sb(f"aqkT{h}", [128, 128], BF16) for h in range(2)]
        qdT = [sb(f"qdT{h}", [128, 128], BF16) for h in range(2)]
        PP = [[sb(f"PP{h}_{i}", [128, 384]) for i in range(2)] for h in range(2)]
        Tt = [[sb(f"Tt{h}_{i}", [128, 128]) for i in range(2)] for h in range(2)]
        TdY = [sb(f"TdY{h}", [128, 256]) for h in range(2)]
        TTb = [sb(f"TTb{h}", [128, 128], BF16) for h in range(2)]
        u_s = sb("u_s", [128, 256])
        wT_s = sb("wT_s", [128, 256], BF16)
        vn = [sb(f"vn{h}", [128, 128], BF16) for h in range(2)]
        junk = [sb(f"junk{h}", [128, 128]) for h in range(2)]
        ss = [sb(f"ss{h}", [128, 1]) for h in range(2)]
        lns = [sb(f"lns{h}", [128, 1]) for h in range(2)]
        rinv = [sb(f"rinv{h}", [128, 1]) for h in range(2)]
        ogt = [sb(f"ogt{i}", [128, 256]) for i in range(2)]

        def pst(name, shape, dt=F32):
            return GTL(fw.ps(name, shape, dt), name)
        b0 = fw.ps("b0", [128, 512]); b1 = fw.ps("b1", [128, 512]); b2 = fw.ps("b2", [128, 512])
        b3 = fw.ps("b3", [128, 1024], BF16)
        b4 = fw.ps("b4", [128, 512]); b5 = fw.ps("b5", [128, 512]); b6 = fw.ps("b6", [128, 512]); b7 = fw.ps("b7", [128, 512])

        RB = [Res(f"bank{i}", psum=True) for i in range(8)]
        banks = [b0, b1, b2, b3, b4, b5, b6, b7]

        class PT:
            def __init__(self, bi, lo, hi, name):
                self.bank = banks[bi]; self.lo = lo; self.hi = hi; self.r = RB[bi]
            @property
            def ap(self):
                return self.bank[:, self.lo:self.hi]
        paA = [PT(0, 0, 512, "paA0"), PT(1, 0, 512, "paA1")]
        pzt = [PT(6, 0, 260, "pzt0"), PT(7, 0, 260, "pzt1")]
        pD = PT(2, 0, 512, "pD")
        pT_k = PT(3, 0, 128, "pT_k")
        pT_v = [PT(3, 128, 256, "pT_v0"), PT(3, 256, 384, "pT_v1")]
        pT_a = [PT(3, 384, 512, "pT_a0"), PT(3, 512, 640, "pT_a1")]
        pKK = PT(6, 0, 128, "pKK"); pQK = PT(6, 128, 256, "pQK"); pgcc = PT(6, 256, 260, "pgcc")
        pi_sq = [PT(4, 0, 256, "pisq0"), PT(5, 0, 256, "pisq1")]
        pi_pr = [PT(4, 256, 384, "pipr0"), PT(5, 256, 384, "pipr1")]
        pu = PT(2, 0, 256, "pu"); pwT = PT(2, 256, 512, "pwT")
        p_wS = [PT(0, 0, 128, "pwS0"), PT(1, 0, 128, "pwS1")]
        p_Sn = [PT(0, 128, 256, "pSn0"), PT(1, 128, 256, "pSn1")]
        p_o = [PT(7, 0, 128, "po0"), PT(7, 128, 256, "po1")]

        xTr = xT.rearrange("(c p) t -> p c t", p=128)

        def load_x(n):
            s = n % 2
            fw.dma("pool", xb[s].t[:], xTr[:, :, n * 512:(n + 1) * 512], f"x{s}", writes=[xb[s].r])

        def body():
            load_x(0)
            for n in range(NTILE):
                s = n % 2
                if n + 1 < NTILE:
                    load_x(n + 1)
                X = xb[s]
                if stop_at == 0: return
                for ch in range(4):
                    pa = paA[ch % 2]
                    mm(pa.ap, [(wqkv_b.t[:, kc, ch * 128:(ch + 1) * 128], X.t[:, kc, :]) for kc in range(8)],
                       [wqkv_b, X], [pa])
                    P_ = pre[s][ch]
                    act(lambda pa=pa, P_=P_: A.copy(out=P_.t[:, 3:515], in_=pa.ap), reads=[pa], writes=[P_])
                    if n + 1 < NTILE:
                        Pn = pre[1 - s][ch]
                        pool(lambda P_=P_, Pn=Pn: G.tensor_copy(out=Pn.t[:, 0:3], in_=P_.t[:, 512:515]), reads=[P_], writes=[Pn])
                if stop_at == 1: return
                for j in (3, 2, 1, 0):
                    for ch in range(4):
                        P_ = pre[s][ch]; C_ = cacc[ch]
                        if j == 3:
                            dve(lambda P_=P_, C_=C_, ch=ch, j=j: V.tensor_scalar(out=C_.t[:], in0=P_.t[:, j:j + 512],
                                scalar1=convw_s.t[:, ch, j:j + 1], scalar2=None, op0=ALU.mult), reads=[P_, convw_s], writes=[C_])
                        else:
                            dve(lambda P_=P_, C_=C_, ch=ch, j=j: V.scalar_tensor_tensor(out=C_.t[:], in0=P_.t[:, j:j + 512],
                                scalar=convw_s.t[:, ch, j:j + 1], in1=C_.t[:], op0=ALU.mult, op1=ALU.add),
                                reads=[P_, convw_s, C_], writes=[C_])
                for i in range(2):
                    act(lambda i=i: A.activation(out=qs[i].t[:], in_=cacc[i].t[:], func=AF.Silu), reads=[cacc[i]], writes=[qs[i]])
                for h in range(2):
                    act(lambda h=h: A.activation(out=vT[h].t[:], in_=cacc[2 + h].t[:], func=AF.Silu), reads=[cacc[2 + h]], writes=[vT[h]])
                if stop_at == 2: return
                for c in range(4):
                    pz = pzt[c % 2]
                    mm(pz.ap, [(X.t[:, kc, c * 128:(c + 1) * 128], wzba_b.t[:, kc, 0:260]) for kc in range(8)], [wzba_b, X], [pz])
                    act(lambda pz=pz, c=c: A.activation(out=zs.t[:, c, :], in_=pz.bank[:, 0:256], func=AF.Silu), reads=[pz], writes=[zs])
                    act(lambda pz=pz, c=c: A.copy(out=ba.t[:, c, :], in_=pz.bank[:, 256:260]), reads=[pz], writes=[ba])
                if stop_at == 25: return
                for c in range(4):
                    for h in range(2):
                        pool(lambda c=c, h=h: G.tensor_tensor(out=nwz.t[:, c, h, :], in0=zs.t[:, c, h * 128:(h + 1) * 128],
                                                              in1=normw_s.t[:], op=ALU.mult), reads=[zs, normw_s], writes=[nwz])
                if stop_at == 3: return
                for i in range(2):
                    act(lambda i=i: A.activation(out=sq[i].t[:], in_=qs[i].t[:], func=AF.Square), reads=[qs[i]], writes=[sq[i]])
                for i in range(2):
                    pa = paA[i]
                    mm(pa.ap, [(onesb.t[:], sq[i].t[:])], [onesb, sq[i]], [pa])
                    act(lambda i=i, pa=pa: A.activation(out=lnr[i].t[:], in_=pa.ap, func=AF.Ln, bias=1e-6), reads=[pa], writes=[lnr[i]])
                for i in range(2):
                    act(lambda i=i: A.activation(out=rr[i].t[:], in_=lnr[i].t[:], func=AF.Exp, scale=-0.5), reads=[lnr[i]], writes=[rr[i]])
                dve(lambda: V.scalar_tensor_tensor(out=qnT.t[:], in0=qs[0].t[:], scalar=float(128 ** -0.5), in1=rr[0].t[:],
                                                   op0=ALU.mult, op1=ALU.mult), reads=[qs[0], rr[0]], writes=[qnT])
                dve(lambda: V.tensor_tensor(out=knT.t[:], in0=qs[1].t[:], in1=rr[1].t[:], op=ALU.mult), reads=[qs[1], rr[1]], writes=[knT])
                if stop_at == 4: return
                act(lambda: A.activation(out=eb.t[:], in_=ba.t[:, :, 0:2], func=AF.Exp, scale=-1.0), reads=[ba], writes=[eb])
                dve(lambda: V.tensor_scalar(out=eb.t[:], in0=eb.t[:], scalar1=1.0, scalar2=None, op0=ALU.add), reads=[eb], writes=[eb])
                dve(lambda: V.reciprocal(out=beta.t[:], in_=eb.t[:]), reads=[eb], writes=[beta])
                dve(lambda: V.tensor_scalar(out=nbeta.t[:], in0=beta.t[:], scalar1=-1.0, scalar2=None, op0=ALU.mult), reads=[beta], writes=[nbeta])
                dve(lambda: V.tensor_tensor(out=t1.t[:], in0=ba.t[:, :, 2:4], in1=dtb8.t[:], op=ALU.add), reads=[ba, dtb8], writes=[t1])
                act(lambda: A.activation(out=e1.t[:], in_=t1.t[:], func=AF.Exp), reads=[t1], writes=[e1])
                act(lambda: A.activation(out=l1.t[:], in_=e1.t[:], func=AF.Ln, bias=1.0), reads=[e1], writes=[l1])
                dve(lambda: V.tensor_tensor(out=gt.t[:], in0=l1.t[:], in1=negA8.t[:], op=ALU.mult), reads=[l1, negA8], writes=[gt])

                if stop_at == 5: return
                for c in range(4):
                    cs = slice(c * 128, (c + 1) * 128)
                    pe(lambda: PE.transpose(pT_k.ap, knT.t[:, cs], identb.t[:]), reads=[knT, identb], writes=[pT_k])
                    for h in range(2):
                        pe(lambda h=h: PE.transpose(pT_v[h].ap, vT[h].t[:, cs], identb.t[:]), reads=[vT[h], identb], writes=[pT_v[h]])
                    mm(b6[:, 256:258], [(U.t[:], gt.t[:, c, :])], [U, gt], [pgcc])
                    mm(b6[:, 258:260], [(onesf.t[:], gt.t[:, c, :])], [onesf, gt], [pgcc])
                    mm(pKK.ap, [(knT.t[:, cs], knT.t[:, cs])], [knT], [pKK])
                    mm(pQK.ap, [(qnT.t[:, cs], knT.t[:, cs])], [qnT, knT], [pQK])
                    act(lambda: A.copy(out=gs.t[:], in_=pgcc.ap), reads=[pgcc], writes=[gs])
                    act(lambda: A.activation(out=exa.t[:], in_=gs.t[:], func=AF.Exp), reads=[gs], writes=[exa])
                    for h in range(2):
                        act(lambda h=h: A.activation(out=ekd.t[:, h:h + 1], in_=gs.t[:, h:h + 1], func=AF.Exp, scale=-1.0,
                                                     bias=gs.t[:, 2 + h:3 + h]), reads=[gs], writes=[ekd])
                    for h in range(2):
                        dve(lambda h=h: V.tensor_scalar(out=vb[h].t[:], in0=pT_v[h].ap, scalar1=beta.t[:, c, h:h + 1], scalar2=None,
                                                        op0=ALU.mult), reads=[pT_v[h], beta], writes=[vb[h]])
                        dve(lambda h=h: V.tensor_scalar(out=kbg[h].t[:], in0=pT_k.ap, scalar1=beta.t[:, c, h:h + 1], scalar2=exa.t[:, h:h + 1],
                                                        op0=ALU.mult, op1=ALU.mult), reads=[pT_k, beta, exa], writes=[kbg[h]])
                        dve(lambda h=h: V.tensor_scalar(out=kd[h].t[:], in0=pT_k.ap, scalar1=ekd.t[:, h:h + 1], scalar2=None, op0=ALU.mult),
                            reads=[pT_k, ekd], writes=[kd[h]])
                        pool(lambda h=h: G.tensor_scalar(out=gB[h].t[:], in0=onesf.t[:], scalar1=gt.t[:, c, h:h + 1], scalar2=None,
                                                         op0=ALU.mult), reads=[onesf, gt], writes=[gB[h]])
                    if stop_at == 6: return
                    for h in range(2):
                        mm(b2[:, h * 128:(h + 1) * 128], [(U.t[:], gB[h].t[:]), (gB[h].t[:], Uneg.t[:]), (identf.t[:], MASKS.t[:])],
                           [U, Uneg, gB[h], identf, MASKS], [pD])
                        mm(b2[:, 256 + h * 128:256 + (h + 1) * 128], [(gB[h].t[:], U.t[:])], [gB[h], U], [pD])
                    act(lambda: A.activation(out=dec.t[:], in_=pD.ap, func=AF.Exp), reads=[pD], writes=[dec])
                    for h in range(2):
                        hs = slice(h * 128, (h + 1) * 128)
                        dve(lambda h=h, hs=hs: V.scalar_tensor_tensor(out=NA[h].t[:], in0=pKK.ap, scalar=nbeta.t[:, c, h:h + 1],
                            in1=dec.t[:, hs], op0=ALU.mult, op1=ALU.mult), reads=[pKK, nbeta, dec], writes=[NA[h]])
                        pool(lambda h=h: G.tensor_tensor(out=PP[h][0].t[:, 128:256], in0=NA[h].t[:], in1=BDm.t[:], op=ALU.mult),
                             reads=[NA[h], BDm], writes=[PP[h][0]])
                        pool(lambda h=h: G.tensor_tensor(out=MoffT[h].t[:], in0=NA[h].t[:], in1=OFFm.t[:], op=ALU.mult),
                             reads=[NA[h], OFFm], writes=[MoffT[h]])
                        pool(lambda h=h, hs=hs: G.tensor_tensor(out=decc[h].t[:], in0=dec.t[:, hs], in1=identf.t[:], op=ALU.add),
                             reads=[dec, identf], writes=[decc[h]])
                        dve(lambda h=h: V.tensor_tensor(out=aqk[h].t[:], in0=pQK.ap, in1=decc[h].t[:], op=ALU.mult),
                            reads=[pQK, decc[h]], writes=[aqk[h]])
                        pool(lambda h=h: G.tensor_tensor(out=qdT[h].t[:], in0=qnT.t[:, cs], in1=dec.t[:, 256 + h * 128:256 + (h + 1) * 128],
                                                         op=ALU.mult), reads=[qnT, dec], writes=[qdT[h]])
                    for h in range(2):
                        pe(lambda h=h: PE.transpose(pT_a[h].ap, aqk[h].t[:], identb.t[:]), reads=[aqk[h], identb], writes=[pT_a[h]])
                    for h in range(2):
                        act(lambda h=h: A.copy(out=aqkT[h].t[:], in_=pT_a[h].ap), reads=[pT_a[h]], writes=[aqkT[h]])
                    if stop_at == 7: return
                    def ev(h, dst_tile, dst_ap, src_pt, src_ap):
                        if h == 0:
                            act(lambda: A.copy(out=dst_ap, in_=src_ap), reads=[src_pt], writes=[dst_tile])
                        else:
                            dve(lambda: V.tensor_copy(out=dst_ap, in_=src_ap), reads=[src_pt], writes=[dst_tile])
                    for h in range(2):
                        X0 = PP[h][0]
                        bk = pi_sq[h].bank
                        pe(lambda: PE.matmul(bk[:, 0:128], lhsT=X0.t[:, 128:256], rhs=identf.t[:], start=True, stop=True),
                           reads=[X0, identf], writes=[pi_sq[h]])
                        mm(bk[:, 256:384], [(X0.t[:, 128:256], identf.t[:]), (identf.t[:], identf.t[:])], [X0, identf], [pi_sq[h]])
                    for h in range(2):
                        X0 = PP[h][0]
                        bk = pi_sq[h].bank
                        ev(h, X0, X0.t[:, 0:128], pi_sq[h], bk[:, 0:128])
                        ev(h, X0, X0.t[:, 256:384], pi_sq[h], bk[:, 256:384])
                    if stop_at == 75: return
                    for k in range(1, 7):
                        for h in range(2):
                            Ps = PP[h][(k - 1) % 2]
                            bk = pi_sq[h].bank
                            if k <= 4:
                                pe(lambda: PE.matmul(bk[:, 0:128], lhsT=Ps.t[:, 128:256], rhs=Ps.t[:, 0:128], start=True, stop=True),
                                   reads=[Ps], writes=[pi_sq[h]])
                            if k <= 5:
                                pe(lambda: PE.matmul(bk[:, 128:256], lhsT=Ps.t[:, 0:128], rhs=Ps.t[:, 128:256], start=True, stop=True),
                                   reads=[Ps], writes=[pi_sq[h]])
                            if k >= 2:
                                mm(bk[:, 256:384], [(Ps.t[:, 128:256], Ps.t[:, 256:384]), (identf.t[:], Ps.t[:, 256:384])], [Ps, identf], [pi_sq[h]])
                            else:
                                mm(bk[:, 256:384], [(identf.t[:], Ps.t[:, 256:384])], [Ps, identf], [pi_sq[h]])
                        for h in range(2):
                            Pd = PP[h][k % 2]
                            bk = pi_sq[h].bank
                            lo = 0 if k <= 4 else (128 if k == 5 else 256)
                            ev(h, Pd, Pd.t[:, lo:384], pi_sq[h], bk[:, lo:384])
                    if stop_at == 8: return
                    for h in range(2):
                        Pf = PP[h][0]
                        bk = pi_sq[h].bank
                        pe(lambda: PE.matmul(bk[:, 0:128], lhsT=Pf.t[:, 256:384], rhs=identf.t[:], start=True, stop=True),
                           reads=[Pf, identf], writes=[pi_sq[h]])
                        pe(lambda: PE.matmul(bk[:, 128:256], lhsT=MoffT[h].t[:], rhs=Pf.t[:, 256:384], start=True, stop=True),
                           reads=[MoffT[h], Pf], writes=[pi_sq[h]])
                    for h in range(2):
                        ev(h, TdY[h], TdY[h].t[:], pi_sq[h], pi_sq[h].bank[:, 0:256])
                    for h in range(2):
                        Pf = PP[h][0]
                        bk = pi_sq[h].bank
                        mm(bk[:, 256:384], [(TdY[h].t[:, 0:128], TdY[h].t[:, 128:256]), (identf.t[:], Pf.t[:, 256:384])],
                           [TdY[h], Pf, identf], [pi_sq[h]])
                    for h in range(2):
                        ev(h, TTb[h], TTb[h].t[:], pi_sq[h], pi_sq[h].bank[:, 256:384])
                    if stop_at == 9: return
                    for h in range(2):
                        hs = slice(h * 128, (h + 1) * 128)
                        pe(lambda h=h, hs=hs: PE.matmul(b2[:, hs], lhsT=TTb[h].t[:], rhs=vb[h].t[:], start=True, stop=True),
                           reads=[TTb[h], vb[h]], writes=[pu])
                    for h in range(2):
                        pe(lambda h=h: PE.matmul(b2[:, 256 + h * 128:256 + (h + 1) * 128], lhsT=kbg[h].t[:], rhs=TTb[h].t[:], start=True, stop=True),
                           reads=[TTb[h], kbg[h]], writes=[pwT])
                    act(lambda: A.copy(out=u_s.t[:], in_=pu.ap), reads=[pu], writes=[u_s])
                    act(lambda: A.copy(out=wT_s.t[:], in_=pwT.ap), reads=[pwT], writes=[wT_s])
                    if stop_at == 10: return
                    og_t = ogt[c % 2]
                    for h in range(2):
                        hs = slice(h * 128, (h + 1) * 128)
                        pe(lambda h=h, hs=hs: PE.matmul(p_wS[h].ap, lhsT=wT_s.t[:, hs], rhs=Sb[h].t[:], start=True, stop=True),
                           reads=[wT_s, Sb[h]], writes=[p_wS[h]])
                        dve(lambda h=h, hs=hs: V.tensor_tensor(out=vn[h].t[:], in0=u_s.t[:, hs], in1=p_wS[h].ap, op=ALU.subtract),
                            reads=[u_s, p_wS[h]], writes=[vn[h]])
                    for h in range(2):
                        mm(p_o[h].ap, [(qdT[h].t[:], Sb[h].t[:]), (aqkT[h].t[:], vn[h].t[:])], [qdT[h], Sb[h], aqkT[h], vn[h]], [p_o[h]])
                        mm(p_Sn[h].ap, [(kd[h].t[:], vn[h].t[:])], [kd[h], vn[h]], [p_Sn[h]])
                    for h in range(2):
                        dve(lambda h=h: V.scalar_tensor_tensor(out=S[h].t[:], in0=S[h].t[:], scalar=exa.t[:, 2 + h:3 + h], in1=p_Sn[h].ap,
                                                               op0=ALU.mult, op1=ALU.add), reads=[S[h], exa, p_Sn[h]], writes=[S[h]])
                        act(lambda h=h: A.copy(out=Sb[h].t[:], in_=S[h].t[:]), reads=[S[h]], writes=[Sb[h]])
                    for h in range(2):
                        act(lambda h=h: A.activation(out=junk[h].t[:], in_=p_o[h].ap, func=AF.Square, accum_out=ss[h].t[:, 0:1]),
                            reads=[p_o[h]], writes=[junk[h], ss[h]])
                    for h in range(2):
                        act(lambda h=h: A.activation(out=lns[h].t[:], in_=ss[h].t[:], func=AF.Ln, scale=1.0 / 128.0, bias=1e-6),
                            reads=[ss[h]], writes=[lns[h]])
                    for h in range(2):
                        act(lambda h=h: A.activation(out=rinv[h].t[:], in_=lns[h].t[:], func=AF.Exp, scale=-0.5), reads=[lns[h]], writes=[rinv[h]])
                    for h in range(2):
                        dve(lambda h=h, og_t=og_t: V.scalar_tensor_tensor(out=og_t.t[:, h * 128:(h + 1) * 128], in0=p_o[h].ap,
                            scalar=rinv[h].t[:, 0:1], in1=nwz.t[:, c, h, :], op0=ALU.mult, op1=ALU.mult),
                            reads=[p_o[h], rinv[h], nwz], writes=[og_t])
                    r0 = n * 512 + c * 128
                    fw.dma("sp", og[r0:r0 + 128, :], og_t.t[:], f"og{c % 2}", reads=[og_t.r])

        body()
        for k in ("og0", "og1"):
            if k in fw.dsem:
                nc.sync.wait_ge(fw.dsem[k], fw.dcnt[k])
        print("GDN instrs", fw.ninstr, "waits", fw.nwaits)
    return nc


ALPHA = float((2.0 * 2) ** 0.25)
MOE_GCH = 4
LN_EPS = 1e-5


class TL:
    def __init__(self, t, name, psum=False):
        self.t = t
        self.r = Res(name, psum=psum)


class KC:
    def __init__(self, nc, es):
        self.nc = nc
        self.es = es
        self.fw = FW(nc, es)
        self.V, self.A, self.G, self.PE = nc.vector, nc.scalar, nc.gpsimd, nc.tensor
        self.banks = []
        self._uid = 0

    def sb(self, name, shape, dt=F32):
        return TL(self.fw.sb(name, shape, dt), name)

    def bank(self, name, dt=F32):
        cols = 512 if dt == F32 else 1024
        return TL(self.fw.ps(name, [128, cols], dt), name, psum=True)

    def op(self, eng, fn, reads=(), writes=()):
        return self.fw.op(eng, fn, [x.r for x in reads], [x.r for x in writes])

    def dve(self, fn, reads=(), writes=()):
        return self.op("dve", fn, reads, writes)

    def act(self, fn, reads=(), writes=()):
        return self.op("act", fn, reads, writes)

    def pool(self, fn, reads=(), writes=()):
        return self.op("pool", fn, reads, writes)

    def pe(self, fn, reads=(), writes=()):
        return self.op("pe", fn, reads, writes)

    def mm(self, out_ap, pairs, reads, writes):
        PE = self.PE
        def f():
            n = len(pairs)
            last = None
            for i, (l, r) in enumerate(pairs):
                last = PE.matmul(out_ap, lhsT=l, rhs=r, start=(i == 0), stop=(i == n - 1))
            return last
        return self.pe(f, reads, writes)

    def dma(self, q, out, in_, slot, reads=(), writes=(), **kw):
        return self.fw.dma(q, out, in_, slot, [x.r for x in reads], [x.r for x in writes], **kw)

    def barrier(self):
        fw = self.fw
        for e in fw.engs:
            for k in list(fw.sem) + list(fw.dsem):
                v = fw.cnt[k] if k in fw.sem else fw.dcnt[k]
                if k == e or v == 0 or fw.known[e].get(k, 0) >= v:
                    continue
                fw.engs[e].wait_ge(fw._semobj(k), v)
                fw.known[e][k] = v

    def finish(self, slots):
        for k in slots:
            if k in self.fw.dsem:
                self.nc.sync.wait_ge(self.fw.dsem[k], self.fw.dcnt[k])

    def consts(self):
        G = self.G
        self.onesf = self.sb("onesf", [128, 128])
        self.identf = self.sb("identf", [128, 128])
        self.identb = self.sb("identb", [128, 128], BF16)
        for t_, v in ((self.onesf, 1.0), (self.identf, 0.0), (self.identb, 0.0)):
            self.pool(lambda: G.memset(t_.t[:], v), writes=[t_])
        for t_ in (self.identf, self.identb):
            self.pool(lambda: G.affine_select(out=t_.t[:], in_=t_.t[:], pattern=[[-1, 128]], compare_op=ALU.not_equal,
                                              fill=1.0, base=0, channel_multiplier=1), reads=[t_], writes=[t_])

    def ln_alloc(self):
        self.ln_sq = [self.sb(f"ln_sq{i}", [128, 512]) for i in range(2)]
        self.ln_mean = self.sb("ln_mean", [128, 512])
        self.ln_msq = self.sb("ln_msq", [128, 512])
        self.ln_var = self.sb("ln_var", [128, 512])
        self.ln_lnv = self.sb("ln_lnv", [128, 512])
        self.ln_rstd = self.sb("ln_rstd", [128, 512])
        self.ln_t = [self.sb(f"ln_t{i}", [128, 512]) for i in range(2)]

    def layernorm(self, y, ycols, gb, gi, bi, ps1, ps2, out_f=None, out_f_cols=None, out_b=None, out_b_cols=None, N=512):
        V, A, G, PE = self.V, self.A, self.G, self.PE
        onesf = self.onesf
        self.mm(ps1.t[:, 0:N], [(onesf.t[:], y.t[:, c, ycols]) for c in range(8)], [onesf, y], [ps1])
        for c in range(8):
            sq = self.ln_sq[c % 2]
            self.act(lambda: A.activation(out=sq.t[:, 0:N], in_=y.t[:, c, ycols], func=AF.Square), reads=[y], writes=[sq])
            self.pe(lambda: PE.matmul(ps2.t[:, 0:N], lhsT=onesf.t[:], rhs=sq.t[:, 0:N], start=(c == 0), stop=(c == 7)),
                    reads=[onesf, sq], writes=[ps2])
        mean, msq, var, lnv, rstd = self.ln_mean, self.ln_msq, self.ln_var, self.ln_lnv, self.ln_rstd
        self.act(lambda: A.activation(out=mean.t[:, 0:N], in_=ps1.t[:, 0:N], func=AF.Copy, scale=1.0 / 1024.0), reads=[ps1], writes=[mean])
        self.act(lambda: A.activation(out=msq.t[:, 0:N], in_=ps1.t[:, 0:N], func=AF.Square, scale=1.0 / 1024.0), reads=[ps1], writes=[msq])
        self.dve(lambda: V.scalar_tensor_tensor(out=var.t[:, 0:N], in0=ps2.t[:, 0:N], scalar=1.0 / 1024.0, in1=msq.t[:, 0:N],
                                                op0=ALU.mult, op1=ALU.subtract), reads=[ps2, msq], writes=[var])
        self.act(lambda: A.activation(out=lnv.t[:, 0:N], in_=var.t[:, 0:N], func=AF.Ln, bias=LN_EPS), reads=[var], writes=[lnv])
        self.act(lambda: A.activation(out=rstd.t[:, 0:N], in_=lnv.t[:, 0:N], func=AF.Exp, scale=-0.5), reads=[lnv], writes=[rstd])
        for c in range(8):
            t = self.ln_t[c % 2]
            self.dve(lambda: V.tensor_tensor(out=t.t[:, 0:N], in0=y.t[:, c, ycols], in1=mean.t[:, 0:N], op=ALU.subtract), reads=[y, mean], writes=[t])
            self.dve(lambda: V.tensor_tensor(out=t.t[:, 0:N], in0=t.t[:, 0:N], in1=rstd.t[:, 0:N], op=ALU.mult), reads=[t, rstd], writes=[t])
            if out_f is not None:
                self.act(lambda: A.activation(out=out_f.t[:, c, out_f_cols], in_=t.t[:, 0:N], func=AF.Identity,
                                              scale=gb.t[:, gi, c:c + 1], bias=gb.t[:, bi, c:c + 1]), reads=[t, gb], writes=[out_f])
            if out_b is not None:
                self.pool(lambda: G.tensor_scalar(out=out_b.t[:, c, out_b_cols], in0=t.t[:, 0:N], scalar1=gb.t[:, gi, c:c + 1],
                                                  scalar2=gb.t[:, bi, c:c + 1], op0=ALU.mult, op1=ALU.add), reads=[t, gb], writes=[out_b])

    def glu(self, xb, acc, NT, wg, wu, wd, F, psA, psB, psD, gate=None, GCH=4):
        V, A, G, PE = self.V, self.A, self.G, self.PE
        if not hasattr(self, "glu_w"):
            self.glu_w = [(self.sb(f"glu_wg{i}", [128, 8, GCH * 128], BF16), self.sb(f"glu_wu{i}", [128, 8, GCH * 128], BF16),
                           self.sb(f"glu_wd{i}", [128, GCH, 1024], BF16)) for i in range(3)]
            self.glu_h = [self.sb(f"glu_h{i}", [128, GCH, 512], BF16) for i in range(2)]
            self.glu_sg = [self.sb(f"glu_sg{i}", [128, 512]) for i in range(2)]
            self.glu_tt = [self.sb(f"glu_tt{i}", [128, 512]) for i in range(2)]
            self.glu_cnt = 0
        wgr = wg.rearrange("(c p) f -> p c f", p=128)
        wur = wu.rearrange("(c p) f -> p c f", p=128)
        groups = []
        f0 = 0
        while f0 < F:
            nch = min(GCH, (F - f0) // 128)
            groups.append((f0, nch))
            f0 += nch * 128
        it = 0
        base = self.glu_cnt
        self.glu_cnt += len(groups)

        def load(gi_):
            f0, nch = groups[gi_]
            s = (base + gi_) % 3
            Wg, Wu, Wd = self.glu_w[s]
            fw_ = nch * 128
            self.dma("pool", Wg.t[:, :, 0:fw_], wgr[:, :, f0:f0 + fw_], f"glw{s}", writes=[Wg])
            self.dma("pool", Wu.t[:, :, 0:fw_], wur[:, :, f0:f0 + fw_], f"glw{s}", writes=[Wu])
            self.dma("pool", Wd.t[:, 0:nch, :], wd[f0:f0 + fw_, :].rearrange("(c p) o -> p c o", p=128), f"glw{s}", writes=[Wd])
        load(0)
        pending = None
        for gi_, (f0, nch) in enumerate(groups):
            if gi_ + 1 < len(groups):
                load(gi_ + 1)
            Wg, Wu, Wd = self.glu_w[(base + gi_) % 3]
            for tt in range(NT):
                ts_ = slice(tt * 512, (tt + 1) * 512)
                H = self.glu_h[it % 2]
                it += 1
                for fc in range(nch):
                    pa = psA[fc % 2]
                    pb = psB[fc % 2]
                    sg = self.glu_sg[fc % 2]
                    t2 = self.glu_tt[fc % 2]
                    self.mm(pa.t[:], [(Wg.t[:, kc, fc * 128:(fc + 1) * 128], xb.t[:, kc, ts_]) for kc in range(8)], [Wg, xb], [pa])
                    self.mm(pb.t[:], [(Wu.t[:, kc, fc * 128:(fc + 1) * 128], xb.t[:, kc, ts_]) for kc in range(8)], [Wu, xb], [pb])
                    self.act(lambda: A.activation(out=sg.t[:], in_=pa.t[:], func=AF.Silu), reads=[pa], writes=[sg])
                    if gate is None:
                        self.dve(lambda: V.tensor_tensor(out=H.t[:, fc, :], in0=sg.t[:], in1=pb.t[:], op=ALU.mult), reads=[sg, pb], writes=[H])
                    else:
                        self.dve(lambda: V.tensor_tensor(out=t2.t[:], in0=sg.t[:], in1=pb.t[:], op=ALU.mult), reads=[sg, pb], writes=[t2])
                        self.pool(lambda: G.tensor_tensor(out=H.t[:, fc, :], in0=t2.t[:], in1=gate.t[:, ts_], op=ALU.mult), reads=[t2, gate], writes=[H])
                def down(Wd=Wd, H=H, nch=nch, ts_=ts_):
                    for oc in range(8):
                        pd = psD[oc % 2]
                        self.mm(pd.t[:], [(Wd.t[:, fc, oc * 128:(oc + 1) * 128], H.t[:, fc, :]) for fc in range(nch)], [Wd, H], [pd])
                        self.dve(lambda: V.tensor_tensor(out=acc.t[:, oc, ts_], in0=acc.t[:, oc, ts_], in1=pd.t[:], op=ALU.add), reads=[acc, pd], writes=[acc])
                if pending is not None:
                    pending()
                pending = down
        if pending is not None:
            pending()


def build_l2(nc, TC):
    NT = TC // 512
    xT = nc.dram_tensor("xT", [1024, TC], F32, kind="ExternalInput").ap()
    ogT = nc.dram_tensor("ogT", [1024, TC], F32, kind="ExternalInput").ap()
    wo = nc.dram_tensor("wo", [1024, 1024], F32, kind="ExternalInput").ap()
    lnp = nc.dram_tensor("lnp", [128, 4, 8], F32, kind="ExternalInput").ap()
    wg = nc.dram_tensor("wg", [1024, 2816], F32, kind="ExternalInput").ap()
    wu = nc.dram_tensor("wu", [1024, 2816], F32, kind="ExternalInput").ap()
    wd = nc.dram_tensor("wd", [2816, 1024], F32, kind="ExternalInput").ap()
    outT = nc.dram_tensor("outT", [1024, TC], F32, kind="ExternalOutput").ap()
    with ExitStack() as es:
        k = KC(nc, es)
        V, A, G, PE = k.V, k.A, k.G, k.PE
        k.consts()
        banks = [k.bank(f"bk{i}") for i in range(8)]
        gb = k.sb("gb", [128, 4, 8])
        k.dma("sp", gb.t[:], lnp, "c0", writes=[gb])
        acc = k.sb("acc", [128, 8, TC])
        x1b = k.sb("x1b", [128, 8, TC], BF16)
        xTr = xT.rearrange("(c p) t -> p c t", p=128)
        ogTr = ogT.rearrange("(c p) t -> p c t", p=128)
        outTr = outT.rearrange("(c p) t -> p c t", p=128)
        with ExitStack() as es2:
            k2es = es2
            wo_b = TL(es2.enter_context(nc.sbuf_tensor("wo_b", [128, 8, 1024], BF16)), "wo_b")
            og_b = TL(es2.enter_context(nc.sbuf_tensor("og_b", [128, 8, 512], BF16)), "og_b")
            xf = TL(es2.enter_context(nc.sbuf_tensor("xf", [128, 8, 512], F32)), "xf")
            y = TL(es2.enter_context(nc.sbuf_tensor("y", [128, 8, 512], F32)), "y")
            k.es = es2
            k.fw.es = es2
            k.ln_alloc()
            k.es = es
            k.fw.es = es
            k.dma("pool", wo_b.t[:], wo.rearrange("(c p) n -> p c n", p=128), "c1", writes=[wo_b])
            for tt in range(NT):
                ts_ = slice(tt * 512, (tt + 1) * 512)
                k.dma("pool", og_b.t[:], ogTr[:, :, ts_], "og", writes=[og_b])
                k.dma("sp", xf.t[:], xTr[:, :, ts_], "xf", writes=[xf])
                for oc in range(8):
                    pb = banks[oc % 2]
                    k.mm(pb.t[:], [(wo_b.t[:, kc, oc * 128:(oc + 1) * 128], og_b.t[:, kc, :]) for kc in range(8)], [wo_b, og_b], [pb])
                    k.dve(lambda: V.scalar_tensor_tensor(out=y.t[:, oc, :], in0=xf.t[:, oc, :], scalar=ALPHA, in1=pb.t[:],
                                                         op0=ALU.mult, op1=ALU.add), reads=[xf, pb], writes=[y])
                k.layernorm(y, slice(0, 512), gb, 0, 1, banks[2], banks[3], out_f=xf, out_f_cols=slice(0, 512), out_b=x1b, out_b_cols=ts_)
                for c in range(8):
                    k.act(lambda: A.activation(out=acc.t[:, c, ts_], in_=xf.t[:, c, :], func=AF.Copy, scale=ALPHA), reads=[xf], writes=[acc])
            k.barrier()
        k.glu(x1b, acc, NT, wg, wu, wd, 2816, banks[0:2], banks[2:4], banks[4:6])
        k.barrier()
        k.ln_alloc2 = True
        k.ln_sq = [k.sb(f"l2_sq{i}", [128, 512]) for i in range(2)]
        k.ln_mean = k.sb("l2_mean", [128, 512]); k.ln_msq = k.sb("l2_msq", [128, 512]); k.ln_var = k.sb("l2_var", [128, 512])
        k.ln_lnv = k.sb("l2_lnv", [128, 512]); k.ln_rstd = k.sb("l2_rstd", [128, 512])
        k.ln_t = [k.sb(f"l2_t{i}", [128, 512]) for i in range(2)]
        for tt in range(NT):
            ts_ = slice(tt * 512, (tt + 1) * 512)
            k.layernorm(acc, ts_, gb, 2, 3, banks[6], banks[7], out_f=acc, out_f_cols=ts_)
            k.dma("sp", outTr[:, :, ts_], acc.t[:, :, ts_], f"out{tt % 2}", reads=[acc])
        k.finish(["out0", "out1"])
        print("L2 instrs", k.fw.ninstr, "waits", k.fw.nwaits)
    return nc


def build_l3b(nc, TC):
    NT = TC // 512
    NB = TC // 128
    x2T = nc.dram_tensor("x2T", [1024, TC], F32, kind="ExternalInput").ap()
    wr = nc.dram_tensor("wr", [1024, 8], F32, kind="ExternalInput").ap()
    lnp = nc.dram_tensor("lnp", [128, 2, 8], F32, kind="ExternalInput").ap()
    mwg = nc.dram_tensor("mwg", [8, 1024, 3584], F32, kind="ExternalInput").ap()
    mwu = nc.dram_tensor("mwu", [8, 1024, 3584], F32, kind="ExternalInput").ap()
    mwd = nc.dram_tensor("mwd", [8, 3584, 1024], F32, kind="ExternalInput").ap()
    outT = nc.dram_tensor("outT", [1024, TC], F32, kind="ExternalOutput").ap()
    with ExitStack() as es:
        k = KC(nc, es)
        V, A, G, PE = k.V, k.A, k.G, k.PE
        k.consts()
        banks = [k.bank(f"bk{i}") for i in range(8)]
        gb = k.sb("gb", [128, 2, 8])
        k.dma("sp", gb.t[:], lnp, "c0", writes=[gb])
        acc = k.sb("acc", [128, 8, TC])
        x2b = k.sb("x2b", [128, 8, TC], BF16)
        gates = k.sb("gates", [128, NB, 8])
        x2Tr = x2T.rearrange("(c p) t -> p c t", p=128)
        outTr = outT.rearrange("(c p) t -> p c t", p=128)
        k.dma("pool", x2b.t[:], x2Tr, "c1", writes=[x2b])
        with ExitStack() as es2:
            k.es = es2; k.fw.es = es2
            xf = [k.sb(f"xf{i}", [128, 8, 512]) for i in range(2)]
            wr_s = k.sb("wr_s", [128, 8, 8])
            lg = [k.sb(f"lg{i}", [128, 8]) for i in range(2)]
            m1 = [k.sb(f"m1{i}", [128, 1]) for i in range(2)]
            nm1 = [k.sb(f"nm1{i}", [128, 1]) for i in range(2)]
            eq1 = [k.sb(f"eq1{i}", [128, 8]) for i in range(2)]
            l2 = [k.sb(f"l2{i}", [128, 8]) for i in range(2)]
            m2 = [k.sb(f"m2{i}", [128, 1]) for i in range(2)]
            sel = [k.sb(f"sel{i}", [128, 8]) for i in range(2)]
            ex = [k.sb(f"ex{i}", [128, 8]) for i in range(2)]
            e2 = [k.sb(f"e2{i}", [128, 8]) for i in range(2)]
            ssum = [k.sb(f"ssum{i}", [128, 1]) for i in range(2)]
            rs = [k.sb(f"rs{i}", [128, 1]) for i in range(2)]
            k.es = es; k.fw.es = es
            k.dma("sp", wr_s.t[:], wr.rearrange("(c p) e -> p c e", p=128), "c0", writes=[wr_s])
            for tt in range(NT):
                ts_ = slice(tt * 512, (tt + 1) * 512)
                X = xf[tt % 2]
                k.dma("sp", X.t[:], x2Tr[:, :, ts_], f"xf{tt % 2}", writes=[X])
                for c in range(8):
                    k.act(lambda: A.activation(out=acc.t[:, c, ts_], in_=X.t[:, c, :], func=AF.Copy, scale=ALPHA), reads=[X], writes=[acc])
                for bl in range(4):
                    b_ = tt * 4 + bl
                    i = b_ % 2
                    pb = banks[i]
                    k.mm(pb.t[:, 0:8], [(X.t[:, kc, bl * 128:(bl + 1) * 128], wr_s.t[:, kc, :]) for kc in range(8)], [X, wr_s], [pb])
                    k.act(lambda: A.copy(out=lg[i].t[:], in_=pb.t[:, 0:8]), reads=[pb], writes=[lg[i]])
                    k.dve(lambda: V.tensor_reduce(out=m1[i].t[:], in_=lg[i].t[:], axis=AX.X, op=ALU.max), reads=[lg[i]], writes=[m1[i]])
                    k.act(lambda: A.activation(out=nm1[i].t[:], in_=m1[i].t[:], func=AF.Copy, scale=-1.0), reads=[m1[i]], writes=[nm1[i]])
                    k.dve(lambda: V.tensor_scalar(out=eq1[i].t[:], in0=lg[i].t[:], scalar1=m1[i].t[:, 0:1], scalar2=None, op0=ALU.is_equal),
                          reads=[lg[i], m1[i]], writes=[eq1[i]])
                    k.dve(lambda: V.scalar_tensor_tensor(out=l2[i].t[:], in0=eq1[i].t[:], scalar=-1e30, in1=lg[i].t[:], op0=ALU.mult, op1=ALU.add),
                          reads=[eq1[i], lg[i]], writes=[l2[i]])
                    k.dve(lambda: V.tensor_reduce(out=m2[i].t[:], in_=l2[i].t[:], axis=AX.X, op=ALU.max), reads=[l2[i]], writes=[m2[i]])
                    k.dve(lambda: V.tensor_scalar(out=sel[i].t[:], in0=lg[i].t[:], scalar1=m2[i].t[:, 0:1], scalar2=None, op0=ALU.is_ge),
                          reads=[lg[i], m2[i]], writes=[sel[i]])
                    k.act(lambda: A.activation(out=ex[i].t[:], in_=lg[i].t[:], func=AF.Exp, bias=nm1[i].t[:, 0:1]), reads=[lg[i], nm1[i]], writes=[ex[i]])
                    k.dve(lambda: V.tensor_tensor(out=e2[i].t[:], in0=ex[i].t[:], in1=sel[i].t[:], op=ALU.mult), reads=[ex[i], sel[i]], writes=[e2[i]])
                    k.dve(lambda: V.tensor_reduce(out=ssum[i].t[:], in_=e2[i].t[:], axis=AX.X, op=ALU.add), reads=[e2[i]], writes=[ssum[i]])
                    k.dve(lambda: V.reciprocal(out=rs[i].t[:], in_=ssum[i].t[:]), reads=[ssum[i]], writes=[rs[i]])
                    k.dve(lambda: V.tensor_scalar(out=gates.t[:, b_, :], in0=e2[i].t[:], scalar1=rs[i].t[:, 0:1], scalar2=None, op0=ALU.mult),
                          reads=[e2[i], rs[i]], writes=[gates])
            k.barrier()
        with ExitStack() as es3:
            k.es = es3; k.fw.es = es3
            gcolB = [k.sb(f"gcolB{i}", [128, 128]) for i in range(2)]
            gbc = [k.sb(f"gbc{i}", [128, TC], BF16) for i in range(2)]
            for e in range(8):
                Gb = gbc[e % 2]
                for b_ in range(NB):
                    gc_ = gcolB[b_ % 2]
                    pb = banks[6 + (b_ // 4) % 2]
                    k.act(lambda: A.activation(out=gc_.t[:], in_=k.onesf.t[:], func=AF.Copy, scale=gates.t[:, b_, e:e + 1]), reads=[k.onesf, gates], writes=[gc_])
                    k.mm(pb.t[:, (b_ % 4) * 128:(b_ % 4 + 1) * 128], [(gc_.t[:], k.identf.t[:])], [gc_, k.identf], [pb])
                    if b_ % 4 == 3:
                        t0 = (b_ // 4) * 512
                        k.act(lambda: A.copy(out=Gb.t[:, t0:t0 + 512], in_=pb.t[:]), reads=[pb], writes=[Gb])
                k.glu(x2b, acc, NT, mwg[e], mwu[e], mwd[e], 3584, banks[0:2], banks[2:4], banks[4:6], gate=Gb, GCH=MOE_GCH)
            k.barrier()
            k.es = es; k.fw.es = es
        k.barrier()
        k.ln_alloc()
        for tt in range(NT):
            ts_ = slice(tt * 512, (tt + 1) * 512)
            k.layernorm(acc, ts_, gb, 0, 1, banks[6], banks[7], out_f=acc, out_f_cols=ts_)
            k.dma("sp", outTr[:, :, ts_], acc.t[:, :, ts_], f"out{tt % 2}", reads=[acc])
        k.finish(["out0", "out1"])
        print("L3b instrs", k.fw.ninstr, "waits", k.fw.nwaits)
    return nc


class _Stop(Exception):
    pass


def build_l3a(nc, TC):
    STOP3 = 99
    NT = TC // 512
    TE = TC + 128
    NBE = TE // 128
    x1e = nc.dram_tensor("x1e", [1024, TE], F32, kind="ExternalInput").ap()
    wq = nc.dram_tensor("wq", [1024, 1024], F32, kind="ExternalInput").ap()
    bq = nc.dram_tensor("bq", [128, 8], F32, kind="ExternalInput").ap()
    wkv = nc.dram_tensor("wkv", [1024, 256], F32, kind="ExternalInput").ap()
    bkv = nc.dram_tensor("bkv", [128, 2], F32, kind="ExternalInput").ap()
    wo = nc.dram_tensor("wo", [1024, 1024], F32, kind="ExternalInput").ap()
    biasT = nc.dram_tensor("biasT", [128, 8, 512], F32, kind="ExternalInput").ap()
    maskT = nc.dram_tensor("maskT", [128, 512], F32, kind="ExternalInput").ap()
    fmT = nc.dram_tensor("fmT", [128, 512], F32, kind="ExternalInput").ap()
    sinkb = nc.dram_tensor("sinkb", [128, 16], F32, kind="ExternalInput").ap()
    lnp = nc.dram_tensor("lnp", [128, 2, 8], F32, kind="ExternalInput").ap()
    x2T = nc.dram_tensor("x2T", [1024, TC], F32, kind="ExternalOutput").ap()
    with ExitStack() as es:
        k = KC(nc, es)
        V, A, G, PE = k.V, k.A, k.G, k.PE
        k.consts()
        bk = [k.bank(f"bk{i}") for i in range(5)]
        btr = k.bank("btr", BF16)
        bl1 = k.bank("bl1"); bl2 = k.bank("bl2")
        gb = k.sb("gb", [128, 2, 8]); k.dma("sp", gb.t[:], lnp, "c0", writes=[gb])
        bq_s = k.sb("bq_s", [128, 8]); k.dma("sp", bq_s.t[:], bq, "c0", writes=[bq_s])
        bkv_s = k.sb("bkv_s", [128, 2]); k.dma("sp", bkv_s.t[:], bkv, "c0", writes=[bkv_s])
        bm = k.sb("bm", [128, 8, 512]); k.dma("sp", bm.t[:], biasT, "c2", writes=[bm])
        mk = k.sb("mk", [128, 512]); k.dma("sp", mk.t[:], maskT, "c0", writes=[mk])
        fm = k.sb("fm", [128, 512]); k.dma("sp", fm.t[:], fmT, "c0", writes=[fm])
        snk = k.sb("snk", [128, 16]); k.dma("sp", snk.t[:], sinkb, "c0", writes=[snk])
        esink = k.sb("esink", [128, 16])
        k.act(lambda: A.activation(out=esink.t[:], in_=snk.t[:], func=AF.Exp), reads=[snk], writes=[esink])
        bqs = k.sb("bqs", [128, 8])
        k.dve(lambda: V.tensor_scalar(out=bqs.t[:], in0=bq_s.t[:], scalar1=0.125, scalar2=None, op0=ALU.mult), reads=[bq_s], writes=[bqs])
        for c in range(8):
            k.pool(lambda: G.tensor_tensor(out=bm.t[:, c, :], in0=bm.t[:, c, :], in1=mk.t[:], op=ALU.add), reads=[bm, mk], writes=[bm])
        wq_b = k.sb("wq_b", [128, 8, 1024], BF16); k.dma("pool", wq_b.t[:], wq.rearrange("(c p) n -> p c n", p=128), "w0", writes=[wq_b])
        wkv_b = k.sb("wkv_b", [128, 8, 256], BF16); k.dma("pool", wkv_b.t[:], wkv.rearrange("(c p) n -> p c n", p=128), "w1", writes=[wkv_b])
        wo_b = k.sb("wo_b", [128, 8, 1024], BF16); k.dma("pool", wo_b.t[:], wo.rearrange("(c p) n -> p c n", p=128), "w2", writes=[wo_b])
        x1er = x1e.rearrange("(c p) t -> p c t", p=128)
        x2Tr = x2T.rearrange("(c p) t -> p c t", p=128)
        xbe = k.sb("xbe", [128, 8, TE], BF16); k.dma("pool", xbe.t[:], x1er, "w3", writes=[xbe])
        kTz = [k.sb(f"kTz{i}", [128, TE], BF16) for i in range(2)]
        for i in range(2):
            k.pool(lambda: G.memset(kTz[i].t[:], 0.0), writes=[kTz[i]])
        vTt = k.sb("vTt", [128, 512], BF16)
        vaug = k.sb("vaug", [128, NBE, 2, 80], BF16)
        k.pool(lambda: G.memset(vaug.t[:], 1.0), writes=[vaug])
        col = 0
        while col < TE:
            n = min(512, TE - col)
            cs = slice(col, col + n)
            k.mm(bk[0].t[:, 0:n], [(wkv_b.t[:, kc, 0:128], xbe.t[:, kc, cs]) for kc in range(8)], [wkv_b, xbe], [bk[0]])
            for kv in range(2):
                ps_ = slice(kv * 64, (kv + 1) * 64)
                k.act(lambda: A.activation(out=kTz[kv].t[ps_, cs], in_=bk[0].t[ps_, 0:n], func=AF.Identity, bias=bkv_s.t[ps_, 0:1]),
                      reads=[bk[0], bkv_s], writes=[kTz[kv]])
            k.mm(bk[1].t[:, 0:n], [(wkv_b.t[:, kc, 128:256], xbe.t[:, kc, cs]) for kc in range(8)], [wkv_b, xbe], [bk[1]])
            k.act(lambda: A.activation(out=vTt.t[:, 0:n], in_=bk[1].t[:, 0:n], func=AF.Identity, bias=bkv_s.t[:, 1:2]), reads=[bk[1], bkv_s], writes=[vTt])
            for j in range(n // 128):
                blk = col // 128 + j
                k.pe(lambda: PE.transpose(btr.t[:, 0:128], vTt.t[:, j * 128:(j + 1) * 128], k.identb.t[:]), reads=[vTt, k.identb], writes=[btr])
                k.act(lambda: A.copy(out=vaug.t[:, blk, :, 0:64], in_=btr.t[:, 0:128].rearrange("p (k d) -> p k d", k=2)), reads=[btr], writes=[vaug])
            col += n
        qT = k.sb("qT", [128, 8, 512], BF16)
        stop_here = (STOP3 == 1)
        attnT = k.sb("attnT", [128, 8, 512], BF16)
        sbs = [k.sb(f"sbs{i}", [128, 512]) for i in range(2)]
        pT = [k.sb(f"pT{i}", [128, 512], BF16) for i in range(2)]
        den = [k.sb(f"den{i}", [128, 2]) for i in range(2)]
        rinv = [k.sb(f"rinv{i}", [128, 2]) for i in range(2)]
        on = [k.sb(f"on{i}", [128, 128], BF16) for i in range(2)]
        xf = k.sb("xf", [128, 8, 512])
        y = k.sb("y", [128, 8, 512])
        k.ln_alloc()
        it = 0
        for tt in range(NT if not stop_here else 0):
            e0 = 128 + tt * 512
            for c in range(8):
                pb = bk[c % 2]
                k.mm(pb.t[:], [(wq_b.t[:, kc, c * 128:(c + 1) * 128], xbe.t[:, kc, e0:e0 + 512]) for kc in range(8)], [wq_b, xbe], [pb])
                k.act(lambda: A.activation(out=qT.t[:, c, :], in_=pb.t[:], func=AF.Identity, scale=0.125, bias=bqs.t[:, c:c + 1]),
                      reads=[pb, bqs], writes=[qT])
            if STOP3 == 2: break
            for bl in range(4):
                ne = 1 + tt * 4 + bl
                first = (tt == 0 and bl == 0)
                for c in range(8):
                    i = it % 2
                    it += 1
                    st = bk[2 + i]
                    def fsc():
                        last = None
                        for kv in range(2):
                            for half in range(2):
                                q0 = (kv * 2 + half) * 128
                                last = PE.matmul(st.t[:, q0:q0 + 128], lhsT=kTz[kv].t[:, (ne - 1 + half) * 128:(ne + half) * 128],
                                                 rhs=qT.t[:, c, bl * 128:(bl + 1) * 128], start=True, stop=True)
                        return last
                    k.pe(fsc, reads=[kTz[0], kTz[1], qT], writes=[st])
                    if STOP3 == 3: continue
                    S_ = sbs[i]
                    k.dve(lambda: V.tensor_tensor(out=S_.t[:], in0=st.t[:], in1=bm.t[:, c, :], op=ALU.add), reads=[st, bm], writes=[S_])
                    if first:
                        k.dve(lambda: V.tensor_tensor(out=S_.t[:], in0=S_.t[:], in1=fm.t[:], op=ALU.add), reads=[S_, fm], writes=[S_])
                    P_ = pT[i]
                    k.act(lambda: A.activation(out=P_.t[:], in_=S_.t[:], func=AF.Exp), reads=[S_], writes=[P_])
                    if STOP3 == 4: continue
                    ob = bk[4]
                    for kv in range(2):
                        k.mm(ob.t[:, kv * 128:kv * 128 + 65],
                             [(P_.t[:, (kv * 2 + half) * 128:(kv * 2 + half + 1) * 128], vaug.t[:, ne - 1 + half, kv, 0:65]) for half in range(2)],
                             [P_, vaug], [ob])
                    if STOP3 == 5: continue
                    D_ = den[i]; R_ = rinv[i]; O_ = on[i]
                    for kv in range(2):
                        k.act(lambda: A.activation(out=D_.t[:, kv:kv + 1], in_=ob.t[:, kv * 128 + 64:kv * 128 + 65], func=AF.Identity,
                                                   bias=esink.t[:, 2 * c + kv:2 * c + kv + 1]), reads=[ob, esink], writes=[D_])
                    k.dve(lambda: V.reciprocal(out=R_.t[:], in_=D_.t[:]), reads=[D_], writes=[R_])
                    for kv in range(2):
                        k.dve(lambda: V.tensor_scalar(out=O_.t[:, kv * 64:(kv + 1) * 64], in0=ob.t[:, kv * 128:kv * 128 + 64], scalar1=R_.t[:, kv:kv + 1],
                                                      scalar2=None, op0=ALU.mult), reads=[ob, R_], writes=[O_])
                    if STOP3 == 6: continue
                    k.pe(lambda: PE.transpose(btr.t[:, 0:128], O_.t[:], k.identb.t[:]), reads=[O_, k.identb], writes=[btr])
                    k.act(lambda: A.copy(out=attnT.t[:, c, bl * 128:(bl + 1) * 128], in_=btr.t[:, 0:128]), reads=[btr], writes=[attnT])
            k.dma("sp", xf.t[:], x1er[:, :, e0:e0 + 512], "xf", writes=[xf])
            for oc in range(8):
                pb = bk[oc % 2]
                k.mm(pb.t[:], [(wo_b.t[:, kc, oc * 128:(oc + 1) * 128], attnT.t[:, kc, :]) for kc in range(8)], [wo_b, attnT], [pb])
                k.dve(lambda: V.scalar_tensor_tensor(out=y.t[:, oc, :], in0=xf.t[:, oc, :], scalar=ALPHA, in1=pb.t[:], op0=ALU.mult, op1=ALU.add),
                      reads=[xf, pb], writes=[y])
            k.layernorm(y, slice(0, 512), gb, 0, 1, bl1, bl2, out_f=y, out_f_cols=slice(0, 512))
            k.dma("sp", x2Tr[:, :, tt * 512:(tt + 1) * 512], y.t[:], "out", reads=[y])
        k.finish(["out"])
        print("L3a instrs", k.fw.ninstr, "waits", k.fw.nwaits)
    return nc


def t5_bucket_np(dist):
    max_exact = 16
    df = np.maximum(dist, 1).astype(np.float32)
    large = max_exact + (np.log(df / max_exact) / math.log(128 / max_exact) * (32 - max_exact)).astype(np.int32)
    large = np.minimum(large, 31)
    return np.where(dist < max_exact, dist, large)
def swa_tables(rel_bias):
    s = np.arange(128)[:, None, None, None]
    kv = np.arange(2)[None, :, None, None]
    half = np.arange(2)[None, None, :, None]
    i = np.arange(128)[None, None, None, :]
    dist = i + 128 - half * 128 - s + 0 * kv
    valid = (dist >= 0) & (dist < 128)
    bucket = t5_bucket_np(np.maximum(dist, 0))
    biasT = np.zeros((128, 8, 2, 2, 128), np.float32)
    for c in range(8):
        for k_ in range(2):
            head = c + 8 * k_
            biasT[:, c, k_] = rel_bias[bucket[:, k_], head]
    maskT = np.where(valid, 0.0, -30000.0).astype(np.float32).reshape(128, 512)
    return biasT.reshape(128, 8, 512), maskT
def swa_inputs(x1e, b_w_in, b_b_in, b_sinks, b_w_out, rel_bias, ln_g, ln_b, first):
    perm = np.concatenate([np.concatenate([np.arange(c * 64, (c + 1) * 64), np.arange((8 + c) * 64, (9 + c) * 64)]) for c in range(8)])
    wq = np.ascontiguousarray(b_w_in[:, :1024][:, perm])
    bq = np.ascontiguousarray(b_b_in[:1024][perm].reshape(8, 128).T)
    wkv = np.ascontiguousarray(b_w_in[:, 1024:1280])
    bkv = np.ascontiguousarray(b_b_in[1024:1280].reshape(2, 128).T)
    wo = np.ascontiguousarray(b_w_out[perm, :])
    biasT, maskT = swa_tables(rel_bias)
    fm = np.zeros((128, 2, 2, 128), np.float32)
    if first:
        fm[:, :, 0, :] = -30000.0
    sink_order = np.array([c + 8 * k_ for c in range(8) for k_ in range(2)])
    sinkb = np.ascontiguousarray(np.broadcast_to(b_sinks[sink_order][None, :], (128, 16))).astype(np.float32)
    lnp = np.stack([ln_g, ln_b], 0).reshape(2, 8, 128).transpose(2, 0, 1).copy().astype(np.float32)
    return {"x1e": np.ascontiguousarray(x1e.T), "wq": wq, "bq": bq, "wkv": wkv, "bkv": bkv, "wo": wo, "biasT": biasT, "maskT": maskT,
            "fmT": fm.reshape(128, 512), "sinkb": sinkb, "lnp": lnp}


_NC_CACHE = {}


def _get_nc(name, builder, *args):
    if name not in _NC_CACHE:
        nc = bass.Bass("TRN2", target_bir_lowering=False)
        builder(nc, *args)
        _NC_CACHE[name] = nc
    return _NC_CACHE[name]


def _gdn_inputs(xb, j, w_in, conv_w, a_log, dt_bias, norm_w):
    q0 = 0; k0 = 512; v0 = 1024; z0 = 2048; b0 = 3072; a0 = 3080
    cols_qkv = np.concatenate([np.arange(q0 + j * 128, q0 + (j + 1) * 128), np.arange(k0 + j * 128, k0 + (j + 1) * 128),
                               np.arange(v0 + 2 * j * 128, v0 + (2 * j + 2) * 128)])
    cols_zba = np.concatenate([np.arange(z0 + 2 * j * 128, z0 + (2 * j + 2) * 128),
                               [b0 + 2 * j, b0 + 2 * j + 1, a0 + 2 * j, a0 + 2 * j + 1]])
    return {
        "xT": np.ascontiguousarray(xb.T),
        "wqkv": np.ascontiguousarray(w_in[:, cols_qkv]),
        "wzba": np.ascontiguousarray(w_in[:, cols_zba]),
        "convw": np.ascontiguousarray(conv_w[:, cols_qkv].T),
        "hp": np.array([[a_log[2 * j], a_log[2 * j + 1], dt_bias[2 * j], dt_bias[2 * j + 1]]], np.float32),
        "normw": np.ascontiguousarray(norm_w[None, :]).astype(np.float32),
    }


def _lncols(rows):
    return np.stack(rows, 0).reshape(len(rows), 8, 128).transpose(2, 0, 1).copy().astype(np.float32)


def kernel(x, a_w_in, a_conv_w, a_a_log, a_dt_bias, a_norm_w, a_w_out, b_w_in, b_b_in, b_sinks,
           b_w_out, rel_bias, ffn_w_gate, ffn_w_up, ffn_w_down, moe_router, moe_w_gate, moe_w_up,
           moe_w_down, ln_g, ln_b):
    f32 = lambda a: np.ascontiguousarray(np.asarray(a, dtype=np.float32))
    x = f32(x)
    B, T, D = x.shape
    NCORE = 8
    TC = (B * T) // NCORE
    per_b = T // TC
    cores = list(range(NCORE))
    ln_g = f32(ln_g); ln_b = f32(ln_b)

    nc1 = _get_nc("gdn", build_gdn, T)
    w_in = f32(a_w_in[0]); conv_w = f32(a_conv_w[0])
    a_log = f32(a_a_log[0]); dtb = f32(a_dt_bias[0]); nw = f32(a_norm_w[0])
    im1 = [_gdn_inputs(x[c // 4], c % 4, w_in, conv_w, a_log, dtb, nw) for c in cores]
    r1 = run_bass_kernel_spmd(nc1, im1, core_ids=cores).results
    og = np.empty((B, T, D), np.float32)
    for c in cores:
        og[c // 4, :, (c % 4) * 256:(c % 4 + 1) * 256] = r1[c]["og"]

    def tok(c):
        return c // per_b, (c % per_b) * TC

    nc2 = _get_nc("l2", build_l2, TC)
    wo0 = f32(a_w_out[0]); wg = f32(ffn_w_gate[0]); wu = f32(ffn_w_up[0]); wd = f32(ffn_w_down[0])
    lnp0 = _lncols([ln_g[0, 0], ln_b[0, 0], ln_g[0, 1], ln_b[0, 1]])
    im2 = []
    for c in cores:
        b, t0 = tok(c)
        im2.append({"xT": np.ascontiguousarray(x[b, t0:t0 + TC].T), "ogT": np.ascontiguousarray(og[b, t0:t0 + TC].T),
                    "wo": wo0, "lnp": lnp0, "wg": wg, "wu": wu, "wd": wd})
    r2 = run_bass_kernel_spmd(nc2, im2, core_ids=cores).results
    x1 = np.empty((B, T, D), np.float32)
    for c in cores:
        b, t0 = tok(c)
        x1[b, t0:t0 + TC] = r2[c]["outT"].T

    nc3 = _get_nc("l3a", build_l3a, TC)
    im3 = []
    for c in cores:
        b, t0 = tok(c)
        x1e = np.zeros((TC + 128, D), np.float32)
        if t0 > 0:
            x1e[:128] = x1[b, t0 - 128:t0]
        x1e[128:] = x1[b, t0:t0 + TC]
        im3.append(swa_inputs(x1e, f32(b_w_in[0]), f32(b_b_in[0]), f32(b_sinks[0]), f32(b_w_out[0]), f32(rel_bias),
                              ln_g[1, 0], ln_b[1, 0], t0 == 0))
    r3 = run_bass_kernel_spmd(nc3, im3, core_ids=cores).results

    nc4 = _get_nc("l3b", build_l3b, TC)
    wr = f32(moe_router[0]); mwg = f32(moe_w_gate[0]); mwu = f32(moe_w_up[0]); mwd = f32(moe_w_down[0])
    lnp1 = _lncols([ln_g[1, 1], ln_b[1, 1]])
    im4 = [{"x2T": np.ascontiguousarray(r3[c]["x2T"]), "wr": wr, "lnp": lnp1, "mwg": mwg, "mwu": mwu, "mwd": mwd} for c in cores]
    r4 = run_bass_kernel_spmd(nc4, im4, core_ids=cores).results
    out = np.empty((B, T, D), np.float32)
    for c in cores:
        b, t0 = tok(c)
        out[b, t0:t0 + TC] = r4[c]["outT"].T
    return out
```

```python
import math
import numpy as np
from contextlib import ExitStack
import concourse.bass as bass
import concourse.mybir as mybir
from concourse.bass_utils import run_bass_kernel_spmd

F32 = mybir.dt.float32
BF16 = mybir.dt.bfloat16
AF = mybir.ActivationFunctionType
ALU = mybir.AluOpType
AX = mybir.AxisListType


class Res:
    __slots__ = ("name", "lw", "rd", "psum")

    def __init__(self, name, psum=False):
        self.name = name
        self.psum = psum
        self.lw = None
        self.rd = {}


class FW:
    def __init__(self, nc, es):
        self.nc = nc
        self.es = es
        self.engs = {"pe": nc.tensor, "dve": nc.vector, "act": nc.scalar, "pool": nc.gpsimd, "sp": nc.sync}
        self.sem = {}
        self.cnt = {}
        for k in self.engs:
            self.sem[k] = es.enter_context(nc.semaphore("sem_" + k))
            self.cnt[k] = 0
        self.known = {k: {} for k in self.engs}
        self.dsem = {}
        self.dcnt = {}
        self.nwaits = 0
        self.ninstr = 0

    def sb(self, name, shape, dt):
        return self.es.enter_context(self.nc.sbuf_tensor(name, list(shape), dt))

    def ps(self, name, shape, dt=F32):
        return self.es.enter_context(self.nc.psum_tensor(name, list(shape), dt))

    def dma_sem(self, name):
        if name not in self.dsem:
            self.dsem[name] = self.es.enter_context(self.nc.semaphore("dq_" + name))
            self.dcnt[name] = 0
        return name

    def _semobj(self, key):
        return self.sem[key] if key in self.sem else self.dsem[key]

    def _deps(self, eng, reads, writes):
        deps = {}
        def add(d):
            if d is None:
                return
            k, v = d
            if deps.get(k, 0) < v:
                deps[k] = v
        for r in reads:
            add(r.lw)
            if r.psum:
                for k, v in r.rd.items():
                    if k != eng:
                        add((k, v))
        for w in writes:
            add(w.lw)
            for k, v in w.rd.items():
                add((k, v))
        for k, v in deps.items():
            if k == eng and eng == "pe":
                continue
            if k in self.dsem:
                v = self.dcnt[k]
            if self.known[eng].get(k, 0) >= v:
                continue
            self.engs[eng].wait_ge(self._semobj(k), v)
            self.known[eng][k] = v
            self.nwaits += 1

    def _commit(self, key, val, reads, writes):
        for w in writes:
            w.lw = (key, val)
            w.rd = {}
        for r in reads:
            if r in writes:
                continue
            if r.rd.get(key, 0) < val:
                r.rd[key] = val

    def op(self, eng, fn, reads=(), writes=()):
        self._deps(eng, reads, writes)
        ins = fn()
        if isinstance(ins, (list, tuple)):
            ins = ins[-1]
        ins.then_inc(self.sem[eng], 1)
        self.cnt[eng] += 1
        self.ninstr += 1
        self._commit(eng, self.cnt[eng], reads, writes)
        return ins

    def dma(self, q, out, in_, slot, reads=(), writes=(), **kw):
        self.dma_sem(slot)
        self._deps(q, reads, writes)
        ins = self.engs[q].dma_start(out=out, in_=in_, **kw)
        ins.then_inc(self.dsem[slot], 16)
        self.dcnt[slot] += 16
        self._commit(slot, self.dcnt[slot], reads, writes)
        return ins

    def wait_all(self, eng, ress):
        self._deps(eng, ress, [])


NEG = -30000.0


class GTL:
    def __init__(self, t, name):
        self.t = t
        self.r = Res(name)


def build_gdn(nc, T, stop_at=99):
    NTILE = T // 512
    xT = nc.dram_tensor("xT", [1024, T], F32, kind="ExternalInput").ap()
    wqkv = nc.dram_tensor("wqkv", [1024, 512], F32, kind="ExternalInput").ap()
    wzba = nc.dram_tensor("wzba", [1024, 260], F32, kind="ExternalInput").ap()
    convw = nc.dram_tensor("convw", [512, 4], F32, kind="ExternalInput").ap()
    hp = nc.dram_tensor("hp", [1, 4], F32, kind="ExternalInput").ap()
    normw = nc.dram_tensor("normw", [1, 128], F32, kind="ExternalInput").ap()
    og = nc.dram_tensor("og", [T, 256], F32, kind="ExternalOutput").ap()

    with ExitStack() as es:
        fw = FW(nc, es)
        V, A, G, PE = nc.vector, nc.scalar, nc.gpsimd, nc.tensor

        def sb(name, shape, dt=F32):
            return GTL(fw.sb(name, shape, dt), name)

        CTX = {'par': 0, 'tile': 0}

        class DB:
            def __init__(self, tiles, key):
                self.tiles = tiles; self.key = key
            @property
            def t(self):
                return self.tiles[CTX[self.key] % 2].t
            @property
            def r(self):
                return self.tiles[CTX[self.key] % 2].r

        identb = sb("identb", [128, 128], BF16)
        identf = sb("identf", [128, 128])
        U = sb("U", [128, 128])
        Uneg = sb("Uneg", [128, 128])
        onesf = sb("onesf", [128, 128])
        onesb = sb("onesb", [128, 128], BF16)
        MASKS = sb("MASKS", [128, 128])
        BDm = sb("BDm", [128, 128])
        OFFm = sb("OFFm", [128, 128])

        def pool(fn, reads=(), writes=()):
            return fw.op("pool", fn, [x.r for x in reads], [x.r for x in writes])

        def dve(fn, reads=(), writes=()):
            return fw.op("dve", fn, [x.r for x in reads], [x.r for x in writes])

        def act(fn, reads=(), writes=()):
            return fw.op("act", fn, [x.r for x in reads], [x.r for x in writes])

        def pe(fn, reads=(), writes=()):
            return fw.op("pe", fn, [x.r for x in reads], [x.r for x in writes])

        def mm(out_ap, pairs, reads, writes):
            def f():
                n = len(pairs)
                last = None
                for i, (l, r) in enumerate(pairs):
                    last = PE.matmul(out_ap, lhsT=l, rhs=r, start=(i == 0), stop=(i == n - 1))
                return last
            return pe(f, reads, writes)

        for t_, val in ((identb, 0.0), (identf, 0.0), (U, 1.0), (Uneg, -1.0), (onesf, 1.0), (onesb, 1.0),
                        (MASKS, 0.0), (BDm, 0.0), (OFFm, 0.0)):
            pool(lambda t_=t_, val=val: G.memset(t_.t[:], val), writes=[t_])
        for t_ in (identb, identf):
            pool(lambda t_=t_: G.affine_select(out=t_.t[:], in_=t_.t[:], pattern=[[-1, 128]], compare_op=ALU.not_equal,
                                               fill=1.0, base=0, channel_multiplier=1), reads=[t_], writes=[t_])
        for t_ in (U, Uneg):
            pool(lambda t_=t_: G.affine_select(out=t_.t[:], in_=t_.t[:], pattern=[[1, 128]], compare_op=ALU.is_ge,
                                               fill=0.0, base=0, channel_multiplier=-1), reads=[t_], writes=[t_])
        pool(lambda: G.affine_select(out=MASKS.t[:], in_=MASKS.t[:], pattern=[[-1, 128]], compare_op=ALU.is_gt,
                                     fill=NEG, base=0, channel_multiplier=1), reads=[MASKS], writes=[MASKS])
        pool(lambda: G.memset(BDm.t[0:64, 0:64], 1.0), writes=[BDm])
        pool(lambda: G.memset(BDm.t[64:128, 64:128], 1.0), writes=[BDm])
        pool(lambda: G.memset(OFFm.t[64:128, 0:64], 1.0), writes=[OFFm])

        wqkv_b = sb("wqkv_b", [128, 8, 512], BF16)
        wzba_b = sb("wzba_b", [128, 8, 320], BF16)
        convw_s = sb("convw_s", [128, 4, 4])
        hp_s = sb("hp_s", [128, 4])
        normw_s = sb("normw_s", [128, 128])
        fw.dma("pool", wqkv_b.t[:], wqkv.rearrange("(c p) n -> p c n", p=128), "w0", writes=[wqkv_b.r])
        fw.dma("pool", wzba_b.t[:, :, 0:260], wzba.rearrange("(c p) n -> p c n", p=128), "w1", writes=[wzba_b.r])
        fw.dma("sp", convw_s.t[:], convw.rearrange("(c p) j -> p c j", p=128), "w2", writes=[convw_s.r])
        fw.dma("sp", hp_s.t[:], hp[0:1, :].partition_broadcast(128), "w2", writes=[hp_s.r])
        fw.dma("sp", normw_s.t[:], normw[0:1, :].partition_broadcast(128), "w2", writes=[normw_s.r])
        negA8 = sb("negA8", [128, 4, 2])
        dtb8 = sb("dtb8", [128, 4, 2])
        ea = sb("ea", [128, 2])
        act(lambda: A.activation(out=ea.t[:], in_=hp_s.t[:, 0:2], func=AF.Exp), reads=[hp_s], writes=[ea])
        for c in range(4):
            dve(lambda c=c: V.tensor_scalar(out=negA8.t[:, c, :], in0=ea.t[:], scalar1=-1.0, scalar2=None, op0=ALU.mult),
                reads=[ea], writes=[negA8])
            dve(lambda c=c: V.tensor_copy(out=dtb8.t[:, c, :], in_=hp_s.t[:, 2:4]), reads=[hp_s], writes=[dtb8])

        S = [sb(f"S{h}", [128, 128]) for h in range(2)]
        Sb = [sb(f"Sb{h}", [128, 128], BF16) for h in range(2)]
        for h in range(2):
            pool(lambda h=h: G.memset(S[h].t[:], 0.0), writes=[S[h]])
            pool(lambda h=h: G.memset(Sb[h].t[:], 0.0), writes=[Sb[h]])

        xb = [sb(f"xb{i}", [128, 8, 512], BF16) for i in range(2)]
        pre = [[sb(f"pre{i}_{ch}", [128, 515]) for ch in range(4)] for i in range(2)]
        for ch in range(4):
            pool(lambda ch=ch: G.memset(pre[0][ch].t[:, 0:3], 0.0), writes=[pre[0][ch]])
        cacc = [sb(f"cacc{ch}", [128, 512]) for ch in range(4)]
        qs = [sb(f"qs{i}", [128, 512]) for i in range(2)]
        sq = [sb(f"sq{i}", [128, 512], BF16) for i in range(2)]
        lnr = [sb(f"lnr{i}", [128, 512]) for i in range(2)]
        rr = [sb(f"rr{i}", [128, 512]) for i in range(2)]
        qnT = sb("qnT", [128, 512], BF16)
        knT = sb("knT", [128, 512], BF16)
        vT = [sb(f"vT{h}", [128, 512], BF16) for h in range(2)]
        zs = sb("zs", [128, 4, 256])
        nwz = DB([sb(f"nwz{i}", [128, 4, 2, 128]) for i in range(2)], 'tile')
        ba = sb("ba", [128, 4, 4])
        eb = sb("eb", [128, 4, 2])
        beta = sb("beta", [128, 4, 2])
        nbeta = sb("nbeta", [128, 4, 2])
        t1 = sb("t1", [128, 4, 2])
        e1 = sb("e1", [128, 4, 2])
        l1 = sb("l1", [128, 4, 2])
        gt = sb("gt", [128, 4, 2])
        gs = sb("gs", [128, 4])
        exa = DB([sb(f"exa{i}", [128, 4]) for i in range(2)], 'par')
        dk = sb("dk", [128, 2])
        ekd = sb("ekd", [128, 2])
        bg = sb("bg", [128, 2])
        vb = [sb(f"vb{h}", [128, 128], BF16) for h in range(2)]
        kbg = [sb(f"kbg{h}", [128, 128], BF16) for h in range(2)]
        kd = [DB([sb(f"kd{h}_{i}", [128, 128], BF16) for i in range(2)], 'par') for h in range(2)]
        gB = [sb(f"gB{h}", [128, 128]) for h in range(2)]
        dec = sb("dec", [128, 512])
        NA = [sb(f"NA{h}", [128, 128]) for h in range(2)]
        MoffT = [sb(f"MoffT{h}", [128, 128]) for h in range(2)]
        decc = [sb(f"decc{h}", [128, 128]) for h in range(2)]
        aqk = [sb(f"aqk{h}", [128, 128], BF16) for h in range(2)]
        aqkT = [DB([sb(f"aqkT{h}_{i}", [128, 128], BF16) for i in range(2)], 'par') for h in range(2)]
        qdT = [DB([sb(f"qdT{h}_{i}", [128, 128], BF16) for i in range(2)], 'par') for h in range(2)]
        PP = [[sb(f"PP{h}_{i}", [128, 384]) for i in range(2)] for h in range(2)]
        Tt = [[sb(f"Tt{h}_{i}", [128, 128]) for i in range(2)] for h in range(2)]
        TdY = [sb(f"TdY{h}", [128, 256]) for h in range(2)]
        TTb = [sb(f"TTb{h}", [128, 128], BF16) for h in range(2)]
        u_s = DB([sb(f"u_s{i}", [128, 256]) for i in range(2)], 'par')
        wT_s = DB([sb(f"wT_s{i}", [128, 256], BF16) for i in range(2)], 'par')
        vn = [sb(f"vn{h}", [128, 128], BF16) for h in range(2)]
        junk = [sb(f"junk{h}", [128, 128]) for h in range(2)]
        ss = [sb(f"ss{h}", [128, 1]) for h in range(2)]
        lns = [sb(f"lns{h}", [128, 1]) for h in range(2)]
        rinv = [sb(f"rinv{h}", [128, 1]) for h in range(2)]
        ogt = [sb(f"ogt{i}", [128, 256]) for i in range(2)]

        def pst(name, shape, dt=F32):
            return GTL(fw.ps(name, shape, dt), name)
        b0 = fw.ps("b0", [128, 512]); b1 = fw.ps("b1", [128, 512]); b2 = fw.ps("b2", [128, 512])
        b3 = fw.ps("b3", [128, 1024], BF16)
        b4 = fw.ps("b4", [128, 512]); b5 = fw.ps("b5", [128, 512]); b6 = fw.ps("b6", [128, 512]); b7 = fw.ps("b7", [128, 512])

        RB = [Res(f"bank{i}", psum=True) for i in range(8)]
        banks = [b0, b1, b2, b3, b4, b5, b6, b7]

        class PT:
            def __init__(self, bi, lo, hi, name):
                self.bank = banks[bi]; self.lo = lo; self.hi = hi; self.r = RB[bi]
            @property
            def ap(self):
                return self.bank[:, self.lo:self.hi]
        paA = [PT(4, 0, 512, "paA0"), PT(5, 0, 512, "paA1")]
        pzt = [PT(2, 0, 260, "pzt0"), PT(6, 0, 260, "pzt1")]
        pD = PT(2, 0, 512, "pD")
        pT_k = PT(3, 0, 128, "pT_k")
        pT_v = [PT(3, 128, 256, "pT_v0"), PT(3, 256, 384, "pT_v1")]
        pT_a = [PT(3, 384, 512, "pT_a0"), PT(3, 512, 640, "pT_a1")]
        pKK = PT(6, 0, 128, "pKK"); pQK = PT(6, 128, 256, "pQK"); pgcc = PT(6, 256, 260, "pgcc")
        pi_sq = [PT(4, 0, 256, "pisq0"), PT(5, 0, 256, "pisq1")]
        pi_pr = [PT(4, 256, 384, "pipr0"), PT(5, 256, 384, "pipr1")]
        pu = PT(2, 0, 256, "pu"); pwT = PT(2, 256, 512, "pwT")
        p_wS = [PT(0, 0, 128, "pwS0"), PT(1, 0, 128, "pwS1")]
        p_Sn = [PT(0, 128, 256, "pSn0"), PT(1, 128, 256, "pSn1")]
        p_o = [PT(7, 0, 128, "po0"), PT(7, 128, 256, "po1")]

        xTr = xT.rearrange("(c p) t -> p c t", p=128)

        def load_x(n):
            s = n % 2
            fw.dma("pool", xb[s].t[:], xTr[:, :, n * 512:(n + 1) * 512], f"x{s}", writes=[xb[s].r])

        def stageA(n):
            s = n % 2
            if n + 1 < NTILE:
                load_x(n + 1)
            X = xb[s]
            yield
            for ch in range(4):
                pa = paA[ch % 2]
                mm(pa.ap, [(wqkv_b.t[:, kc, ch * 128:(ch + 1) * 128], X.t[:, kc, :]) for kc in range(8)],
                   [wqkv_b, X], [pa])
                P_ = pre[s][ch]
                act(lambda pa=pa, P_=P_: A.copy(out=P_.t[:, 3:515], in_=pa.ap), reads=[pa], writes=[P_])
                if n + 1 < NTILE:
                    Pn = pre[1 - s][ch]
                    pool(lambda P_=P_, Pn=Pn: G.tensor_copy(out=Pn.t[:, 0:3], in_=P_.t[:, 512:515]), reads=[P_], writes=[Pn])
            yield
            for j in (3, 2, 1, 0):
                for ch in range(4):
                    P_ = pre[s][ch]; C_ = cacc[ch]
                    if j == 3:
                        dve(lambda P_=P_, C_=C_, ch=ch, j=j: V.tensor_scalar(out=C_.t[:], in0=P_.t[:, j:j + 512],
                            scalar1=convw_s.t[:, ch, j:j + 1], scalar2=None, op0=ALU.mult), reads=[P_, convw_s], writes=[C_])
                    else:
                        dve(lambda P_=P_, C_=C_, ch=ch, j=j: V.scalar_tensor_tensor(out=C_.t[:], in0=P_.t[:, j:j + 512],
                            scalar=convw_s.t[:, ch, j:j + 1], in1=C_.t[:], op0=ALU.mult, op1=ALU.add),
                            reads=[P_, convw_s, C_], writes=[C_])
            for i in range(2):
                act(lambda i=i: A.activation(out=qs[i].t[:], in_=cacc[i].t[:], func=AF.Silu), reads=[cacc[i]], writes=[qs[i]])
            for h in range(2):
                act(lambda h=h: A.activation(out=vT[h].t[:], in_=cacc[2 + h].t[:], func=AF.Silu), reads=[cacc[2 + h]], writes=[vT[h]])
            yield
            for c in range(4):
                pz = pzt[c % 2]
                mm(pz.ap, [(X.t[:, kc, c * 128:(c + 1) * 128], wzba_b.t[:, kc, 0:260]) for kc in range(8)], [wzba_b, X], [pz])
                act(lambda pz=pz, c=c: A.activation(out=zs.t[:, c, :], in_=pz.bank[:, 0:256], func=AF.Silu), reads=[pz], writes=[zs])
                act(lambda pz=pz, c=c: A.copy(out=ba.t[:, c, :], in_=pz.bank[:, 256:260]), reads=[pz], writes=[ba])
            yield
            for c in range(4):
                for h in range(2):
                    pool(lambda c=c, h=h: G.tensor_tensor(out=nwz.t[:, c, h, :], in0=zs.t[:, c, h * 128:(h + 1) * 128],
                                                          in1=normw_s.t[:], op=ALU.mult), reads=[zs, normw_s], writes=[nwz])
            yield
            for i in range(2):
                act(lambda i=i: A.activation(out=sq[i].t[:], in_=qs[i].t[:], func=AF.Square), reads=[qs[i]], writes=[sq[i]])
            for i in range(2):
                pa = paA[i]
                mm(pa.ap, [(onesb.t[:], sq[i].t[:])], [onesb, sq[i]], [pa])
                act(lambda i=i, pa=pa: A.activation(out=lnr[i].t[:], in_=pa.ap, func=AF.Ln, bias=1e-6), reads=[pa], writes=[lnr[i]])
            for i in range(2):
                act(lambda i=i: A.activation(out=rr[i].t[:], in_=lnr[i].t[:], func=AF.Exp, scale=-0.5), reads=[lnr[i]], writes=[rr[i]])
            dve(lambda: V.scalar_tensor_tensor(out=qnT.t[:], in0=qs[0].t[:], scalar=float(128 ** -0.5), in1=rr[0].t[:],
                                               op0=ALU.mult, op1=ALU.mult), reads=[qs[0], rr[0]], writes=[qnT])
            dve(lambda: V.tensor_tensor(out=knT.t[:], in0=qs[1].t[:], in1=rr[1].t[:], op=ALU.mult), reads=[qs[1], rr[1]], writes=[knT])
            yield
            act(lambda: A.activation(out=eb.t[:], in_=ba.t[:, :, 0:2], func=AF.Exp, scale=-1.0), reads=[ba], writes=[eb])
            dve(lambda: V.tensor_scalar(out=eb.t[:], in0=eb.t[:], scalar1=1.0, scalar2=None, op0=ALU.add), reads=[eb], writes=[eb])
            dve(lambda: V.reciprocal(out=beta.t[:], in_=eb.t[:]), reads=[eb], writes=[beta])
            dve(lambda: V.tensor_scalar(out=nbeta.t[:], in0=beta.t[:], scalar1=-1.0, scalar2=None, op0=ALU.mult), reads=[beta], writes=[nbeta])
            dve(lambda: V.tensor_tensor(out=t1.t[:], in0=ba.t[:, :, 2:4], in1=dtb8.t[:], op=ALU.add), reads=[ba, dtb8], writes=[t1])
            act(lambda: A.activation(out=e1.t[:], in_=t1.t[:], func=AF.Exp), reads=[t1], writes=[e1])
            act(lambda: A.activation(out=l1.t[:], in_=e1.t[:], func=AF.Ln, bias=1.0), reads=[e1], writes=[l1])
            dve(lambda: V.tensor_tensor(out=gt.t[:], in0=l1.t[:], in1=negA8.t[:], op=ALU.mult), reads=[l1, negA8], writes=[gt])

            yield
            yield

        def stageX(n, c):
            cs = slice(c * 128, (c + 1) * 128)
            pe(lambda: PE.transpose(pT_k.ap, knT.t[:, cs], identb.t[:]), reads=[knT, identb], writes=[pT_k])
            for h in range(2):
                pe(lambda h=h: PE.transpose(pT_v[h].ap, vT[h].t[:, cs], identb.t[:]), reads=[vT[h], identb], writes=[pT_v[h]])
            mm(b6[:, 256:258], [(U.t[:], gt.t[:, c, :])], [U, gt], [pgcc])
            mm(b6[:, 258:260], [(onesf.t[:], gt.t[:, c, :])], [onesf, gt], [pgcc])
            mm(pKK.ap, [(knT.t[:, cs], knT.t[:, cs])], [knT], [pKK])
            mm(pQK.ap, [(qnT.t[:, cs], knT.t[:, cs])], [qnT, knT], [pQK])
            act(lambda: A.copy(out=gs.t[:], in_=pgcc.ap), reads=[pgcc], writes=[gs])
            act(lambda: A.activation(out=exa.t[:], in_=gs.t[:], func=AF.Exp), reads=[gs], writes=[exa])
            for h in range(2):
                act(lambda h=h: A.activation(out=ekd.t[:, h:h + 1], in_=gs.t[:, h:h + 1], func=AF.Exp, scale=-1.0,
                                             bias=gs.t[:, 2 + h:3 + h]), reads=[gs], writes=[ekd])
            for h in range(2):
                dve(lambda h=h: V.tensor_scalar(out=vb[h].t[:], in0=pT_v[h].ap, scalar1=beta.t[:, c, h:h + 1], scalar2=None,
                                                op0=ALU.mult), reads=[pT_v[h], beta], writes=[vb[h]])
                dve(lambda h=h: V.tensor_scalar(out=kbg[h].t[:], in0=pT_k.ap, scalar1=beta.t[:, c, h:h + 1], scalar2=exa.t[:, h:h + 1],
                                                op0=ALU.mult, op1=ALU.mult), reads=[pT_k, beta, exa], writes=[kbg[h]])
                dve(lambda h=h: V.tensor_scalar(out=kd[h].t[:], in0=pT_k.ap, scalar1=ekd.t[:, h:h + 1], scalar2=None, op0=ALU.mult),
                    reads=[pT_k, ekd], writes=[kd[h]])
                pool(lambda h=h: G.tensor_scalar(out=gB[h].t[:], in0=onesf.t[:], scalar1=gt.t[:, c, h:h + 1], scalar2=None,
                                                 op0=ALU.mult), reads=[onesf, gt], writes=[gB[h]])
            yield
            for h in range(2):
                mm(b2[:, h * 128:(h + 1) * 128], [(U.t[:], gB[h].t[:]), (gB[h].t[:], Uneg.t[:]), (identf.t[:], MASKS.t[:])],
                   [U, Uneg, gB[h], identf, MASKS], [pD])
                mm(b2[:, 256 + h * 128:256 + (h + 1) * 128], [(gB[h].t[:], U.t[:])], [gB[h], U], [pD])
            act(lambda: A.activation(out=dec.t[:], in_=pD.ap, func=AF.Exp), reads=[pD], writes=[dec])
            for h in range(2):
                hs = slice(h * 128, (h + 1) * 128)
                dve(lambda h=h, hs=hs: V.scalar_tensor_tensor(out=NA[h].t[:], in0=pKK.ap, scalar=nbeta.t[:, c, h:h + 1],
                    in1=dec.t[:, hs], op0=ALU.mult, op1=ALU.mult), reads=[pKK, nbeta, dec], writes=[NA[h]])
                pool(lambda h=h: G.tensor_tensor(out=PP[h][0].t[:, 128:256], in0=NA[h].t[:], in1=BDm.t[:], op=ALU.mult),
                     reads=[NA[h], BDm], writes=[PP[h][0]])
                pool(lambda h=h: G.tensor_tensor(out=MoffT[h].t[:], in0=NA[h].t[:], in1=OFFm.t[:], op=ALU.mult),
                     reads=[NA[h], OFFm], writes=[MoffT[h]])
                pool(lambda h=h, hs=hs: G.tensor_tensor(out=decc[h].t[:], in0=dec.t[:, hs], in1=identf.t[:], op=ALU.add),
                     reads=[dec, identf], writes=[decc[h]])
                dve(lambda h=h: V.tensor_tensor(out=aqk[h].t[:], in0=pQK.ap, in1=decc[h].t[:], op=ALU.mult),
                    reads=[pQK, decc[h]], writes=[aqk[h]])
                pool(lambda h=h: G.tensor_tensor(out=qdT[h].t[:], in0=qnT.t[:, cs], in1=dec.t[:, 256 + h * 128:256 + (h + 1) * 128],
                                                 op=ALU.mult), reads=[qnT, dec], writes=[qdT[h]])
            for h in range(2):
                pe(lambda h=h: PE.transpose(pT_a[h].ap, aqk[h].t[:], identb.t[:]), reads=[aqk[h], identb], writes=[pT_a[h]])
            for h in range(2):
                act(lambda h=h: A.copy(out=aqkT[h].t[:], in_=pT_a[h].ap), reads=[pT_a[h]], writes=[aqkT[h]])
            yield
            def ev(h, dst_tile, dst_ap, src_pt, src_ap):
                if h == 0:
                    act(lambda: A.copy(out=dst_ap, in_=src_ap), reads=[src_pt], writes=[dst_tile])
                else:
                    dve(lambda: V.tensor_copy(out=dst_ap, in_=src_ap), reads=[src_pt], writes=[dst_tile])
            for h in range(2):
                X0 = PP[h][0]
                bk = pi_sq[h].bank
                pe(lambda: PE.matmul(bk[:, 0:128], lhsT=X0.t[:, 128:256], rhs=identf.t[:], start=True, stop=True),
                   reads=[X0, identf], writes=[pi_sq[h]])
                mm(bk[:, 256:384], [(X0.t[:, 128:256], identf.t[:]), (identf.t[:], identf.t[:])], [X0, identf], [pi_sq[h]])
            for h in range(2):
                X0 = PP[h][0]
                bk = pi_sq[h].bank
                ev(h, X0, X0.t[:, 0:128], pi_sq[h], bk[:, 0:128])
                ev(h, X0, X0.t[:, 256:384], pi_sq[h], bk[:, 256:384])
            yield
            for k in range(1, 7):
                for h in range(2):
                    Ps = PP[h][(k - 1) % 2]
                    bk = pi_sq[h].bank
                    if k <= 4:
                        pe(lambda: PE.matmul(bk[:, 0:128], lhsT=Ps.t[:, 128:256], rhs=Ps.t[:, 0:128], start=True, stop=True),
                           reads=[Ps], writes=[pi_sq[h]])
                    if k <= 5:
                        pe(lambda: PE.matmul(bk[:, 128:256], lhsT=Ps.t[:, 0:128], rhs=Ps.t[:, 128:256], start=True, stop=True),
                           reads=[Ps], writes=[pi_sq[h]])
                    if k >= 2:
                        mm(bk[:, 256:384], [(Ps.t[:, 128:256], Ps.t[:, 256:384]), (identf.t[:], Ps.t[:, 256:384])], [Ps, identf], [pi_sq[h]])
                    else:
                        mm(bk[:, 256:384], [(identf.t[:], Ps.t[:, 256:384])], [Ps, identf], [pi_sq[h]])
                for h in range(2):
                    Pd = PP[h][k % 2]
                    bk = pi_sq[h].bank
                    lo = 0 if k <= 4 else (128 if k == 5 else 256)
                    ev(h, Pd, Pd.t[:, lo:384], pi_sq[h], bk[:, lo:384])
                yield
            yield
            for h in range(2):
                Pf = PP[h][0]
                bk = pi_sq[h].bank
                pe(lambda: PE.matmul(bk[:, 0:128], lhsT=Pf.t[:, 256:384], rhs=identf.t[:], start=True, stop=True),
                   reads=[Pf, identf], writes=[pi_sq[h]])
                pe(lambda: PE.matmul(bk[:, 128:256], lhsT=MoffT[h].t[:], rhs=Pf.t[:, 256:384], start=True, stop=True),
                   reads=[MoffT[h], Pf], writes=[pi_sq[h]])
            for h in range(2):
                ev(h, TdY[h], TdY[h].t[:], pi_sq[h], pi_sq[h].bank[:, 0:256])
            for h in range(2):
                Pf = PP[h][0]
                bk = pi_sq[h].bank
                mm(bk[:, 256:384], [(TdY[h].t[:, 0:128], TdY[h].t[:, 128:256]), (identf.t[:], Pf.t[:, 256:384])],
                   [TdY[h], Pf, identf], [pi_sq[h]])
            for h in range(2):
                ev(h, TTb[h], TTb[h].t[:], pi_sq[h], pi_sq[h].bank[:, 256:384])
            yield
            for h in range(2):
                hs = slice(h * 128, (h + 1) * 128)
                pe(lambda h=h, hs=hs: PE.matmul(b2[:, hs], lhsT=TTb[h].t[:], rhs=vb[h].t[:], start=True, stop=True),
                   reads=[TTb[h], vb[h]], writes=[pu])
            for h in range(2):
                pe(lambda h=h: PE.matmul(b2[:, 256 + h * 128:256 + (h + 1) * 128], lhsT=kbg[h].t[:], rhs=TTb[h].t[:], start=True, stop=True),
                   reads=[TTb[h], kbg[h]], writes=[pwT])
            act(lambda: A.copy(out=u_s.t[:], in_=pu.ap), reads=[pu], writes=[u_s])
            act(lambda: A.copy(out=wT_s.t[:], in_=pwT.ap), reads=[pwT], writes=[wT_s])
            yield

        def stageY(n, c):
            yield
            og_t = ogt[c % 2]
            for h in range(2):
                hs = slice(h * 128, (h + 1) * 128)
                pe(lambda h=h, hs=hs: PE.matmul(p_wS[h].ap, lhsT=wT_s.t[:, hs], rhs=Sb[h].t[:], start=True, stop=True),
                   reads=[wT_s, Sb[h]], writes=[p_wS[h]])
                dve(lambda h=h, hs=hs: V.tensor_tensor(out=vn[h].t[:], in0=u_s.t[:, hs], in1=p_wS[h].ap, op=ALU.subtract),
                    reads=[u_s, p_wS[h]], writes=[vn[h]])
            yield
            for h in range(2):
                mm(p_o[h].ap, [(qdT[h].t[:], Sb[h].t[:]), (aqkT[h].t[:], vn[h].t[:])], [qdT[h], Sb[h], aqkT[h], vn[h]], [p_o[h]])
                mm(p_Sn[h].ap, [(kd[h].t[:], vn[h].t[:])], [kd[h], vn[h]], [p_Sn[h]])
            yield
            for h in range(2):
                dve(lambda h=h: V.scalar_tensor_tensor(out=S[h].t[:], in0=S[h].t[:], scalar=exa.t[:, 2 + h:3 + h], in1=p_Sn[h].ap,
                                                       op0=ALU.mult, op1=ALU.add), reads=[S[h], exa, p_Sn[h]], writes=[S[h]])
                act(lambda h=h: A.copy(out=Sb[h].t[:], in_=S[h].t[:]), reads=[S[h]], writes=[Sb[h]])
            yield
            for h in range(2):
                act(lambda h=h: A.activation(out=junk[h].t[:], in_=p_o[h].ap, func=AF.Square, accum_out=ss[h].t[:, 0:1]),
                    reads=[p_o[h]], writes=[junk[h], ss[h]])
            for h in range(2):
                act(lambda h=h: A.activation(out=lns[h].t[:], in_=ss[h].t[:], func=AF.Ln, scale=1.0 / 128.0, bias=1e-6),
                    reads=[ss[h]], writes=[lns[h]])
            for h in range(2):
                act(lambda h=h: A.activation(out=rinv[h].t[:], in_=lns[h].t[:], func=AF.Exp, scale=-0.5), reads=[lns[h]], writes=[rinv[h]])
            for h in range(2):
                dve(lambda h=h, og_t=og_t: V.scalar_tensor_tensor(out=og_t.t[:, h * 128:(h + 1) * 128], in0=p_o[h].ap,
                    scalar=rinv[h].t[:, 0:1], in1=nwz.t[:, c, h, :], op0=ALU.mult, op1=ALU.mult),
                    reads=[p_o[h], rinv[h], nwz], writes=[og_t])
            r0 = n * 512 + c * 128
            fw.dma("sp", og[r0:r0 + 128, :], og_t.t[:], f"og{c % 2}", reads=[og_t.r])

            yield

        def run(main, side, main_ctx, side_ctx, ratio=2):
            done_m = main is None
            done_s = side is None
            while not (done_m and done_s):
                if not done_m:
                    for _ in range(ratio):
                        CTX.update(main_ctx)
                        try:
                            next(main)
                        except StopIteration:
                            done_m = True
                            break
                if not done_s:
                    CTX.update(side_ctx)
                    try:
                        next(side)
                    except StopIteration:
                        done_s = True

        load_x(0)
        prevY = None
        prev_ctx = None
        gidx = 0
        for n in range(NTILE):
            run(stageA(n), prevY, {'tile': n}, prev_ctx)
            prevY = None
            for c in range(4):
                ctx = {'par': gidx, 'tile': n}
                run(stageX(n, c), prevY, ctx, prev_ctx)
                prevY = stageY(n, c)
                prev_ctx = dict(ctx)
                gidx += 1
        run(None, prevY, None, prev_ctx)
        for k in ("og0", "og1"):
            if k in fw.dsem:
                nc.sync.wait_ge(fw.dsem[k], fw.dcnt[k])
        print("GDN instrs", fw.ninstr, "waits", fw.nwaits)
    return nc


ALPHA = float((2.0 * 2) ** 0.25)
MOE_GCH = 4
LN_EPS = 1e-5


class TL:
    def __init__(self, t, name, psum=False):
        self.t = t
        self.r = Res(name, psum=psum)


class KC:
    def __init__(self, nc, es):
        self.nc = nc
        self.es = es
        self.fw = FW(nc, es)
        self.V, self.A, self.G, self.PE = nc.vector, nc.scalar, nc.gpsimd, nc.tensor
        self.banks = []
        self._uid = 0

    def sb(self, name, shape, dt=F32):
        return TL(self.fw.sb(name, shape, dt), name)

    def bank(self, name, dt=F32):
        cols = 512 if dt == F32 else 1024
        return TL(self.fw.ps(name, [128, cols], dt), name, psum=True)

    def op(self, eng, fn, reads=(), writes=()):
        return self.fw.op(eng, fn, [x.r for x in reads], [x.r for x in writes])

    def dve(self, fn, reads=(), writes=()):
        return self.op("dve", fn, reads, writes)

    def act(self, fn, reads=(), writes=()):
        return self.op("act", fn, reads, writes)

    def pool(self, fn, reads=(), writes=()):
        return self.op("pool", fn, reads, writes)

    def pe(self, fn, reads=(), writes=()):
        return self.op("pe", fn, reads, writes)

    def mm(self, out_ap, pairs, reads, writes):
        PE = self.PE
        def f():
            n = len(pairs)
            last = None
            for i, (l, r) in enumerate(pairs):
                last = PE.matmul(out_ap, lhsT=l, rhs=r, start=(i == 0), stop=(i == n - 1))
            return last
        return self.pe(f, reads, writes)

    def dma(self, q, out, in_, slot, reads=(), writes=(), **kw):
        return self.fw.dma(q, out, in_, slot, [x.r for x in reads], [x.r for x in writes], **kw)

    def barrier(self):
        fw = self.fw
        for e in fw.engs:
            for k in list(fw.sem) + list(fw.dsem):
                v = fw.cnt[k] if k in fw.sem else fw.dcnt[k]
                if k == e or v == 0 or fw.known[e].get(k, 0) >= v:
                    continue
                fw.engs[e].wait_ge(fw._semobj(k), v)
                fw.known[e][k] = v

    def finish(self, slots):
        for k in slots:
            if k in self.fw.dsem:
                self.nc.sync.wait_ge(self.fw.dsem[k], self.fw.dcnt[k])

    def consts(self):
        G = self.G
        self.onesf = self.sb("onesf", [128, 128])
        self.identf = self.sb("identf", [128, 128])
        self.identb = self.sb("identb", [128, 128], BF16)
        for t_, v in ((self.onesf, 1.0), (self.identf, 0.0), (self.identb, 0.0)):
            self.pool(lambda: G.memset(t_.t[:], v), writes=[t_])
        for t_ in (self.identf, self.identb):
            self.pool(lambda: G.affine_select(out=t_.t[:], in_=t_.t[:], pattern=[[-1, 128]], compare_op=ALU.not_equal,
                                              fill=1.0, base=0, channel_multiplier=1), reads=[t_], writes=[t_])

    def ln_alloc(self):
        self.ln_sq = [self.sb(f"ln_sq{i}", [128, 512]) for i in range(2)]
        self.ln_mean = self.sb("ln_mean", [128, 512])
        self.ln_msq = self.sb("ln_msq", [128, 512])
        self.ln_var = self.sb("ln_var", [128, 512])
        self.ln_lnv = self.sb("ln_lnv", [128, 512])
        self.ln_rstd = self.sb("ln_rstd", [128, 512])
        self.ln_t = [self.sb(f"ln_t{i}", [128, 512]) for i in range(2)]

    def layernorm(self, y, ycols, gb, gi, bi, ps1, ps2, out_f=None, out_f_cols=None, out_b=None, out_b_cols=None, N=512):
        V, A, G, PE = self.V, self.A, self.G, self.PE
        onesf = self.onesf
        self.mm(ps1.t[:, 0:N], [(onesf.t[:], y.t[:, c, ycols]) for c in range(8)], [onesf, y], [ps1])
        for c in range(8):
            sq = self.ln_sq[c % 2]
            self.act(lambda: A.activation(out=sq.t[:, 0:N], in_=y.t[:, c, ycols], func=AF.Square), reads=[y], writes=[sq])
            self.pe(lambda: PE.matmul(ps2.t[:, 0:N], lhsT=onesf.t[:], rhs=sq.t[:, 0:N], start=(c == 0), stop=(c == 7)),
                    reads=[onesf, sq], writes=[ps2])
        mean, msq, var, lnv, rstd = self.ln_mean, self.ln_msq, self.ln_var, self.ln_lnv, self.ln_rstd
        self.act(lambda: A.activation(out=mean.t[:, 0:N], in_=ps1.t[:, 0:N], func=AF.Copy, scale=1.0 / 1024.0), reads=[ps1], writes=[mean])
        self.act(lambda: A.activation(out=msq.t[:, 0:N], in_=ps1.t[:, 0:N], func=AF.Square, scale=1.0 / 1024.0), reads=[ps1], writes=[msq])
        self.dve(lambda: V.scalar_tensor_tensor(out=var.t[:, 0:N], in0=ps2.t[:, 0:N], scalar=1.0 / 1024.0, in1=msq.t[:, 0:N],
                                                op0=ALU.mult, op1=ALU.subtract), reads=[ps2, msq], writes=[var])
        self.act(lambda: A.activation(out=lnv.t[:, 0:N], in_=var.t[:, 0:N], func=AF.Ln, bias=LN_EPS), reads=[var], writes=[lnv])
        self.act(lambda: A.activation(out=rstd.t[:, 0:N], in_=lnv.t[:, 0:N], func=AF.Exp, scale=-0.5), reads=[lnv], writes=[rstd])
        for c in range(8):
            t = self.ln_t[c % 2]
            self.dve(lambda: V.tensor_tensor(out=t.t[:, 0:N], in0=y.t[:, c, ycols], in1=mean.t[:, 0:N], op=ALU.subtract), reads=[y, mean], writes=[t])
            self.dve(lambda: V.tensor_tensor(out=t.t[:, 0:N], in0=t.t[:, 0:N], in1=rstd.t[:, 0:N], op=ALU.mult), reads=[t, rstd], writes=[t])
            if out_f is not None:
                self.act(lambda: A.activation(out=out_f.t[:, c, out_f_cols], in_=t.t[:, 0:N], func=AF.Identity,
                                              scale=gb.t[:, gi, c:c + 1], bias=gb.t[:, bi, c:c + 1]), reads=[t, gb], writes=[out_f])
            if out_b is not None:
                self.pool(lambda: G.tensor_scalar(out=out_b.t[:, c, out_b_cols], in0=t.t[:, 0:N], scalar1=gb.t[:, gi, c:c + 1],
                                                  scalar2=gb.t[:, bi, c:c + 1], op0=ALU.mult, op1=ALU.add), reads=[t, gb], writes=[out_b])

    def glu(self, xb, acc, NT, wg, wu, wd, F, psA, psB, psD, gate=None, GCH=4):
        V, A, G, PE = self.V, self.A, self.G, self.PE
        if not hasattr(self, "glu_w"):
            self.glu_w = [(self.sb(f"glu_wg{i}", [128, 8, GCH * 128], BF16), self.sb(f"glu_wu{i}", [128, 8, GCH * 128], BF16),
                           self.sb(f"glu_wd{i}", [128, GCH, 1024], BF16)) for i in range(3)]
            self.glu_h = [self.sb(f"glu_h{i}", [128, GCH, 512], BF16) for i in range(2)]
            self.glu_sg = [self.sb(f"glu_sg{i}", [128, 512]) for i in range(2)]
            self.glu_tt = [self.sb(f"glu_tt{i}", [128, 512]) for i in range(2)]
            self.glu_cnt = 0
        wgr = wg.rearrange("(c p) f -> p c f", p=128)
        wur = wu.rearrange("(c p) f -> p c f", p=128)
        groups = []
        f0 = 0
        while f0 < F:
            nch = min(GCH, (F - f0) // 128)
            groups.append((f0, nch))
            f0 += nch * 128
        it = 0
        base = self.glu_cnt
        self.glu_cnt += len(groups)

        def load(gi_):
            f0, nch = groups[gi_]
            s = (base + gi_) % 3
            Wg, Wu, Wd = self.glu_w[s]
            fw_ = nch * 128
            self.dma("pool", Wg.t[:, :, 0:fw_], wgr[:, :, f0:f0 + fw_], f"glw{s}", writes=[Wg])
            self.dma("pool", Wu.t[:, :, 0:fw_], wur[:, :, f0:f0 + fw_], f"glw{s}", writes=[Wu])
            self.dma("pool", Wd.t[:, 0:nch, :], wd[f0:f0 + fw_, :].rearrange("(c p) o -> p c o", p=128), f"glw{s}", writes=[Wd])
        load(0)
        pending = None
        for gi_, (f0, nch) in enumerate(groups):
            if gi_ + 1 < len(groups):
                load(gi_ + 1)
            Wg, Wu, Wd = self.glu_w[(base + gi_) % 3]
            for tt in range(NT):
                ts_ = slice(tt * 512, (tt + 1) * 512)
                H = self.glu_h[it % 2]
                it += 1
                for fc in range(nch):
                    pa = psA[fc % 2]
                    pb = psB[fc % 2]
                    sg = self.glu_sg[fc % 2]
                    t2 = self.glu_tt[fc % 2]
                    self.mm(pa.t[:], [(Wg.t[:, kc, fc * 128:(fc + 1) * 128], xb.t[:, kc, ts_]) for kc in range(8)], [Wg, xb], [pa])
                    self.mm(pb.t[:], [(Wu.t[:, kc, fc * 128:(fc + 1) * 128], xb.t[:, kc, ts_]) for kc in range(8)], [Wu, xb], [pb])
                    self.act(lambda: A.activation(out=sg.t[:], in_=pa.t[:], func=AF.Silu), reads=[pa], writes=[sg])
                    if gate is None:
                        self.dve(lambda: V.tensor_tensor(out=H.t[:, fc, :], in0=sg.t[:], in1=pb.t[:], op=ALU.mult), reads=[sg, pb], writes=[H])
                    else:
                        self.dve(lambda: V.tensor_tensor(out=t2.t[:], in0=sg.t[:], in1=pb.t[:], op=ALU.mult), reads=[sg, pb], writes=[t2])
                        self.pool(lambda: G.tensor_tensor(out=H.t[:, fc, :], in0=t2.t[:], in1=gate.t[:, ts_], op=ALU.mult), reads=[t2, gate], writes=[H])
                def down(Wd=Wd, H=H, nch=nch, ts_=ts_):
                    for oc in range(8):
                        pd = psD[oc % 2]
                        self.mm(pd.t[:], [(Wd.t[:, fc, oc * 128:(oc + 1) * 128], H.t[:, fc, :]) for fc in range(nch)], [Wd, H], [pd])
                        self.dve(lambda: V.tensor_tensor(out=acc.t[:, oc, ts_], in0=acc.t[:, oc, ts_], in1=pd.t[:], op=ALU.add), reads=[acc, pd], writes=[acc])
                if pending is not None:
                    pending()
                pending = down
        if pending is not None:
            pending()


def build_l2(nc, TC):
    NT = TC // 512
    xT = nc.dram_tensor("xT", [1024, TC], F32, kind="ExternalInput").ap()
    ogT = nc.dram_tensor("ogT", [1024, TC], F32, kind="ExternalInput").ap()
    wo = nc.dram_tensor("wo", [1024, 1024], F32, kind="ExternalInput").ap()
    lnp = nc.dram_tensor("lnp", [128, 4, 8], F32, kind="ExternalInput").ap()
    wg = nc.dram_tensor("wg", [1024, 2816], F32, kind="ExternalInput").ap()
    wu = nc.dram_tensor("wu", [1024, 2816], F32, kind="ExternalInput").ap()
    wd = nc.dram_tensor("wd", [2816, 1024], F32, kind="ExternalInput").ap()
    outT = nc.dram_tensor("outT", [1024, TC], F32, kind="ExternalOutput").ap()
    with ExitStack() as es:
        k = KC(nc, es)
        V, A, G, PE = k.V, k.A, k.G, k.PE
        k.consts()
        banks = [k.bank(f"bk{i}") for i in range(8)]
        gb = k.sb("gb", [128, 4, 8])
        k.dma("sp", gb.t[:], lnp, "c0", writes=[gb])
        acc = k.sb("acc", [128, 8, TC])
        x1b = k.sb("x1b", [128, 8, TC], BF16)
        xTr = xT.rearrange("(c p) t -> p c t", p=128)
        ogTr = ogT.rearrange("(c p) t -> p c t", p=128)
        outTr = outT.rearrange("(c p) t -> p c t", p=128)
        with ExitStack() as es2:
            k2es = es2
            wo_b = TL(es2.enter_context(nc.sbuf_tensor("wo_b", [128, 8, 1024], BF16)), "wo_b")
            og_b = TL(es2.enter_context(nc.sbuf_tensor("og_b", [128, 8, 512], BF16)), "og_b")
            xf = TL(es2.enter_context(nc.sbuf_tensor("xf", [128, 8, 512], F32)), "xf")
            y = TL(es2.enter_context(nc.sbuf_tensor("y", [128, 8, 512], F32)), "y")
            k.es = es2
            k.fw.es = es2
            k.ln_alloc()
            k.es = es
            k.fw.es = es
            k.dma("pool", wo_b.t[:], wo.rearrange("(c p) n -> p c n", p=128), "c1", writes=[wo_b])
            for tt in range(NT):
                ts_ = slice(tt * 512, (tt + 1) * 512)
                k.dma("pool", og_b.t[:], ogTr[:, :, ts_], "og", writes=[og_b])
                k.dma("sp", xf.t[:], xTr[:, :, ts_], "xf", writes=[xf])
                for oc in range(8):
                    pb = banks[oc % 2]
                    k.mm(pb.t[:], [(wo_b.t[:, kc, oc * 128:(oc + 1) * 128], og_b.t[:, kc, :]) for kc in range(8)], [wo_b, og_b], [pb])
                    k.dve(lambda: V.scalar_tensor_tensor(out=y.t[:, oc, :], in0=xf.t[:, oc, :], scalar=ALPHA, in1=pb.t[:],
                                                         op0=ALU.mult, op1=ALU.add), reads=[xf, pb], writes=[y])
                k.layernorm(y, slice(0, 512), gb, 0, 1, banks[2], banks[3], out_f=xf, out_f_cols=slice(0, 512), out_b=x1b, out_b_cols=ts_)
                for c in range(8):
                    k.act(lambda: A.activation(out=acc.t[:, c, ts_], in_=xf.t[:, c, :], func=AF.Copy, scale=ALPHA), reads=[xf], writes=[acc])
            k.barrier()
        k.glu(x1b, acc, NT, wg, wu, wd, 2816, banks[0:2], banks[2:4], banks[4:6])
        k.barrier()
        k.ln_alloc2 = True
        k.ln_sq = [k.sb(f"l2_sq{i}", [128, 512]) for i in range(2)]
        k.ln_mean = k.sb("l2_mean", [128, 512]); k.ln_msq = k.sb("l2_msq", [128, 512]); k.ln_var = k.sb("l2_var", [128, 512])
        k.ln_lnv = k.sb("l2_lnv", [128, 512]); k.ln_rstd = k.sb("l2_rstd", [128, 512])
        k.ln_t = [k.sb(f"l2_t{i}", [128, 512]) for i in range(2)]
        for tt in range(NT):
            ts_ = slice(tt * 512, (tt + 1) * 512)
            k.layernorm(acc, ts_, gb, 2, 3, banks[6], banks[7], out_f=acc, out_f_cols=ts_)
            k.dma("sp", outTr[:, :, ts_], acc.t[:, :, ts_], f"out{tt % 2}", reads=[acc])
        k.finish(["out0", "out1"])
        print("L2 instrs", k.fw.ninstr, "waits", k.fw.nwaits)
    return nc


def build_l3b(nc, TC):
    NT = TC // 512
    NB = TC // 128
    x2T = nc.dram_tensor("x2T", [1024, TC], F32, kind="ExternalInput").ap()
    wr = nc.dram_tensor("wr", [1024, 8], F32, kind="ExternalInput").ap()
    lnp = nc.dram_tensor("lnp", [128, 2, 8], F32, kind="ExternalInput").ap()
    mwg = nc.dram_tensor("mwg", [8, 1024, 3584], F32, kind="ExternalInput").ap()
    mwu = nc.dram_tensor("mwu", [8, 1024, 3584], F32, kind="ExternalInput").ap()
    mwd = nc.dram_tensor("mwd", [8, 3584, 1024], F32, kind="ExternalInput").ap()
    outT = nc.dram_tensor("outT", [1024, TC], F32, kind="ExternalOutput").ap()
    with ExitStack() as es:
        k = KC(nc, es)
        V, A, G, PE = k.V, k.A, k.G, k.PE
        k.consts()
        banks = [k.bank(f"bk{i}") for i in range(8)]
        gb = k.sb("gb", [128, 2, 8])
        k.dma("sp", gb.t[:], lnp, "c0", writes=[gb])
        acc = k.sb("acc", [128, 8, TC])
        x2b = k.sb("x2b", [128, 8, TC], BF16)
        gates = k.sb("gates", [128, NB, 8])
        x2Tr = x2T.rearrange("(c p) t -> p c t", p=128)
        outTr = outT.rearrange("(c p) t -> p c t", p=128)
        k.dma("pool", x2b.t[:], x2Tr, "c1", writes=[x2b])
        with ExitStack() as es2:
            k.es = es2; k.fw.es = es2
            xf = [k.sb(f"xf{i}", [128, 8, 512]) for i in range(2)]
            wr_s = k.sb("wr_s", [128, 8, 8])
            lg = [k.sb(f"lg{i}", [128, 8]) for i in range(2)]
            m1 = [k.sb(f"m1{i}", [128, 1]) for i in range(2)]
            nm1 = [k.sb(f"nm1{i}", [128, 1]) for i in range(2)]
            eq1 = [k.sb(f"eq1{i}", [128, 8]) for i in range(2)]
            l2 = [k.sb(f"l2{i}", [128, 8]) for i in range(2)]
            m2 = [k.sb(f"m2{i}", [128, 1]) for i in range(2)]
            sel = [k.sb(f"sel{i}", [128, 8]) for i in range(2)]
            ex = [k.sb(f"ex{i}", [128, 8]) for i in range(2)]
            e2 = [k.sb(f"e2{i}", [128, 8]) for i in range(2)]
            ssum = [k.sb(f"ssum{i}", [128, 1]) for i in range(2)]
            rs = [k.sb(f"rs{i}", [128, 1]) for i in range(2)]
            k.es = es; k.fw.es = es
            k.dma("sp", wr_s.t[:], wr.rearrange("(c p) e -> p c e", p=128), "c0", writes=[wr_s])
            for tt in range(NT):
                ts_ = slice(tt * 512, (tt + 1) * 512)
                X = xf[tt % 2]
                k.dma("sp", X.t[:], x2Tr[:, :, ts_], f"xf{tt % 2}", writes=[X])
                for c in range(8):
                    k.act(lambda: A.activation(out=acc.t[:, c, ts_], in_=X.t[:, c, :], func=AF.Copy, scale=ALPHA), reads=[X], writes=[acc])
                for bl in range(4):
                    b_ = tt * 4 + bl
                    i = b_ % 2
                    pb = banks[i]
                    k.mm(pb.t[:, 0:8], [(X.t[:, kc, bl * 128:(bl + 1) * 128], wr_s.t[:, kc, :]) for kc in range(8)], [X, wr_s], [pb])
                    k.act(lambda: A.copy(out=lg[i].t[:], in_=pb.t[:, 0:8]), reads=[pb], writes=[lg[i]])
                    k.dve(lambda: V.tensor_reduce(out=m1[i].t[:], in_=lg[i].t[:], axis=AX.X, op=ALU.max), reads=[lg[i]], writes=[m1[i]])
                    k.act(lambda: A.activation(out=nm1[i].t[:], in_=m1[i].t[:], func=AF.Copy, scale=-1.0), reads=[m1[i]], writes=[nm1[i]])
                    k.dve(lambda: V.tensor_scalar(out=eq1[i].t[:], in0=lg[i].t[:], scalar1=m1[i].t[:, 0:1], scalar2=None, op0=ALU.is_equal),
                          reads=[lg[i], m1[i]], writes=[eq1[i]])
                    k.dve(lambda: V.scalar_tensor_tensor(out=l2[i].t[:], in0=eq1[i].t[:], scalar=-1e30, in1=lg[i].t[:], op0=ALU.mult, op1=ALU.add),
                          reads=[eq1[i], lg[i]], writes=[l2[i]])
                    k.dve(lambda: V.tensor_reduce(out=m2[i].t[:], in_=l2[i].t[:], axis=AX.X, op=ALU.max), reads=[l2[i]], writes=[m2[i]])
                    k.dve(lambda: V.tensor_scalar(out=sel[i].t[:], in0=lg[i].t[:], scalar1=m2[i].t[:, 0:1], scalar2=None, op0=ALU.is_ge),
                          reads=[lg[i], m2[i]], writes=[sel[i]])
                    k.act(lambda: A.activation(out=ex[i].t[:], in_=lg[i].t[:], func=AF.Exp, bias=nm1[i].t[:, 0:1]), reads=[lg[i], nm1[i]], writes=[ex[i]])
                    k.dve(lambda: V.tensor_tensor(out=e2[i].t[:], in0=ex[i].t[:], in1=sel[i].t[:], op=ALU.mult), reads=[ex[i], sel[i]], writes=[e2[i]])
                    k.dve(lambda: V.tensor_reduce(out=ssum[i].t[:], in_=e2[i].t[:], axis=AX.X, op=ALU.add), reads=[e2[i]], writes=[ssum[i]])
                    k.dve(lambda: V.reciprocal(out=rs[i].t[:], in_=ssum[i].t[:]), reads=[ssum[i]], writes=[rs[i]])
                    k.dve(lambda: V.tensor_scalar(out=gates.t[:, b_, :], in0=e2[i].t[:], scalar1=rs[i].t[:, 0:1], scalar2=None, op0=ALU.mult),
                          reads=[e2[i], rs[i]], writes=[gates])
            k.barrier()
        with ExitStack() as es3:
            k.es = es3; k.fw.es = es3
            gcolB = [k.sb(f"gcolB{i}", [128, 128]) for i in range(2)]
            gbc = [k.sb(f"gbc{i}", [128, TC], BF16) for i in range(2)]
            for e in range(8):
                Gb = gbc[e % 2]
                for b_ in range(NB):
                    gc_ = gcolB[b_ % 2]
                    pb = banks[6 + (b_ // 4) % 2]
                    k.act(lambda: A.activation(out=gc_.t[:], in_=k.onesf.t[:], func=AF.Copy, scale=gates.t[:, b_, e:e + 1]), reads=[k.onesf, gates], writes=[gc_])
                    k.mm(pb.t[:, (b_ % 4) * 128:(b_ % 4 + 1) * 128], [(gc_.t[:], k.identf.t[:])], [gc_, k.identf], [pb])
                    if b_ % 4 == 3:
                        t0 = (b_ // 4) * 512
                        k.act(lambda: A.copy(out=Gb.t[:, t0:t0 + 512], in_=pb.t[:]), reads=[pb], writes=[Gb])
                k.glu(x2b, acc, NT, mwg[e], mwu[e], mwd[e], 3584, banks[0:2], banks[2:4], banks[4:6], gate=Gb, GCH=MOE_GCH)
            k.barrier()
            k.es = es; k.fw.es = es
        k.barrier()
        k.ln_alloc()
        for tt in range(NT):
            ts_ = slice(tt * 512, (tt + 1) * 512)
            k.layernorm(acc, ts_, gb, 0, 1, banks[6], banks[7], out_f=acc, out_f_cols=ts_)
            k.dma("sp", outTr[:, :, ts_], acc.t[:, :, ts_], f"out{tt % 2}", reads=[acc])
        k.finish(["out0", "out1"])
        print("L3b instrs", k.fw.ninstr, "waits", k.fw.nwaits)
    return nc


class _Stop(Exception):
    pass


def build_l3a(nc, TC):
    STOP3 = 99
    NT = TC // 512
    TE = TC + 128
    NBE = TE // 128
    x1e = nc.dram_tensor("x1e", [1024, TE], F32, kind="ExternalInput").ap()
    wq = nc.dram_tensor("wq", [1024, 1024], F32, kind="ExternalInput").ap()
    bq = nc.dram_tensor("bq", [128, 8], F32, kind="ExternalInput").ap()
    wkv = nc.dram_tensor("wkv", [1024, 256], F32, kind="ExternalInput").ap()
    bkv = nc.dram_tensor("bkv", [128, 2], F32, kind="ExternalInput").ap()
    wo = nc.dram_tensor("wo", [1024, 1024], F32, kind="ExternalInput").ap()
    biasT = nc.dram_tensor("biasT", [128, 8, 512], F32, kind="ExternalInput").ap()
    maskT = nc.dram_tensor("maskT", [128, 512], F32, kind="ExternalInput").ap()
    fmT = nc.dram_tensor("fmT", [128, 512], F32, kind="ExternalInput").ap()
    sinkb = nc.dram_tensor("sinkb", [128, 16], F32, kind="ExternalInput").ap()
    lnp = nc.dram_tensor("lnp", [128, 2, 8], F32, kind="ExternalInput").ap()
    x2T = nc.dram_tensor("x2T", [1024, TC], F32, kind="ExternalOutput").ap()
    with ExitStack() as es:
        k = KC(nc, es)
        V, A, G, PE = k.V, k.A, k.G, k.PE
        k.consts()
        bk = [k.bank(f"bk{i}") for i in range(5)]
        btr = k.bank("btr", BF16)
        bl1 = k.bank("bl1"); bl2 = k.bank("bl2")
        gb = k.sb("gb", [128, 2, 8]); k.dma("sp", gb.t[:], lnp, "c0", writes=[gb])
        bq_s = k.sb("bq_s", [128, 8]); k.dma("sp", bq_s.t[:], bq, "c0", writes=[bq_s])
        bkv_s = k.sb("bkv_s", [128, 2]); k.dma("sp", bkv_s.t[:], bkv, "c0", writes=[bkv_s])
        bm = k.sb("bm", [128, 8, 512]); k.dma("sp", bm.t[:], biasT, "c2", writes=[bm])
        mk = k.sb("mk", [128, 512]); k.dma("sp", mk.t[:], maskT, "c0", writes=[mk])
        fm = k.sb("fm", [128, 512]); k.dma("sp", fm.t[:], fmT, "c0", writes=[fm])
        snk = k.sb("snk", [128, 16]); k.dma("sp", snk.t[:], sinkb, "c0", writes=[snk])
        esink = k.sb("esink", [128, 16])
        k.act(lambda: A.activation(out=esink.t[:], in_=snk.t[:], func=AF.Exp), reads=[snk], writes=[esink])
        bqs = k.sb("bqs", [128, 8])
        k.dve(lambda: V.tensor_scalar(out=bqs.t[:], in0=bq_s.t[:], scalar1=0.125, scalar2=None, op0=ALU.mult), reads=[bq_s], writes=[bqs])
        for c in range(8):
            k.pool(lambda: G.tensor_tensor(out=bm.t[:, c, :], in0=bm.t[:, c, :], in1=mk.t[:], op=ALU.add), reads=[bm, mk], writes=[bm])
        wq_b = k.sb("wq_b", [128, 8, 1024], BF16); k.dma("pool", wq_b.t[:], wq.rearrange("(c p) n -> p c n", p=128), "w0", writes=[wq_b])
        wkv_b = k.sb("wkv_b", [128, 8, 256], BF16); k.dma("pool", wkv_b.t[:], wkv.rearrange("(c p) n -> p c n", p=128), "w1", writes=[wkv_b])
        wo_b = k.sb("wo_b", [128, 8, 1024], BF16); k.dma("pool", wo_b.t[:], wo.rearrange("(c p) n -> p c n", p=128), "w2", writes=[wo_b])
        x1er = x1e.rearrange("(c p) t -> p c t", p=128)
        x2Tr = x2T.rearrange("(c p) t -> p c t", p=128)
        xbe = k.sb("xbe", [128, 8, TE], BF16); k.dma("pool", xbe.t[:], x1er, "w3", writes=[xbe])
        kTz = [k.sb(f"kTz{i}", [128, TE], BF16) for i in range(2)]
        for i in range(2):
            k.pool(lambda: G.memset(kTz[i].t[:], 0.0), writes=[kTz[i]])
        vTt = k.sb("vTt", [128, 512], BF16)
        vaug = k.sb("vaug", [128, NBE, 2, 80], BF16)
        k.pool(lambda: G.memset(vaug.t[:], 1.0), writes=[vaug])
        col = 0
        while col < TE:
            n = min(512, TE - col)
            cs = slice(col, col + n)
            k.mm(bk[0].t[:, 0:n], [(wkv_b.t[:, kc, 0:128], xbe.t[:, kc, cs]) for kc in range(8)], [wkv_b, xbe], [bk[0]])
            for kv in range(2):
                ps_ = slice(kv * 64, (kv + 1) * 64)
                k.act(lambda: A.activation(out=kTz[kv].t[ps_, cs], in_=bk[0].t[ps_, 0:n], func=AF.Identity, bias=bkv_s.t[ps_, 0:1]),
                      reads=[bk[0], bkv_s], writes=[kTz[kv]])
            k.mm(bk[1].t[:, 0:n], [(wkv_b.t[:, kc, 128:256], xbe.t[:, kc, cs]) for kc in range(8)], [wkv_b, xbe], [bk[1]])
            k.act(lambda: A.activation(out=vTt.t[:, 0:n], in_=bk[1].t[:, 0:n], func=AF.Identity, bias=bkv_s.t[:, 1:2]), reads=[bk[1], bkv_s], writes=[vTt])
            for j in range(n // 128):
                blk = col // 128 + j
                k.pe(lambda: PE.transpose(btr.t[:, 0:128], vTt.t[:, j * 128:(j + 1) * 128], k.identb.t[:]), reads=[vTt, k.identb], writes=[btr])
                k.act(lambda: A.copy(out=vaug.t[:, blk, :, 0:64], in_=btr.t[:, 0:128].rearrange("p (k d) -> p k d", k=2)), reads=[btr], writes=[vaug])
            col += n
        qT = k.sb("qT", [128, 8, 512], BF16)
        stop_here = (STOP3 == 1)
        attnT = k.sb("attnT", [128, 8, 512], BF16)
        sbs = [k.sb(f"sbs{i}", [128, 512]) for i in range(2)]
        pT = [k.sb(f"pT{i}", [128, 512], BF16) for i in range(2)]
        den = [k.sb(f"den{i}", [128, 2]) for i in range(2)]
        rinv = [k.sb(f"rinv{i}", [128, 2]) for i in range(2)]
        on = [k.sb(f"on{i}", [128, 128], BF16) for i in range(2)]
        xf = k.sb("xf", [128, 8, 512])
        y = k.sb("y", [128, 8, 512])
        k.ln_alloc()
        it = 0
        for tt in range(NT if not stop_here else 0):
            e0 = 128 + tt * 512
            for c in range(8):
                pb = bk[c % 2]
                k.mm(pb.t[:], [(wq_b.t[:, kc, c * 128:(c + 1) * 128], xbe.t[:, kc, e0:e0 + 512]) for kc in range(8)], [wq_b, xbe], [pb])
                k.act(lambda: A.activation(out=qT.t[:, c, :], in_=pb.t[:], func=AF.Identity, scale=0.125, bias=bqs.t[:, c:c + 1]),
                      reads=[pb, bqs], writes=[qT])
            if STOP3 == 2: break
            for bl in range(4):
                ne = 1 + tt * 4 + bl
                first = (tt == 0 and bl == 0)
                for c in range(8):
                    i = it % 2
                    it += 1
                    st = bk[2 + i]
                    def fsc():
                        last = None
                        for kv in range(2):
                            for half in range(2):
                                q0 = (kv * 2 + half) * 128
                                last = PE.matmul(st.t[:, q0:q0 + 128], lhsT=kTz[kv].t[:, (ne - 1 + half) * 128:(ne + half) * 128],
                                                 rhs=qT.t[:, c, bl * 128:(bl + 1) * 128], start=True, stop=True)
                        return last
                    k.pe(fsc, reads=[kTz[0], kTz[1], qT], writes=[st])
                    if STOP3 == 3: continue
                    S_ = sbs[i]
                    k.dve(lambda: V.tensor_tensor(out=S_.t[:], in0=st.t[:], in1=bm.t[:, c, :], op=ALU.add), reads=[st, bm], writes=[S_])
                    if first:
                        k.dve(lambda: V.tensor_tensor(out=S_.t[:], in0=S_.t[:], in1=fm.t[:], op=ALU.add), reads=[S_, fm], writes=[S_])
                    P_ = pT[i]
                    k.act(lambda: A.activation(out=P_.t[:], in_=S_.t[:], func=AF.Exp), reads=[S_], writes=[P_])
                    if STOP3 == 4: continue
                    ob = bk[4]
                    for kv in range(2):
                        k.mm(ob.t[:, kv * 128:kv * 128 + 65],
                             [(P_.t[:, (kv * 2 + half) * 128:(kv * 2 + half + 1) * 128], vaug.t[:, ne - 1 + half, kv, 0:65]) for half in range(2)],
                             [P_, vaug], [ob])
                    if STOP3 == 5: continue
                    D_ = den[i]; R_ = rinv[i]; O_ = on[i]
                    for kv in range(2):
                        k.act(lambda: A.activation(out=D_.t[:, kv:kv + 1], in_=ob.t[:, kv * 128 + 64:kv * 128 + 65], func=AF.Identity,
                                                   bias=esink.t[:, 2 * c + kv:2 * c + kv + 1]), reads=[ob, esink], writes=[D_])
                    k.dve(lambda: V.reciprocal(out=R_.t[:], in_=D_.t[:]), reads=[D_], writes=[R_])
                    for kv in range(2):
                        k.dve(lambda: V.tensor_scalar(out=O_.t[:, kv * 64:(kv + 1) * 64], in0=ob.t[:, kv * 128:kv * 128 + 64], scalar1=R_.t[:, kv:kv + 1],
                                                      scalar2=None, op0=ALU.mult), reads=[ob, R_], writes=[O_])
                    if STOP3 == 6: continue
                    k.pe(lambda: PE.transpose(btr.t[:, 0:128], O_.t[:], k.identb.t[:]), reads=[O_, k.identb], writes=[btr])
                    k.act(lambda: A.copy(out=attnT.t[:, c, bl * 128:(bl + 1) * 128], in_=btr.t[:, 0:128]), reads=[btr], writes=[attnT])
            k.dma("sp", xf.t[:], x1er[:, :, e0:e0 + 512], "xf", writes=[xf])
            for oc in range(8):
                pb = bk[oc % 2]
                k.mm(pb.t[:], [(wo_b.t[:, kc, oc * 128:(oc + 1) * 128], attnT.t[:, kc, :]) for kc in range(8)], [wo_b, attnT], [pb])
                k.dve(lambda: V.scalar_tensor_tensor(out=y.t[:, oc, :], in0=xf.t[:, oc, :], scalar=ALPHA, in1=pb.t[:], op0=ALU.mult, op1=ALU.add),
                      reads=[xf, pb], writes=[y])
            k.layernorm(y, slice(0, 512), gb, 0, 1, bl1, bl2, out_f=y, out_f_cols=slice(0, 512))
            k.dma("sp", x2Tr[:, :, tt * 512:(tt + 1) * 512], y.t[:], "out", reads=[y])
        k.finish(["out"])
        print("L3a instrs", k.fw.ninstr, "waits", k.fw.nwaits)
    return nc


def t5_bucket_np(dist):
    max_exact = 16
    df = np.maximum(dist, 1).astype(np.float32)
    large = max_exact + (np.log(df / max_exact) / math.log(128 / max_exact) * (32 - max_exact)).astype(np.int32)
    large = np.minimum(large, 31)
    return np.where(dist < max_exact, dist, large)
def swa_tables(rel_bias):
    s = np.arange(128)[:, None, None, None]
    kv = np.arange(2)[None, :, None, None]
    half = np.arange(2)[None, None, :, None]
    i = np.arange(128)[None, None, None, :]
    dist = i + 128 - half * 128 - s + 0 * kv
    valid = (dist >= 0) & (dist < 128)
    bucket = t5_bucket_np(np.maximum(dist, 0))
    biasT = np.zeros((128, 8, 2, 2, 128), np.float32)
    for c in range(8):
        for k_ in range(2):
            head = c + 8 * k_
            biasT[:, c, k_] = rel_bias[bucket[:, k_], head]
    maskT = np.where(valid, 0.0, -30000.0).astype(np.float32).reshape(128, 512)
    return biasT.reshape(128, 8, 512), maskT
def swa_inputs(x1e, b_w_in, b_b_in, b_sinks, b_w_out, rel_bias, ln_g, ln_b, first):
    perm = np.concatenate([np.concatenate([np.arange(c * 64, (c + 1) * 64), np.arange((8 + c) * 64, (9 + c) * 64)]) for c in range(8)])
    wq = np.ascontiguousarray(b_w_in[:, :1024][:, perm])
    bq = np.ascontiguousarray(b_b_in[:1024][perm].reshape(8, 128).T)
    wkv = np.ascontiguousarray(b_w_in[:, 1024:1280])
    bkv = np.ascontiguousarray(b_b_in[1024:1280].reshape(2, 128).T)
    wo = np.ascontiguousarray(b_w_out[perm, :])
    biasT, maskT = swa_tables(rel_bias)
    fm = np.zeros((128, 2, 2, 128), np.float32)
    if first:
        fm[:, :, 0, :] = -30000.0
    sink_order = np.array([c + 8 * k_ for c in range(8) for k_ in range(2)])
    sinkb = np.ascontiguousarray(np.broadcast_to(b_sinks[sink_order][None, :], (128, 16))).astype(np.float32)
    lnp = np.stack([ln_g, ln_b], 0).reshape(2, 8, 128).transpose(2, 0, 1).copy().astype(np.float32)
    return {"x1e": np.ascontiguousarray(x1e.T), "wq": wq, "bq": bq, "wkv": wkv, "bkv": bkv, "wo": wo, "biasT": biasT, "maskT": maskT,
            "fmT": fm.reshape(128, 512), "sinkb": sinkb, "lnp": lnp}


_NC_CACHE = {}


def _get_nc(name, builder, *args):
    if name not in _NC_CACHE:
        nc = bass.Bass("TRN2", target_bir_lowering=False)
        builder(nc, *args)
        _NC_CACHE[name] = nc
    return _NC_CACHE[name]


def _gdn_inputs(xb, j, w_in, conv_w, a_log, dt_bias, norm_w):
    q0 = 0; k0 = 512; v0 = 1024; z0 = 2048; b0 = 3072; a0 = 3080
    cols_qkv = np.concatenate([np.arange(q0 + j * 128, q0 + (j + 1) * 128), np.arange(k0 + j * 128, k0 + (j + 1) * 128),
                               np.arange(v0 + 2 * j * 128, v0 + (2 * j + 2) * 128)])
    cols_zba = np.concatenate([np.arange(z0 + 2 * j * 128, z0 + (2 * j + 2) * 128),
                               [b0 + 2 * j, b0 + 2 * j + 1, a0 + 2 * j, a0 + 2 * j + 1]])
    return {
        "xT": np.ascontiguousarray(xb.T),
        "wqkv": np.ascontiguousarray(w_in[:, cols_qkv]),
        "wzba": np.ascontiguousarray(w_in[:, cols_zba]),
        "convw": np.ascontiguousarray(conv_w[:, cols_qkv].T),
        "hp": np.array([[a_log[2 * j], a_log[2 * j + 1], dt_bias[2 * j], dt_bias[2 * j + 1]]], np.float32),
        "normw": np.ascontiguousarray(norm_w[None, :]).astype(np.float32),
    }


def _lncols(rows):
    return np.stack(rows, 0).reshape(len(rows), 8, 128).transpose(2, 0, 1).copy().astype(np.float32)


def kernel(x, a_w_in, a_conv_w, a_a_log, a_dt_bias, a_norm_w, a_w_out, b_w_in, b_b_in, b_sinks,
           b_w_out, rel_bias, ffn_w_gate, ffn_w_up, ffn_w_down, moe_router, moe_w_gate, moe_w_up,
           moe_w_down, ln_g, ln_b):
    f32 = lambda a: np.ascontiguousarray(np.asarray(a, dtype=np.float32))
    x = f32(x)
    B, T, D = x.shape
    NCORE = 8
    TC = (B * T) // NCORE
    per_b = T // TC
    cores = list(range(NCORE))
    ln_g = f32(ln_g); ln_b = f32(ln_b)

    nc1 = _get_nc("gdn", build_gdn, T)
    w_in = f32(a_w_in[0]); conv_w = f32(a_conv_w[0])
    a_log = f32(a_a_log[0]); dtb = f32(a_dt_bias[0]); nw = f32(a_norm_w[0])
    im1 = [_gdn_inputs(x[c // 4], c % 4, w_in, conv_w, a_log, dtb, nw) for c in cores]
    r1 = run_bass_kernel_spmd(nc1, im1, core_ids=cores).results
    og = np.empty((B, T, D), np.float32)
    for c in cores:
        og[c // 4, :, (c % 4) * 256:(c % 4 + 1) * 256] = r1[c]["og"]

    def tok(c):
        return c // per_b, (c % per_b) * TC

    nc2 = _get_nc("l2", build_l2, TC)
    wo0 = f32(a_w_out[0]); wg = f32(ffn_w_gate[0]); wu = f32(ffn_w_up[0]); wd = f32(ffn_w_down[0])
    lnp0 = _lncols([ln_g[0, 0], ln_b[0, 0], ln_g[0, 1], ln_b[0, 1]])
    im2 = []
    for c in cores:
        b, t0 = tok(c)
        im2.append({"xT": np.ascontiguousarray(x[b, t0:t0 + TC].T), "ogT": np.ascontiguousarray(og[b, t0:t0 + TC].T),
                    "wo": wo0, "lnp": lnp0, "wg": wg, "wu": wu, "wd": wd})
    r2 = run_bass_kernel_spmd(nc2, im2, core_ids=cores).results
    x1 = np.empty((B, T, D), np.float32)
    for c in cores:
        b, t0 = tok(c)
        x1[b, t0:t0 + TC] = r2[c]["outT"].T

    nc3 = _get_nc("l3a", build_l3a, TC)
    im3 = []
    for c in cores:
        b, t0 = tok(c)
        x1e = np.zeros((TC + 128, D), np.float32)
        if t0 > 0:
            x1e[:128] = x1[b, t0 - 128:t0]
        x1e[128:] = x1[b, t0:t0 + TC]
        im3.append(swa_inputs(x1e, f32(b_w_in[0]), f32(b_b_in[0]), f32(b_sinks[0]), f32(b_w_out[0]), f32(rel_bias),
                              ln_g[1, 0], ln_b[1, 0], t0 == 0))
    r3 = run_bass_kernel_spmd(nc3, im3, core_ids=cores).results

    nc4 = _get_nc("l3b", build_l3b, TC)
    wr = f32(moe_router[0]); mwg = f32(moe_w_gate[0]); mwu = f32(moe_w_up[0]); mwd = f32(moe_w_down[0])
    lnp1 = _lncols([ln_g[1, 1], ln_b[1, 1]])
    im4 = [{"x2T": np.ascontiguousarray(r3[c]["x2T"]), "wr": wr, "lnp": lnp1, "mwg": mwg, "mwu": mwu, "mwd": mwd} for c in cores]
    r4 = run_bass_kernel_spmd(nc4, im4, core_ids=cores).results
    out = np.empty((B, T, D), np.float32)
    for c in cores:
        b, t0 = tok(c)
        out[b, t0:t0 + TC] = r4[c]["outT"].T
    return out
```
